# Optimizing a Trainium2 kernel written in Bass

```python
import jax, jax.numpy as jnp
from jax import lax
import numpy as np

D_MODEL = 2048
BATCH = 4
SEQ = 2048
DEPTH = 1

D_MIX = D_MODEL
MLA_HEADS = 8
QK_NOPE_DIM = 128
QK_ROPE_DIM = 64
V_HEAD_DIM = 128
Q_LORA_RANK = 768
KV_LORA_RANK = 512
ROPE_THETA = 10000.0
MLA_WIDTH = MLA_HEADS * V_HEAD_DIM
CONV_CHANNELS = D_MIX - MLA_WIDTH
CONV_WIDTH = 31
Q_BLOCK = 128
IN_COLS = Q_LORA_RANK + KV_LORA_RANK + QK_ROPE_DIM + 2 * CONV_CHANNELS
N_EXPERT_GROUPS = 8
EXPERTS_PER_GROUP = 8
N_EXPERTS = N_EXPERT_GROUPS * EXPERTS_PER_GROUP
TOP_K = 2
D_EXPERT = 512
MOE_BLOCK = 128
EPS = 1e-6

kernel_name = 'hymba_mla_conformer_hiermoe_encoder'


def rms_norm(x, g):
    xf = x.astype(jnp.float32)
    y = xf * lax.rsqrt(jnp.mean(xf * xf, axis=-1, keepdims=True) + EPS)
    return (y * g.astype(jnp.float32)).astype(x.dtype)


def layer_norm(x, g, b):
    xf = x.astype(jnp.float32)
    mu = jnp.mean(xf, axis=-1, keepdims=True)
    var = jnp.mean(jnp.square(xf - mu), axis=-1, keepdims=True)
    y = (xf - mu) * lax.rsqrt(var + EPS)
    return (y * g.astype(jnp.float32) + b.astype(jnp.float32)).astype(x.dtype)


def rope_tables(seq):
    pos = jnp.arange(seq, dtype=jnp.float32)
    inv_freq = ROPE_THETA ** (-jnp.arange(0, QK_ROPE_DIM, 2, dtype=jnp.float32) / QK_ROPE_DIM)
    ang = pos[:, None] * inv_freq[None, :]
    return jnp.cos(ang), jnp.sin(ang)


def apply_rope(x, cos, sin):
    cos = cos.astype(x.dtype)
    sin = sin.astype(x.dtype)
    x1, x2 = jnp.split(x, 2, axis=-1)
    return jnp.concatenate([x1 * cos - x2 * sin, x2 * cos + x1 * sin], axis=-1)


def bidirectional_mla_attention(q_nope, q_rope, k_nope, k_rope, v):
    b, s, h, _ = q_nope.shape
    nb = s // Q_BLOCK
    scale = (QK_NOPE_DIM + QK_ROPE_DIM) ** -0.5

    def to_blocks(t):
        return t.reshape(b, nb, Q_BLOCK, *t.shape[2:]).swapaxes(0, 1)

    def block(args):
        qn, qr = args
        sc = jnp.einsum('bqhd,bkhd->bhqk', qn, k_nope) + jnp.einsum('bqhr,bkr->bhqk', qr, k_rope)
        p = jax.nn.softmax(sc.astype(jnp.float32) * scale, axis=-1).astype(v.dtype)
        return jnp.einsum('bhqk,bkhd->bqhd', p, v)

    o = lax.map(block, (to_blocks(q_nope), to_blocks(q_rope)))
    return o.swapaxes(0, 1).reshape(b, s, h * V_HEAD_DIM)


def conformer_conv_group(u, b_glu, w_dw, b_dw, ln_g, ln_b):
    u = u + b_glu
    a, gte = jnp.split(u, 2, axis=-1)
    c = a * jax.nn.sigmoid(gte)
    c = lax.conv_general_dilated(
        c, w_dw[:, None, :].astype(c.dtype), window_strides=(1,),
        padding=[(CONV_WIDTH // 2, CONV_WIDTH // 2)],
        dimension_numbers=('NWC', 'WIO', 'NWC'),
        feature_group_count=CONV_CHANNELS) + b_dw
    c = layer_norm(c, ln_g, ln_b)
    return jax.nn.silu(c)


def hierarchical_moe(h, w_group, b_group, w_router, b_router, w_gate, w_up, w_down):
    b, s, d = h.shape
    t = b * s
    xt = h.reshape(t, d)
    g_prob = jax.nn.softmax((xt @ w_group).astype(jnp.float32) + b_group.astype(jnp.float32), axis=-1)
    p_g, g_sel = lax.top_k(g_prob, 1)
    e_logits = ((xt @ w_router).astype(jnp.float32) + b_router.astype(jnp.float32)).reshape(t, N_EXPERT_GROUPS, EXPERTS_PER_GROUP)
    e_logits = jnp.take_along_axis(e_logits, g_sel[:, :, None], axis=1)[:, 0]
    e_prob = jax.nn.softmax(e_logits, axis=-1)
    top_w, top_i = lax.top_k(e_prob, TOP_K)
    top_w = top_w / jnp.sum(top_w, axis=-1, keepdims=True)
    gates = (p_g * top_w).reshape(-1)
    expert_ids = (g_sel * EXPERTS_PER_GROUP + top_i).reshape(-1)
    token_ids = jnp.repeat(jnp.arange(t, dtype=jnp.int32), TOP_K)
    n_assign = t * TOP_K
    n_blocks = (n_assign + N_EXPERTS * (MOE_BLOCK - 1) + MOE_BLOCK - 1) // MOE_BLOCK
    n_rows = n_blocks * MOE_BLOCK
    order = jnp.argsort(expert_ids)
    sorted_e = expert_ids[order]
    counts = jnp.bincount(expert_ids, length=N_EXPERTS)
    padded = (counts + MOE_BLOCK - 1) // MOE_BLOCK * MOE_BLOCK
    pad_end = jnp.cumsum(padded)
    pad_start = pad_end - padded
    start = jnp.cumsum(counts) - counts
    dest = pad_start[sorted_e] + jnp.arange(n_assign, dtype=jnp.int32) - start[sorted_e]
    row_token = jnp.full((n_rows,), t, jnp.int32).at[dest].set(token_ids[order])
    row_gate = jnp.zeros((n_rows,), gates.dtype).at[dest].set(gates[order])
    block_expert = jnp.minimum(
        jnp.searchsorted(pad_end, jnp.arange(n_blocks, dtype=pad_end.dtype) * MOE_BLOCK, side='right'),
        N_EXPERTS - 1)
    x_pad = jnp.concatenate([xt, jnp.zeros((1, d), xt.dtype)], axis=0)

    def expert_block(args):
        tok, gate, e = args
        xb = x_pad[tok]
        hb = jax.nn.silu(xb @ w_gate[e]) * (xb @ w_up[e])
        return (hb @ w_down[e]) * gate[:, None].astype(xb.dtype)

    y = lax.map(expert_block, (row_token.reshape(n_blocks, MOE_BLOCK),
                               row_gate.reshape(n_blocks, MOE_BLOCK), block_expert))
    out = jnp.zeros((t + 1, d), y.dtype).at[row_token].add(y.reshape(n_rows, d))[:t]
    return out.reshape(b, s, d).astype(h.dtype)


def setup_inputs(seed: int = 0) -> dict:
    key = jax.random.key(seed)
    ks = jax.random.split(key, 24)

    def w(k, shape, fan_in):
        return jax.random.normal(k, shape, jnp.float32) * (fan_in ** -0.5)

    def gain(k, shape):
        return 1.0 + 0.02 * jax.random.normal(k, shape, jnp.float32)

    def bias(k, shape, s=0.02):
        return s * jax.random.normal(k, shape, jnp.float32)

    L = DEPTH
    return {
        'x': jax.random.normal(ks[0], (BATCH, SEQ, D_MODEL), jnp.float32),
        'ln1_g': gain(ks[1], (L, D_MODEL)),
        'w_in': w(ks[2], (L, D_MODEL, IN_COLS), D_MODEL),
        'b_glu': bias(ks[3], (L, 2 * CONV_CHANNELS)),
        'q_norm_g': gain(ks[4], (L, Q_LORA_RANK)),
        'w_uq': w(ks[5], (L, Q_LORA_RANK, MLA_HEADS * (QK_NOPE_DIM + QK_ROPE_DIM)), Q_LORA_RANK),
        'kv_norm_g': gain(ks[6], (L, KV_LORA_RANK)),
        'w_ukv': w(ks[7], (L, KV_LORA_RANK, MLA_HEADS * (QK_NOPE_DIM + V_HEAD_DIM)), KV_LORA_RANK),
        'w_dw': w(ks[8], (L, CONV_WIDTH, CONV_CHANNELS), CONV_WIDTH),
        'b_dw': bias(ks[9], (L, CONV_CHANNELS)),
        'conv_ln_g': gain(ks[10], (L, CONV_CHANNELS)),
        'conv_ln_b': bias(ks[11], (L, CONV_CHANNELS)),
        'w_o': w(ks[12], (L, D_MIX, D_MODEL), D_MIX),
        'ln2_g': gain(ks[13], (L, D_MODEL)),
        'w_group': w(ks[14], (L, D_MODEL, N_EXPERT_GROUPS), D_MODEL),
        'b_group': bias(ks[15], (L, N_EXPERT_GROUPS), 0.01),
        'w_router': w(ks[16], (L, D_MODEL, N_EXPERTS), D_MODEL),
        'b_router': bias(ks[17], (L, N_EXPERTS), 0.01),
        'w_gate': w(ks[18], (L, N_EXPERTS, D_MODEL, D_EXPERT), D_MODEL),
        'w_up': w(ks[19], (L, N_EXPERTS, D_MODEL, D_EXPERT), D_MODEL),
        'w_down': w(ks[20], (L, N_EXPERTS, D_EXPERT, D_MODEL), D_EXPERT),
        'final_g': gain(ks[21], (D_MODEL,)),
    }


def reference(x, ln1_g, w_in, b_glu, q_norm_g, w_uq, kv_norm_g, w_ukv, w_dw, b_dw,
              conv_ln_g, conv_ln_b, w_o, ln2_g, w_group, b_group, w_router, b_router,
              w_gate, w_up, w_down, final_g):
    b, s, _ = x.shape
    cos, sin = rope_tables(s)
    split_at = [Q_LORA_RANK, Q_LORA_RANK + KV_LORA_RANK, Q_LORA_RANK + KV_LORA_RANK + QK_ROPE_DIM]
    h = x
    for l in range(DEPTH):
        n = rms_norm(h, ln1_g[l])
        proj = n @ w_in[l]
        q_lat, kv_lat, k_rope, u = jnp.split(proj, split_at, axis=-1)
        q = (rms_norm(q_lat, q_norm_g[l]) @ w_uq[l]).reshape(b, s, MLA_HEADS, QK_NOPE_DIM + QK_ROPE_DIM)
        q_nope, q_rope = jnp.split(q, [QK_NOPE_DIM], axis=-1)
        q_rope = apply_rope(q_rope, cos[:, None, :], sin[:, None, :])
        kv = (rms_norm(kv_lat, kv_norm_g[l]) @ w_ukv[l]).reshape(b, s, MLA_HEADS, QK_NOPE_DIM + V_HEAD_DIM)
        k_nope, v = jnp.split(kv, [QK_NOPE_DIM], axis=-1)
        k_rope = apply_rope(k_rope, cos, sin)
        attn = bidirectional_mla_attention(q_nope, q_rope, k_nope, k_rope, v)
        conv = conformer_conv_group(u, b_glu[l], w_dw[l], b_dw[l], conv_ln_g[l], conv_ln_b[l])
        h = h + jnp.concatenate([attn, conv], axis=-1) @ w_o[l]
        h = h + hierarchical_moe(rms_norm(h, ln2_g[l]), w_group[l], b_group[l], w_router[l],
                                 b_router[l], w_gate[l], w_up[l], w_down[l])
    return rms_norm(h, final_g)
```

```python
import contextlib
import numpy as np
import ml_dtypes
import concourse.bass as bass
import concourse.mybir as mybir
from concourse.bass_utils import run_bass_kernel_spmd

F32 = mybir.dt.float32
BF16 = mybir.dt.bfloat16
I32 = mybir.dt.int32
U8 = mybir.dt.uint8
AF = mybir.ActivationFunctionType
ALU = mybir.AluOpType
AX = mybir.AxisListType

NCORES = 8
D = 2048
S = 2048
TOWN = 1024
NEXT = TOWN + 32
NE = 64
CAP = 128
EPS = 1e-6
SCALE = 192.0 ** -0.5
SBUF_BYTES = 206 * 1024

ENGS = ("sync", "act", "pe", "dve", "pool")


class Tok:
    __slots__ = ("sem", "val", "rec", "eng")

    def __init__(self, sem=None, val=None, rec=None, eng=None):
        self.sem, self.val, self.rec, self.eng = sem, val, rec, eng


class Rec:
    __slots__ = ("eng", "fn", "waits", "sig", "kind", "key", "tok", "seq")
    _seq = 0

    def __init__(self, eng, fn, waits, sig, kind, key=None):
        self.eng, self.fn, self.waits, self.sig, self.kind, self.key = eng, fn, list(waits), sig, kind, key
        self.tok = Tok(rec=self, eng=eng)
        Rec._seq += 1
        self.seq = Rec._seq


def _ap_range(v):
    esz = {F32: 4, BF16: 2, I32: 4, U8: 1}.get(v.dtype, 4)
    dims = v.ap
    pitch = dims[0][0]
    off = v.offset % pitch if pitch > 0 else v.offset
    ext = 1
    for st, sz in dims[1:]:
        ext += abs(st) * (sz - 1)
    return (v.tensor.name, off * esz, (off + ext) * esz)


class _Dummy:
    hit = False
    reads = []
    writes = []

    def __getattr__(self, name):
        def f(*a, **k):
            for key, v in [(None, x) for x in a] + list(k.items()):
                if not hasattr(v, "space") or not hasattr(v, "ap"):
                    continue
                if str(v.space) == "PSUM":
                    _Dummy.hit = True
                r = _ap_range(v)
                if key is None:
                    _Dummy.reads.append(r)
                    _Dummy.writes.append(r)
                elif key in ("out", "accum_out"):
                    _Dummy.writes.append(r)
                else:
                    _Dummy.reads.append(r)
            return _Dummy()
        return f


def _overlap(xs, ys):
    for (t0, a0, b0) in xs:
        for (t1, a1, b1) in ys:
            if t0 == t1 and a0 < b1 and a1 < b0:
                return True
    return False


FENCED = ("act", "dve", "pool")


class Prog:
    def __init__(self, nc):
        self.nc = nc
        self.q = {e: [] for e in ENGS}
        self.dma_last = {}
        self.dma_eng = {}

    def op(self, eng, fn, waits=(), sig=False):
        r = Rec(eng, fn, [w for w in waits if w is not None], sig, "op")
        self.q[eng].append(r)
        return r.tok

    def dma(self, eng, fn, key, waits=()):
        assert self.dma_eng.setdefault(key, eng) == eng, key
        r = Rec(eng, fn, [w for w in waits if w is not None], True, "dma", key)
        self.q[eng].append(r)
        self.dma_last[key] = r.tok
        return r.tok

    def wait_only(self, eng, waits):
        r = Rec(eng, None, [w for w in waits if w is not None], False, "wait")
        self.q[eng].append(r)

    def last_tok(self, eng):
        for r in reversed(self.q[eng]):
            if r.kind == "op":
                r.sig = True
                return r.tok
        return None

    def barrier(self):
        toks = [self.last_tok(e) for e in ("act", "pe", "dve", "pool")]
        toks += list(self.dma_last.values())
        for e in ENGS:
            self.wait_only(e, toks)

    def emit(self, stack, ps_probe=None):
        nc = self.nc
        sems = {}
        if ps_probe is not None:
            recs = sorted([r for e in ("act", "dve") for r in self.q[e] if r.kind == "op"], key=lambda r: r.seq)
            last = {"act": None, "dve": None}
            for r in recs:
                if ps_probe(r.fn):
                    other = "dve" if r.eng == "act" else "act"
                    if last[other] is not None:
                        last[other].rec.sig = True
                        r.waits.append(last[other])
                    last[r.eng] = r.tok
        self.fence = {}
        for e in FENCED:
            hist = []
            for r in self.q[e]:
                if r.kind != "op":
                    continue
                _Dummy.reads, _Dummy.writes = [], []
                r.fn(_Dummy())
                rd, wr = _Dummy.reads, _Dummy.writes
                for pr, pw in hist[-2:]:
                    if _overlap(pw, rd) or _overlap(pw, wr):
                        prev = hist[-1][0]
                        prev.sig = True
                        self.fence[id(r)] = prev
                        break
                hist.append((r, wr))

        def getsem(name):
            if name not in sems:
                sems[name] = stack.enter_context(nc.semaphore(name))
            return sems[name]

        for e in ENGS:
            cnt = 0
            dcnt = {}
            for r in self.q[e]:
                if r.kind == "op" and r.sig:
                    cnt += 1
                    r.tok.sem, r.tok.val = "c_" + e, cnt
                elif r.kind == "dma":
                    dcnt[r.key] = dcnt.get(r.key, 0) + 16
                    r.tok.sem, r.tok.val = "d_" + r.key, dcnt[r.key]
        block = stack.enter_context(nc.Block())
        q = self.q

        def run(eng_name, e):
            waited = {}
            prev_cnt = 0
            for r in q[eng_name]:
                for w in r.waits:
                    if w.rec.kind == "op" and w.eng == eng_name and r.kind == "op":
                        continue
                    assert w.sem is not None, "wait on unsignalled op"
                    if waited.get(w.sem, 0) >= w.val:
                        continue
                    waited[w.sem] = w.val
                    e.wait_ge(getsem(w.sem), w.val)
                if r.fn is None:
                    continue
                if r.kind == "op" and id(r) in self.fence:
                    pv = self.fence[id(r)].tok.val
                    if waited.get("c_" + eng_name, 0) < pv:
                        waited["c_" + eng_name] = pv
                        e.wait_ge(getsem("c_" + eng_name), pv)
                ins = r.fn(e)
                if r.kind == "dma":
                    ins.then_inc(getsem(r.tok.sem), 16)
                elif r.sig:
                    ins.then_inc(getsem(r.tok.sem), 1)
                    prev_cnt = r.tok.val

        @block.sync
        def _(e):
            run("sync", e)

        @block.scalar
        def _(e):
            run("act", e)

        @block.tensor
        def _(e):
            run("pe", e)

        @block.vector
        def _(e):
            run("dve", e)

        @block.gpsimd
        def _(e):
            run("pool", e)


class Arena:
    def __init__(self):
        self.items = []

    def add(self, name, nbytes, p0, p1):
        self.items.append((name, (nbytes + 63) // 64 * 64, p0, p1))

    def solve(self, limit):
        placed = {}
        for name, nb, p0, p1 in sorted(self.items, key=lambda t: -t[1]):
            conflicts = sorted((o, o + b) for (o, b, q0, q1) in placed.values() if not (q1 < p0 or p1 < q0))
            off = 0
            for a, b in conflicts:
                if off + nb <= a:
                    break
                off = max(off, b)
            assert off + nb <= limit, f"SBUF overflow placing {name}: {off + nb} > {limit}"
            placed[name] = (off, nb, p0, p1)
        return {k: v[0] for k, v in placed.items()}


def build_program(debug=False, stop=99):
    nc = bass.Bass("TRN2", target_bir_lowering=False)
    dbg_out = {}

    def din(name, shape, dt=F32):
        return nc.dram_tensor(name, list(shape), dt, kind="ExternalInput").ap()

    xT = din("xT", [D, 2 * TOWN + 32])
    xown = din("xown", [TOWN, D])
    cosk = din("cosk", [64, S])
    sink = din("sink", [64, S])
    halo_mask = din("halo_mask", [128, 32])
    ident_d = din("ident", [128, 128])
    triu_d = din("triu", [128, 128])
    e128_d = din("e128", [128, NE])
    g1_d = din("g1t", [128, 16])
    bglu_d = din("bglut", [128, 16])
    qg_d = din("qgt", [128, 6])
    kvg_d = din("kvgt", [128, 4])
    wdw_d = din("wdwt", [128, 8, 31])
    bdw_d = din("bdwt", [128, 8])
    lng_d = din("lngt", [128, 8])
    lnb_d = din("lnbt", [128, 8])
    g2b_d = din("g2b", [128, D])
    fgb_d = din("fgb", [128, D])
    brb_d = din("brb", [128, 72])
    w_in = din("w_in", [D, 3392])
    w_uq = din("w_uq", [768, 1536])
    w_ukv = din("w_ukv", [512, 2048])
    w_o = din("w_o", [D, D])
    w_r = din("w_r", [D, 72])
    NEW = NE if stop >= 8 else 1
    w_gate = din("w_gate", [NEW, D, 512])
    w_up = din("w_up", [NEW, D, 512])
    w_down = din("w_down", [NEW, 512, D])
    out = nc.dram_tensor("out", [TOWN, D], F32, kind="ExternalOutput").ap()
    Xd = nc.dram_tensor("Xd", [NE * CAP, D], BF16).ap()
    Ybuf = nc.dram_tensor("Ybuf", [NE * CAP, D], F32).ap()
    hbuf = nc.dram_tensor("hbuf", [TOWN, D], F32).ap()

    def dbg(name, shape, dt):
        if debug:
            dbg_out[name] = nc.dram_tensor("dbg_" + name, list(shape), dt, kind="ExternalOutput").ap()
            return dbg_out[name]
        return None

    ar = Arena()
    A = ar.add
    A("ident_f", 512, 0, 9); A("ident_b", 256, 0, 9); A("ones_b", 256, 0, 9); A("ones_f", 512, 0, 9)
    A("triu_b", 256, 0, 9); A("triu_f", 512, 0, 0); A("e128", 256, 0, 9)
    A("g1t", 64, 0, 9); A("bglut", 64, 0, 9); A("qgt", 24, 0, 9); A("kvgt", 16, 0, 9)
    A("wdwt", 8 * 31 * 4, 0, 9); A("bdwt", 32, 0, 9); A("lngt", 32, 0, 9); A("lnbt", 32, 0, 9)
    A("hmask", 128, 0, 9); A("brb", 288, 0, 9); A("zero_b", 4096, 0, 9)
    A("cosk", S * 4, 0, 1); A("sink", S * 4, 0, 1); A("cosq", TOWN * 4, 0, 3); A("sinq", TOWN * 4, 0, 3)
    A("xg_own", 16 * NEXT * 2, 1, 2); A("xoth", 16 * 512 * 2, 1, 1); A("sq", 16 * 512 * 2, 1, 1)
    A("rstd_all", (2 * TOWN + 32) * 4, 1, 2); A("wkv", 16 * 640 * 2, 1, 1)
    A("kvlat", 4 * 512 * 4, 1, 1); A("sq2", 4 * 512 * 2, 1, 1); A("kro", 512 * 4, 1, 1); A("krs", 512 * 4, 1, 1)
    A("rstd2", 512 * 4, 1, 1); A("tmpa", 512 * 4, 1, 1)
    A("kvn", 4 * S * 2, 1, 4); A("kr", S * 2, 1, 5)
    A("wq0", 16 * 384 * 2, 2, 2); A("wq1", 16 * 384 * 2, 2, 2); A("wu0", 16 * 256 * 2, 2, 2); A("wu1", 16 * 256 * 2, 2, 2)
    A("qlat", 6 * 512 * 4, 2, 2); A("sqq", 6 * 512 * 2, 2, 2); A("t1", 512 * 4, 2, 2); A("t2", 512 * 4, 2, 2)
    A("sg", 512 * 4, 2, 2); A("rstdq", 512 * 4, 2, 2)
    A("qn", 6 * TOWN * 2, 2, 3); A("c_ext", 8 * NEXT * 2, 2, 3)
    A("AT", 16 * TOWN * 2, 3, 6)
    A("sum1", TOWN * 4, 3, 3); A("sum2", TOWN * 4, 3, 3)
    A("convf0", 512 * 4, 3, 3); A("convf1", 512 * 4, 3, 3); A("convsq0", 512 * 4, 3, 3); A("convsq1", 512 * 4, 3, 3)
    A("diag0", 31 * 128 * 2, 3, 3); A("diag1", 31 * 128 * 2, 3, 3)
    A("lnmean", TOWN * 4, 3, 3); A("lnrstd", TOWN * 4, 3, 3)
    A("wuq", 6 * 1536 * 2, 3, 3); A("wqs", 6 * 512 * 2, 3, 3); A("QN", 8 * TOWN * 2, 3, 5); A("QR", 8 * TOWN * 2, 3, 5)
    A("rt1", 512 * 4, 3, 3); A("rt2", 512 * 4, 3, 3)
    A("wukv", 4 * 2048 * 2, 4, 4); A("KT", 8 * S * 2, 4, 5); A("Vaug", 16 * 8 * 129 * 2 + 64, 4, 5)
    A("PT0", 16 * 512 * 2, 5, 5); A("PT1", 16 * 512 * 2, 5, 5); A("rcp", 64, 5, 5)
    A("Otok0", 256, 5, 5); A("Otok1", 256, 5, 5)
    A("wo0", 16 * 512 * 2, 6, 6); A("wo1", 16 * 512 * 2, 6, 6); A("h", 8 * D * 4, 6, 7)
    A("g2b", D * 4, 7, 7); A("hnf", D * 4, 7, 7); A("hnb", 8 * D * 2, 7, 7); A("hnT", 16 * 128 * 4, 7, 7)
    A("hnf1", D * 4, 7, 7); A("hnT1", 16 * 128 * 4, 7, 7)
    A("wr", 16 * 72 * 4, 7, 7); A("ssq7", 64, 7, 7); A("L", 8 * 72 * 4, 7, 7)
    for nm in ("gmax", "gsum", "pg", "m1", "m2", "w1", "w2", "r1", "r2", "v1", "v2", "d1", "d2"):
        A(nm, 32, 7, 7)
    for nm in ("gd", "gmask"):
        A(nm, 8 * 8 * 4, 7, 7)
    for nm in ("sel", "sel2", "mask1", "mask2"):
        A(nm, 8 * 8 * 4, 7, 7)
    for nm in ("rl4", "A1", "A2", "rank", "tmp64"):
        A(nm, 8 * 64 * 4, 7, 7)
    A("Abf", 8 * 64 * 2, 7, 7)
    A("dest_i", 16 * 4, 7, 9); A("gates", 16 * 4, 7, 9)
    for i in range(2):
        A(f"wg{i}", 16 * 512 * 2, 8, 8); A(f"wu_{i}", 16 * 512 * 2, 8, 8); A(f"wd{i}", 4 * D * 2, 8, 8)
        A(f"Xe{i}", D * 2, 8, 8); A(f"XT{i}", 16 * 128 * 2, 8, 8); A(f"sgt{i}", 512 * 4, 8, 8)
        A(f"hb{i}", 512 * 2, 8, 8); A(f"HT{i}", 4 * 128 * 2, 8, 8); A(f"Ysb{i}", D * 4, 8, 8)
        A(f"wdf{i}", 2 * D * 4, 8, 8)
    A("fgb", D * 4, 9, 9)
    for i in range(2):
        A(f"Y1_{i}", D * 4, 9, 9); A(f"Y2_{i}", D * 4, 9, 9); A(f"hz{i}", D * 4, 9, 9); A(f"ss9_{i}", 64, 9, 9)
    offs = ar.solve(SBUF_BYTES)
    sizes = {n: b for (n, b, _, _) in ar.items}

    stack = contextlib.ExitStack()
    arena = stack.enter_context(nc.sbuf_tensor("arena", [128, SBUF_BYTES], U8))
    banks = [stack.enter_context(nc.psum_tensor(f"ps{i}", [128, 512], F32)) for i in range(8)]

    def V(name, dt, shape=None, parts=128):
        nb = sizes[name]
        esz = {F32: 4, BF16: 2, I32: 4}[dt]
        ap = arena[0:parts, offs[name]:offs[name] + nb].bitcast(dt)
        if shape is None:
            return ap
        n = int(np.prod(shape))
        ap = ap[:, 0:n]
        if len(shape) == 1:
            return ap
        if len(shape) == 2:
            return ap.rearrange("p (a b) -> p a b", b=shape[1])
        if len(shape) == 3:
            return ap.rearrange("p (a b c) -> p a b c", b=shape[1], c=shape[2])
        raise ValueError

    ps_flag = [False]

    def ps_probe(fn):
        _Dummy.hit = False
        fn(_Dummy())
        return _Dummy.hit

    def PS(i, dt=F32, parts=128):
        ps_flag[0] = True
        ap = banks[i][0:parts, :]
        return ap if dt == F32 else ap.bitcast(dt)

    def emit_rstd(dst, src, inv_n, waits=()):
        t_a = P.op("dve", lambda e: e.tensor_scalar(out=dst, in0=src, scalar1=inv_n, scalar2=EPS, op0=ALU.mult, op1=ALU.add),
                   waits=list(waits), sig=True)
        t_b = P.op("act", lambda e: e.activation(out=dst, in_=dst, func=AF.Sqrt), waits=[t_a], sig=True)
        t_c = P.op("dve", lambda e: e.reciprocal(out=dst, in_=dst), waits=[t_b], sig=True)
        return t_a, t_c

    P = Prog(nc)

    def finish():
        P.barrier()
        fin = [P.dma_last[k] for k in ("out0", "out1", "dbg") if k in P.dma_last]
        P.wait_only("sync", fin)
        P.emit(stack, ps_probe)
        stack.close()
        return nc, list(dbg_out.keys())

    def dump(name, ap, shape, dt):
        if debug:
            d = dbg(name, shape, dt)
            P.dma("sync", lambda e: e.dma_start(out=d, in_=ap), "dbg")

    ident_f = V("ident_f", F32, [128]); ident_b = V("ident_b", BF16, [128])
    ones_b = V("ones_b", BF16, [128]); ones_f = V("ones_f", F32, [128])
    triu_b = V("triu_b", BF16, [128]); triu_f = V("triu_f", F32, [128]); e128 = V("e128", F32, [NE])
    g1t = V("g1t", F32, [16]); bglut = V("bglut", F32, [16]); qgt = V("qgt", F32, [6]); kvgt = V("kvgt", F32, [4])
    wdwt = V("wdwt", F32, [8, 31]); bdwt = V("bdwt", F32, [8]); lngt = V("lngt", F32, [8]); lnbt = V("lnbt", F32, [8])
    hmask = V("hmask", F32, [32]); brb = V("brb", F32, [72]); zero_b = V("zero_b", BF16, [2048])
    cosk_t = V("cosk", F32, [S], parts=64); sink_t = V("sink", F32, [S], parts=64)
    cosq_t = V("cosq", F32, [TOWN], parts=64); sinq_t = V("sinq", F32, [TOWN], parts=64)

    cl = []
    for dst, src in ((ident_f, ident_d), (triu_f, triu_d), (e128, e128_d), (g1t, g1_d), (bglut, bglu_d), (qgt, qg_d),
                     (kvgt, kvg_d), (wdwt, wdw_d), (bdwt, bdw_d), (lngt, lng_d), (lnbt, lnb_d), (hmask, halo_mask),
                     (brb, brb_d), (cosk_t, cosk), (sink_t, sink), (cosq_t, cosk[:, 0:TOWN]), (sinq_t, sink[:, 0:TOWN])):
        cl.append(P.dma("sync", (lambda d, s: (lambda e: e.dma_start(out=d, in_=s)))(dst, src), "const"))
    tc0 = cl[-1]
    P.op("dve", lambda e: e.tensor_copy(out=ident_b, in_=ident_f), waits=[tc0])
    P.op("dve", lambda e: e.tensor_copy(out=triu_b, in_=triu_f))
    P.op("dve", lambda e: e.memset(ones_b, 1.0))
    P.op("dve", lambda e: e.memset(ones_f, 1.0))
    P.op("dve", lambda e: e.memset(zero_b, 0.0))
    P.barrier()
    if stop <= 0:
        return finish()

    xg_own = V("xg_own", BF16, [16, NEXT]); xoth = V("xoth", BF16, [16, 512]); sq = V("sq", BF16, [16, 512])
    rstd_all = V("rstd_all", F32, [2 * TOWN + 32]); wkv = V("wkv", BF16, [16, 640])
    kvlat = V("kvlat", F32, [4, 512]); sq2 = V("sq2", BF16, [4, 512])
    kro = V("kro", F32, [512], parts=64); krs = V("krs", F32, [512], parts=64)
    rstd2 = V("rstd2", F32, [512]); tmpa = V("tmpa", F32, [512])
    kvn = V("kvn", BF16, [4, S]); kr = V("kr", BF16, [S], parts=64)
    xTv = xT.rearrange("(kc p) t -> p kc t", p=128)
    w_in_v = w_in.rearrange("(kc p) c -> p kc c", p=128)

    t_wkv = P.dma("pool", lambda e: e.dma_start(out=wkv[:, :, 0:576], in_=w_in_v[:, :, 768:1344]), "wkv")
    t_sw = P.op("act", lambda e: e.copy(out=wkv[:, :, 576:608], in_=wkv[:, :, 544:576]), waits=[t_wkv])
    t_sw = P.op("act", lambda e: e.copy(out=wkv[:, :, 608:640], in_=wkv[:, :, 512:544]), sig=True)

    blocks = [(0, 512, xg_own[:, :, 0:512], True), (512, 512, xg_own[:, :, 512:1024], True),
              (1024, 512, xoth, True), (1536, 512, xoth, True), (2048, 32, xg_own[:, :, 1024:1056], False)]
    xoth_free = None
    sq_free = None
    ps_free = [None] * 8
    for bi, (c0, nb, xb, is_kv) in enumerate(blocks):
        key = f"xblk{bi}" if bi != 3 else "xblk2"
        tx = P.dma("pool", (lambda xb=xb, c0=c0, nb=nb: (lambda e: e.dma_start(out=xb, in_=xTv[:, :, c0:c0 + nb])))(),
                   key, waits=[xoth_free] if bi == 3 else [])
        sqv = sq[:, :, 0:nb]
        tsq = P.op("act", (lambda xb=xb, sqv=sqv: (lambda e: e.activation(out=sqv, in_=xb, func=AF.Square)))(),
                   waits=[tx, sq_free], sig=True)
        for kc in range(16):
            tpe = P.op("pe", (lambda kc=kc, sqv=sqv, nb=nb: (lambda e: e.matmul(PS(0)[:, 0:nb], ones_b, sqv[:, kc, :],
                       start=(kc == 0), stop=(kc == 15))))(), waits=[tsq, ps_free[0]] if kc == 0 else [], sig=(kc == 15))
        sq_free = tpe
        rs = rstd_all[:, c0:c0 + nb]
        ps_free[0], t_rs = emit_rstd(rs, PS(0)[:, 0:nb], 1.0 / D, waits=[tpe])
        for kc in range(16):
            t_xg = P.op("dve", (lambda xb=xb, kc=kc: (lambda e: e.tensor_scalar(out=xb[:, kc, :], in0=xb[:, kc, :],
                        scalar1=g1t[:, kc:kc + 1], scalar2=None, op0=ALU.mult)))(), waits=[tsq] if kc == 0 else [], sig=(kc == 15))
        if not is_kv:
            continue
        for m in range(4):
            for kc in range(16):
                tpe = P.op("pe", (lambda m=m, kc=kc, xb=xb: (lambda e: e.matmul(PS(1 + m), wkv[:, kc, 128 * m:128 * m + 128],
                           xb[:, kc, :], start=(kc == 0), stop=(kc == 15))))(),
                           waits=[t_xg, t_sw, ps_free[1 + m]] if kc == 0 else [], sig=(kc == 15))
            ps_free[1 + m] = P.op("dve", (lambda m=m, rs=rs: (lambda e: e.tensor_tensor(out=kvlat[:, m, :], in0=PS(1 + m), in1=rs,
                                  op=ALU.mult)))(), waits=[tpe, t_rs], sig=True)
        t_kvl = ps_free[4]
        for j, (cc, dst) in enumerate(((512, kro), (576, krs))):
            for kc in range(16):
                tpe = P.op("pe", (lambda j=j, kc=kc, cc=cc, xb=xb: (lambda e: e.matmul(PS(5 + j, parts=64), wkv[:, kc, cc:cc + 64],
                           xb[:, kc, :], start=(kc == 0), stop=(kc == 15))))(),
                           waits=[ps_free[5 + j]] if kc == 0 else [], sig=(kc == 15))
            ps_free[5 + j] = P.op("dve", (lambda j=j, dst=dst, rs=rs: (lambda e: e.tensor_tensor(out=dst, in0=PS(5 + j, parts=64),
                                  in1=rs[0:64, :], op=ALU.mult)))(), waits=[tpe], sig=True)
        if bi == 2:
            xoth_free = tpe
        P.op("dve", (lambda c0=c0: (lambda e: e.tensor_tensor(out=kro, in0=kro, in1=cosk_t[:, c0:c0 + 512], op=ALU.mult)))())
        P.op("dve", (lambda c0=c0: (lambda e: e.tensor_tensor(out=krs, in0=krs, in1=sink_t[:, c0:c0 + 512], op=ALU.mult)))())
        P.op("dve", (lambda c0=c0: (lambda e: e.tensor_tensor(out=kr[:, c0:c0 + 512], in0=kro, in1=krs, op=ALU.add)))())
        tsq2 = P.op("act", lambda e: e.activation(out=sq2, in_=kvlat, func=AF.Square), waits=[t_kvl], sig=True)
        for m in range(4):
            tpe = P.op("pe", (lambda m=m: (lambda e: e.matmul(PS(7), ones_b, sq2[:, m, :], start=(m == 0), stop=(m == 3))))(),
                       waits=[tsq2, ps_free[7]] if m == 0 else [], sig=(m == 3))
        ps_free[7], _t = emit_rstd(rstd2, PS(7), 1.0 / 512, waits=[tpe])
        for m in range(4):
            P.op("dve", (lambda m=m, c0=c0: (lambda e: e.scalar_tensor_tensor(out=kvn[:, m, c0:c0 + 512], in0=kvlat[:, m, :],
                 scalar=kvgt[:, m:m + 1], in1=rstd2, op0=ALU.mult, op1=ALU.mult)))())
    P.barrier()
    dump("kvn", kvn, [128, 4, S], BF16)
    dump("kr", kr, [64, S], BF16)
    dump("rstd_all", rstd_all, [128, 2 * TOWN + 32], F32)
    dump("xg_own", xg_own, [128, 16, NEXT], BF16)
    if stop <= 1:
        return finish()

    wqb = [V("wq0", BF16, [16, 384]), V("wq1", BF16, [16, 384])]
    wub = [V("wu0", BF16, [16, 256]), V("wu1", BF16, [16, 256])]
    qlat = V("qlat", F32, [6, 512]); sqq = V("sqq", BF16, [6, 512])
    t1 = V("t1", F32, [512]); t2 = V("t2", F32, [512]); sgv = V("sg", F32, [512]); rstdq = V("rstdq", F32, [512])
    qn = V("qn", BF16, [6, TOWN]); c_ext = V("c_ext", BF16, [8, NEXT])
    ps_free = [None] * 8
    t_wq = [P.dma("pool", (lambda i=i: (lambda e: e.dma_start(out=wqb[i], in_=w_in_v[:, :, 384 * i:384 * i + 384])))(), f"wq{i}")
            for i in range(2)]
    t_qn_done = None
    for blk in range(2):
        tk = slice(512 * blk, 512 * blk + 512)
        for m in range(6):
            bank = m % 4
            for kc in range(16):
                tpe = P.op("pe", (lambda m=m, kc=kc, bank=bank, tk=tk: (lambda e: e.matmul(PS(bank), wqb[m // 3][:, kc, 128 * (m % 3):128 * (m % 3) + 128],
                           xg_own[:, kc, tk], start=(kc == 0), stop=(kc == 15))))(),
                           waits=[t_wq[m // 3], ps_free[bank]] if kc == 0 else [], sig=(kc == 15))
            ps_free[bank] = P.op("dve", (lambda m=m, bank=bank, tk=tk: (lambda e: e.tensor_tensor(out=qlat[:, m, :], in0=PS(bank),
                                 in1=rstd_all[:, tk], op=ALU.mult)))(), waits=[tpe, t_qn_done], sig=True)
        tsq = P.op("act", lambda e: e.activation(out=sqq, in_=qlat, func=AF.Square), waits=[ps_free[1]], sig=True)
        for m in range(6):
            tpe = P.op("pe", (lambda m=m: (lambda e: e.matmul(PS(4), ones_b, sqq[:, m, :], start=(m == 0), stop=(m == 5))))(),
                       waits=[tsq, ps_free[4]] if m == 0 else [], sig=(m == 5))
        ps_free[4], _t = emit_rstd(rstdq, PS(4), 1.0 / 768, waits=[tpe])
        for m in range(6):
            t_qn_done = P.op("dve", (lambda m=m, tk=tk: (lambda e: e.scalar_tensor_tensor(out=qn[:, m, tk], in0=qlat[:, m, :],
                             scalar=qgt[:, m:m + 1], in1=rstdq, op0=ALU.mult, op1=ALU.mult)))(), sig=(m == 5))
    wu_free = [None, None]
    ublocks = [(0, 512, 16), (512, 512, 16 + 512), (1024, 16, 0), (1040, 16, 16 + 1024)]
    for j in range(8):
        wb = wub[j % 2]
        ta = P.dma("pool", (lambda wb=wb, j=j: (lambda e: e.dma_start(out=wb[:, :, 0:128], in_=w_in_v[:, :, 1344 + 128 * j:1344 + 128 * j + 128])))(),
                   f"wua{j % 2}", waits=[wu_free[j % 2]])
        tg = P.dma("pool", (lambda wb=wb, j=j: (lambda e: e.dma_start(out=wb[:, :, 128:256], in_=w_in_v[:, :, 2368 + 128 * j:2368 + 128 * j + 128])))(),
                   f"wug{j % 2}", waits=[wu_free[j % 2]])
        for ui, (xc, n, cc) in enumerate(ublocks):
            ba, bg = (5, 6) if ui % 2 == 0 else (7, 0)
            for half, bank in ((0, ba), (1, bg)):
                for kc in range(16):
                    tpe = P.op("pe", (lambda wb=wb, half=half, bank=bank, kc=kc, xc=xc, n=n: (lambda e: e.matmul(PS(bank)[:, 0:n],
                               wb[:, kc, 128 * half:128 * half + 128], xg_own[:, kc, xc:xc + n], start=(kc == 0), stop=(kc == 15))))(),
                               waits=[ta, tg, ps_free[bank]] if kc == 0 else [], sig=(kc == 15))
                if half == 0:
                    tpa = tpe
            rsl = rstd_all[:, xc:xc + n] if xc < 1024 else rstd_all[:, 2048 + (xc - 1024):2048 + (xc - 1024) + n]
            ps_free[ba] = P.op("dve", (lambda ba=ba, n=n, rsl=rsl: (lambda e: e.tensor_tensor(out=t1[:, 0:n], in0=PS(ba)[:, 0:n], in1=rsl,
                               op=ALU.mult)))(), waits=[tpa], sig=True)
            ps_free[bg] = P.op("dve", (lambda bg=bg, n=n, rsl=rsl: (lambda e: e.tensor_tensor(out=t2[:, 0:n], in0=PS(bg)[:, 0:n], in1=rsl,
                               op=ALU.mult)))(), waits=[tpe], sig=True)
            tsg = P.op("act", (lambda n=n, j=j: (lambda e: e.activation(out=sgv[:, 0:n], in_=t2[:, 0:n], func=AF.Sigmoid,
                       bias=bglut[:, 8 + j:9 + j], scale=1.0)))(), waits=[ps_free[bg]], sig=True)
            P.op("dve", (lambda n=n, j=j, cc=cc: (lambda e: e.scalar_tensor_tensor(out=c_ext[:, j, cc:cc + n], in0=t1[:, 0:n],
                 scalar=bglut[:, j:j + 1], in1=sgv[:, 0:n], op0=ALU.add, op1=ALU.mult)))(), waits=[tsg])
            if ui >= 2:
                hm = hmask[:, 0:16] if ui == 2 else hmask[:, 16:32]
                P.op("dve", (lambda n=n, j=j, cc=cc, hm=hm: (lambda e: e.tensor_tensor(out=c_ext[:, j, cc:cc + n],
                     in0=c_ext[:, j, cc:cc + n], in1=hm, op=ALU.mult)))())
        wu_free[j % 2] = tpe
    P.barrier()
    dump("qn", qn, [128, 6, TOWN], BF16)
    dump("c_ext", c_ext, [128, 8, NEXT], BF16)
    if stop <= 2:
        return finish()

    AT = V("AT", BF16, [16, TOWN])
    sum1 = V("sum1", F32, [TOWN]); sum2 = V("sum2", F32, [TOWN])
    convf = [V("convf0", F32, [512]), V("convf1", F32, [512])]
    convsq = [V("convsq0", F32, [512]), V("convsq1", F32, [512])]
    diag = [V("diag0", BF16, [31, 128]), V("diag1", BF16, [31, 128])]
    lnmean = V("lnmean", F32, [TOWN]); lnrstd = V("lnrstd", F32, [TOWN])
    wuq = V("wuq", BF16, [6, 1536]); wqs = V("wqs", BF16, [6, 512])
    QN = V("QN", BF16, [8, TOWN]); QR = V("QR", BF16, [8, TOWN], parts=64)
    rt1 = V("rt1", F32, [512], parts=64); rt2 = V("rt2", F32, [512], parts=64)
    w_uq_v = w_uq.rearrange("(kc p) c -> p kc c", p=128)
    KDC = ""
    if "w" in KDC:
        t_wuq = None; t_wqs = None
    else:
        t_wuq = P.dma("pool", lambda e: e.dma_start(out=wuq, in_=w_uq_v), "wuq")
    wuq4 = wuq.rearrange("p k (h c) -> p k h c", c=192)
    wqs4 = wqs.rearrange("p k (h c) -> p k h c", c=64)
    for kc in range(6):
        if "w" in KDC:
            break
        P.op("act", (lambda kc=kc: (lambda e: e.copy(out=wqs4[:, kc, :, 0:32], in_=wuq4[:, kc, :, 160:192])))(), waits=[t_wuq])
        t_wqs = P.op("act", (lambda kc=kc: (lambda e: e.copy(out=wqs4[:, kc, :, 32:64], in_=wuq4[:, kc, :, 128:160])))(), sig=True)
    qp_free = [None] * 4

    def emit_qproj(u):
        h, blk = u // 2, u % 2
        tk = slice(512 * blk, 512 * blk + 512)
        bank = u % 2
        for kc in range(6):
            tpe = P.op("pe", (lambda h=h, kc=kc, tk=tk, bank=bank: (lambda e: e.matmul(PS(bank), wuq[:, kc, 192 * h:192 * h + 128],
                       qn[:, kc, tk], start=(kc == 0), stop=(kc == 5))))(), waits=[t_wuq, qp_free[bank]] if kc == 0 else [], sig=(kc == 5))
        qp_free[bank] = P.op("act", (lambda h=h, tk=tk, bank=bank: (lambda e: e.copy(out=QN[:, h, tk], in_=PS(bank))))(),
                             waits=[tpe], sig=True)
        for kc in range(6):
            tpr = P.op("pe", (lambda h=h, kc=kc, tk=tk: (lambda e: e.matmul(PS(2, parts=64), wuq[:, kc, 192 * h + 128:192 * h + 192],
                       qn[:, kc, tk], start=(kc == 0), stop=(kc == 5))))(), waits=[qp_free[2]] if kc == 0 else [], sig=(kc == 5))
        for kc in range(6):
            tps = P.op("pe", (lambda h=h, kc=kc, tk=tk: (lambda e: e.matmul(PS(3, parts=64), wqs[:, kc, 64 * h:64 * h + 64],
                       qn[:, kc, tk], start=(kc == 0), stop=(kc == 5))))(), waits=[t_wqs, qp_free[3]] if kc == 0 else [], sig=(kc == 5))
        qp_free[2] = P.op("dve", (lambda tk=tk: (lambda e: e.tensor_tensor(out=rt1, in0=PS(2, parts=64), in1=cosq_t[:, tk], op=ALU.mult)))(),
                          waits=[tpr], sig=True)
        qp_free[3] = P.op("dve", (lambda tk=tk: (lambda e: e.tensor_tensor(out=rt2, in0=PS(3, parts=64), in1=sinq_t[:, tk], op=ALU.mult)))(),
                          waits=[tps], sig=True)
        P.op("dve", (lambda h=h, tk=tk: (lambda e: e.tensor_tensor(out=QR[:, h, tk], in0=rt1, in1=rt2, op=ALU.add)))())

    PENG = "pool"
    P.op(PENG, lambda e: e.memset(sum1, 0.0))
    P.op(PENG, lambda e: e.memset(sum2, 0.0))
    diag_free = [None, None]
    cv_free = [None, None]
    cbank_free = [[], [], [], []]
    ci = 0
    for j in range(8):
        db = diag[j % 2]
        t_dg = P.op(PENG, (lambda db=db, j=j: (lambda e: e.tensor_tensor(out=db, in0=ident_f.unsqueeze(1).to_broadcast([128, 31, 128]),
                    in1=wdwt[:, j, :].unsqueeze(2).to_broadcast([128, 31, 128]), op=ALU.mult)))(), waits=[diag_free[j % 2]], sig=True)
        for blk in range(2):
            bank = 4 + ci % 4
            cb = ci % 2
            ci += 1
            sl = slice(512 * blk, 512 * blk + 512)
            for k in range(31):
                if "m" in KDC:
                    tpe = None
                    break
                c0 = k + 1 + 512 * blk
                if None:
                    c0 = (c0 // 2) * 2
                tpe = P.op("pe", (lambda db=db, j=j, k=k, c0=c0, bank=bank: (lambda e: e.matmul(PS(bank), db[:, k, :], c_ext[:, j, c0:c0 + 512],
                           start=(k == 0), stop=(k == 30))))(), waits=([t_dg] + cbank_free[bank - 4]) if k == 0 else [], sig=(k == 30))
            t_cf = None if "d" in KDC else P.op("dve", (lambda j=j, cb=cb, bank=bank: (lambda e: e.tensor_scalar(out=convf[cb], in0=PS(bank), scalar1=bdwt[:, j:j + 1],
                        scalar2=None, op0=ALU.add)))(), waits=[tpe, cv_free[cb]], sig=True)
            if "a" in KDC:
                cbank_free[bank - 4] = [t_cf]
                continue
            t_at = P.op("act", (lambda j=j, sl=sl, bank=bank: (lambda e: e.activation(out=AT[:, 8 + j, sl], in_=PS(bank), func=AF.Identity,
                        bias=bdwt[:, j:j + 1], scale=1.0)))(), waits=[tpe])
            t_sq = P.op("act", (lambda j=j, cb=cb, bank=bank: (lambda e: e.activation(out=convsq[cb], in_=PS(bank), func=AF.Square,
                        bias=bdwt[:, j:j + 1], scale=1.0)))(), waits=[cv_free[cb]], sig=True)
            cbank_free[bank - 4] = [t_cf, t_sq]
            if "p" in KDC:
                continue
            P.op(PENG, (lambda cb=cb, sl=sl: (lambda e: e.tensor_tensor(out=sum1[:, sl], in0=sum1[:, sl], in1=convf[cb], op=ALU.add)))(),
                 waits=[t_cf])
            cv_free[cb] = P.op(PENG, (lambda cb=cb, sl=sl: (lambda e: e.tensor_tensor(out=sum2[:, sl], in0=sum2[:, sl], in1=convsq[cb],
                               op=ALU.add)))(), waits=[t_sq], sig=True)
        diag_free[j % 2] = tpe
        if not None:
            emit_qproj(2 * j)
            emit_qproj(2 * j + 1)
    if None == "1":
        return finish()
    for blk in range(2):
        for which, src in ((0, sum1), (1, sum2)):
            bank = 4 + 2 * which + blk
            pe_ln = P.op("pe", (lambda blk=blk, src=src, bank=bank: (lambda e: e.matmul(PS(bank), ones_f,
                         src[:, 512 * blk:512 * blk + 512], start=True, stop=True)))(),
                         waits=[cv_free[0], cv_free[1]] + cbank_free[bank - 4], sig=True)
    if None == "2":
        return finish()
    for blk in range(2):
        sl = slice(512 * blk, 512 * blk + 512)
        P.op("dve", (lambda blk=blk, sl=sl: (lambda e: e.tensor_scalar(out=lnmean[:, sl], in0=PS(4 + blk), scalar1=1.0 / 1024, scalar2=None,
             op0=ALU.mult)))(), waits=[pe_ln])
        P.op("dve", (lambda blk=blk, sl=sl: (lambda e: e.tensor_scalar(out=lnrstd[:, sl], in0=PS(6 + blk), scalar1=1.0 / 1024, scalar2=None,
             op0=ALU.mult)))())
    P.op("dve", lambda e: e.tensor_tensor(out=sum1, in0=lnmean, in1=lnmean, op=ALU.mult))
    P.op("dve", lambda e: e.tensor_tensor(out=sum2, in0=lnrstd, in1=sum1, op=ALU.subtract))
    _t, t_lr = emit_rstd(lnrstd, sum2, 1.0)
    ybufs = [sum1, sum2]
    y_free = [None, None]
    for j in range(8):
        yb = ybufs[j % 2]
        P.op("dve", (lambda j=j, yb=yb: (lambda e: e.tensor_tensor(out=yb, in0=AT[:, 8 + j, :], in1=lnmean, op=ALU.subtract)))(),
             waits=[y_free[j % 2], t_lr])
        ty = P.op("dve", (lambda yb=yb: (lambda e: e.tensor_tensor(out=yb, in0=yb, in1=lnrstd, op=ALU.mult)))(), sig=True)
        y_free[j % 2] = P.op("act", (lambda j=j, yb=yb: (lambda e: e.activation(out=AT[:, 8 + j, :], in_=yb, func=AF.Silu,
                             bias=lnbt[:, j:j + 1], scale=lngt[:, j:j + 1])))(), waits=[ty], sig=True)
    P.barrier()
    dump("AT3", AT, [128, 16, TOWN], BF16)
    dump("QN", QN, [128, 8, TOWN], BF16)
    dump("QR", QR, [64, 8, TOWN], BF16)
    if stop <= 3:
        return finish()

    wukv = V("wukv", BF16, [4, 2048]); KT = V("KT", BF16, [8, S])
    Vaug = V("Vaug", BF16, [16 * 8 * 129 + 32])[:, 0:16 * 8 * 129].rearrange("p (k h c) -> p k h c", h=8, c=129)
    w_ukv_v = w_ukv.rearrange("(kc p) c -> p kc c", p=128)
    t_wukv = P.dma("pool", lambda e: e.dma_start(out=wukv, in_=w_ukv_v), "wukv")
    for kc16 in range(16):
        P.op("pool", (lambda kc16=kc16: (lambda e: e.memset(Vaug[:, kc16, :, 128:129], 1.0)))())
    ps_free = [None] * 8
    i = 0
    for h in range(8):
        for kb in range(4):
            bank = i % 4
            i += 1
            for c in range(4):
                tpe = P.op("pe", (lambda h=h, kb=kb, c=c, bank=bank: (lambda e: e.matmul(PS(bank), wukv[:, c, 256 * h:256 * h + 128],
                           kvn[:, c, 512 * kb:512 * kb + 512], start=(c == 0), stop=(c == 3))))(),
                           waits=[t_wukv, ps_free[bank]] if c == 0 else [], sig=(c == 3))
            eng = "act" if i % 2 == 0 else "dve"
            if eng == "act":
                ps_free[bank] = P.op("act", (lambda h=h, kb=kb, bank=bank: (lambda e: e.copy(out=KT[:, h, 512 * kb:512 * kb + 512],
                                     in_=PS(bank))))(), waits=[tpe], sig=True)
            else:
                ps_free[bank] = P.op("dve", (lambda h=h, kb=kb, bank=bank: (lambda e: e.tensor_copy(out=KT[:, h, 512 * kb:512 * kb + 512],
                                     in_=PS(bank))))(), waits=[tpe], sig=True)
    wukv4 = wukv.rearrange("p k (h c) -> p k h c", c=256)
    i = 0
    for kc16 in range(16):
        for hg in range(2):
            bank = 4 + i % 4
            i += 1
            for c in range(4):
                tpe = P.op("pe", (lambda kc16=kc16, hg=hg, c=c, bank=bank: (lambda e: e.matmul(
                           PS(bank).rearrange("p (h c) -> p h c", c=128), kvn[:, c, 128 * kc16:128 * kc16 + 128],
                           wukv4[:, c, 4 * hg:4 * hg + 4, 128:256], start=(c == 0), stop=(c == 3))))(),
                           waits=[t_wukv, ps_free[bank]] if c == 0 else [], sig=(c == 3))
            if i % 2 == 0:
                ps_free[bank] = P.op("act", (lambda kc16=kc16, hg=hg, bank=bank: (lambda e: e.copy(out=Vaug[:, kc16, 4 * hg:4 * hg + 4, 0:128],
                                     in_=PS(bank).rearrange("p (h c) -> p h c", c=128))))(), waits=[tpe], sig=True)
            else:
                ps_free[bank] = P.op("dve", (lambda kc16=kc16, hg=hg, bank=bank: (lambda e: e.tensor_copy(out=Vaug[:, kc16, 4 * hg:4 * hg + 4, 0:128],
                                     in_=PS(bank).rearrange("p (h c) -> p h c", c=128))))(), waits=[tpe], sig=True)
    P.barrier()
    dump("KT", KT, [128, 8, S], BF16)
    dump("Vaug", Vaug, [128, 16, 8, 129], BF16)
    if stop <= 4:
        return finish()

    PT = [V("PT0", BF16, [16, 512]), V("PT1", BF16, [16, 512])]
    rcp = V("rcp", F32, [16]); Otok = [V("Otok0", BF16, [128]), V("Otok1", BF16, [128])]
    Xd_v = Xd.rearrange("(e p) d -> e p d", p=128)
    for e_ in range(NE):
        P.dma("sync", (lambda e_=e_: (lambda e: e.dma_start(out=Xd_v[e_], in_=zero_b)))(), "xdz")
    steps = [(h, qb) for h in range(8) for qb in range(2)]
    st_bank_free = [None] * 4
    exp_done = {}
    pv_done = {}
    o_bank_free = [None, None]
    otok_free = [None, None]
    tile_i = 0

    def emit_ST(s):
        nonlocal tile_i
        h, qb = steps[s]
        qs = slice(512 * qb, 512 * qb + 512)
        for kc in range(16):
            bank = tile_i % 4
            tile_i += 1
            P.op("pe", (lambda h=h, kc=kc, qs=qs, bank=bank: (lambda e: e.matmul(PS(bank), KT[:, h, 128 * kc:128 * kc + 128], QN[:, h, qs],
                 start=True, stop=False)))(), waits=[st_bank_free[bank]])
            tpe = P.op("pe", (lambda h=h, kc=kc, qs=qs, bank=bank: (lambda e: e.matmul(PS(bank), kr[:, 128 * kc:128 * kc + 128], QR[:, h, qs],
                       start=False, stop=True)))(), sig=True)
            w = [tpe]
            if kc == 0 and s >= 2:
                w.append(pv_done[s - 2])
            tex = P.op("act", (lambda s=s, kc=kc, bank=bank: (lambda e: e.activation(out=PT[s % 2][:, kc, :], in_=PS(bank), func=AF.Exp,
                       scale=SCALE)))(), waits=w, sig=True)
            st_bank_free[bank] = tex
        exp_done[s] = tex

    pending_tr = []

    def emit_PV(s):
        h, qb = steps[s]
        for qc in range(4):
            ob = qc % 2
            for kc in range(16):
                tpe = P.op("pe", (lambda s=s, h=h, kc=kc, qc=qc, ob=ob: (lambda e: e.matmul(PS(4 + ob)[:, 0:129],
                           PT[s % 2][:, kc, 128 * qc:128 * qc + 128], Vaug[:, kc, h, :], start=(kc == 0), stop=(kc == 15))))(),
                           waits=[exp_done[s], o_bank_free[ob]] if kc == 0 else [], sig=(kc == 15))
            P.op("dve", (lambda ob=ob, qc=qc: (lambda e: e.reciprocal(out=rcp[:, qc:qc + 1], in_=PS(4 + ob)[:, 128:129])))(), waits=[tpe])
            tn = P.op("dve", (lambda ob=ob, qc=qc: (lambda e: e.tensor_scalar(out=Otok[ob], in0=PS(4 + ob)[:, 0:128],
                      scalar1=rcp[:, qc:qc + 1], scalar2=None, op0=ALU.mult)))(), waits=[otok_free[ob]], sig=True)
            o_bank_free[ob] = tn
            pending_tr.append((s, qc, ob, tn))
            if len(pending_tr) >= 2:
                emit_TR(pending_tr.pop(0))
        pv_done[s] = tpe

    tr_i = 0

    tr_bank_free = [None, None]

    def emit_TR(item):
        s, qc, ob, tn = item
        h, qb = steps[s]
        bank = 6 + s % 2
        tt = P.op("pe", (lambda ob=ob, bank=bank, qc=qc: (lambda e: e.transpose(out=PS(bank, BF16)[:, 128 * qc:128 * qc + 128], in_=Otok[ob],
                  identity=ident_b)))(), waits=[tn] + ([tr_bank_free[s % 2]] if qc == 0 else []), sig=True)
        otok_free[ob] = tt
        if qc == 3:
            tr_bank_free[s % 2] = P.op("dve", (lambda h=h, qb=qb, bank=bank: (lambda e: e.tensor_copy(out=AT[:, h, 512 * qb:512 * qb + 512],
                                       in_=PS(bank, BF16)[:, 0:512])))(), waits=[tt], sig=True)

    KD5 = ""
    if "t" in KD5:
        emit_TR = lambda item: None
    if "z" in KD5:
        steps = steps[:2]
    emit_ST(0)
    for s in range(len(steps)):
        if s + 1 < len(steps):
            emit_ST(s + 1)
        if "p" not in KD5:
            emit_PV(s)
    while pending_tr:
        emit_TR(pending_tr.pop(0))
    if debug:
        d_at = dbg("AT", [128, 16, TOWN], BF16)
        P.barrier()
        P.dma("sync", lambda e: e.dma_start(out=d_at, in_=AT), "dbg")
    P.barrier()
    if stop <= 5:
        return finish()

    wob = [V("wo0", BF16, [16, 512]), V("wo1", BF16, [16, 512])]
    hT = V("h", F32, [8, D])
    w_o_v = w_o.rearrange("(kc p) c -> p kc c", p=128)
    t_x = P.dma("sync", lambda e: e.dma_start(out=hT, in_=xown.rearrange("(t p) d -> p t d", p=128)), "xown")
    wo_free = [None, None]
    ps_free = [None] * 8
    i = 0
    for g in range(4):
        two = P.dma("pool", (lambda g=g: (lambda e: e.dma_start(out=wob[g % 2], in_=w_o_v[:, :, 512 * g:512 * g + 512])))(),
                    f"wo{g % 2}", waits=[wo_free[g % 2]])
        for tt in range(8):
            bank = i % 4
            i += 1
            for f in range(16):
                tpe = P.op("pe", (lambda g=g, tt=tt, f=f, bank=bank: (lambda e: e.matmul(PS(bank), AT[:, f, 128 * tt:128 * tt + 128],
                           wob[g % 2][:, f, :], start=(f == 0), stop=(f == 15))))(), waits=[two, ps_free[bank]] if f == 0 else [], sig=(f == 15))
            ps_free[bank] = P.op("dve", (lambda g=g, tt=tt, bank=bank: (lambda e: e.tensor_tensor(out=hT[:, tt, 512 * g:512 * g + 512],
                                 in0=hT[:, tt, 512 * g:512 * g + 512], in1=PS(bank), op=ALU.add)))(), waits=[tpe, t_x], sig=True)
        wo_free[g % 2] = tpe
    P.barrier()
    if stop <= 6:
        return finish()
    if debug:
        d_h = dbg("h", [TOWN, D], F32)
        P.dma("sync", lambda e: e.dma_start(out=d_h.rearrange("(t p) d -> p t d", p=128), in_=hT), "dbg")

    g2b = V("g2b", F32, [D]); hnf = V("hnf", F32, [D]); hnb = V("hnb", BF16, [8, D]); hnT = V("hnT", F32, [16, 128])
    wr = V("wr", F32, [16, 72]); ssq7 = V("ssq7", F32, [16]); L = V("L", F32, [8, 72])
    sm = {nm: V(nm, F32, [8]) for nm in ("gmax", "gsum", "pg", "m1", "m2", "w1", "w2", "r1", "r2", "v1", "v2", "d1", "d2")}
    gd = V("gd", F32, [8, 8]); gmask = V("gmask", F32, [8, 8])
    sel = V("sel", F32, [8, 8]); sel2 = V("sel2", F32, [8, 8]); mask1 = V("mask1", F32, [8, 8]); mask2 = V("mask2", F32, [8, 8])
    rl4 = V("rl4", F32, [8, 8, 8]); A1 = V("A1", F32, [8, 8, 8]); A2 = V("A2", F32, [8, 8, 8])
    rank = V("rank", F32, [8, 64]); tmp64 = V("tmp64", F32, [8, 64]); Abf = V("Abf", BF16, [8, 64])
    dest_i = V("dest_i", I32, [16]); gates = V("gates", F32, [16])
    t_g2 = P.dma("sync", lambda e: e.dma_start(out=g2b, in_=g2b_d), "g2b")
    t_wr = P.dma("sync", lambda e: e.dma_start(out=wr, in_=w_r.rearrange("(kc p) c -> p kc c", p=128)), "wr")
    t_hs = P.dma("sync", lambda e: e.dma_start(out=hbuf.rearrange("(t p) d -> p t d", p=128), in_=hT), "hbuf")
    ps_free = [None] * 8
    hnfb = [hnf, V("hnf1", F32, [D])]
    hnTb = [hnT, V("hnT1", F32, [16, 128])]
    hnf_free = [None, None]
    hnT_free = [None, None]
    hn_tok = {}
    P.op("dve", lambda e: e.memset(ssq7, 0.0))
    t_ms = P.op("dve", lambda e: e.memset(hnb[:, 0, 0:2], 0.0), sig=True)

    def stage_S(tt):
        hf = hnfb[tt % 2]
        t_sq = P.op("act", (lambda tt=tt, hf=hf: (lambda e: e.activation(out=hf, in_=hT[:, tt, :], func=AF.Square, accum_out=ssq7[:, tt:tt + 1])))(),
                    waits=[hnf_free[tt % 2], t_ms], sig=True)
        _t, t_r = emit_rstd(ssq7[:, 8 + tt:9 + tt], ssq7[:, tt:tt + 1], 1.0 / D, waits=[t_sq])
        t_hn = P.op("dve", (lambda tt=tt, hf=hf: (lambda e: e.scalar_tensor_tensor(out=hf, in0=hT[:, tt, :], scalar=ssq7[:, 8 + tt:9 + tt], in1=g2b,
                    op0=ALU.mult, op1=ALU.mult)))(), waits=[t_g2], sig=True)
        t_hb_ = P.op("act", (lambda tt=tt, hf=hf: (lambda e: e.copy(out=hnb[:, tt, :], in_=hf)))(), waits=[t_hn], sig=True)
        hn_tok[tt] = (t_hn, t_hb_)

    def stage_T(tt):
        hf = hnfb[tt % 2]
        hTt = hnTb[tt % 2]
        t_hn = hn_tok[tt][0]
        for q4 in range(4):
            bank = q4
            for r in range(4):
                kc = 4 * q4 + r
                tpe = P.op("pe", (lambda kc=kc, bank=bank, r=r, hf=hf: (lambda e: e.transpose(out=PS(bank)[:, 128 * r:128 * r + 128],
                           in_=hf[:, 128 * kc:128 * kc + 128], identity=ident_f)))(), waits=[t_hn, ps_free[bank]] if r == 0 else [], sig=(r == 3))
            if q4 % 2 == 0:
                ps_free[bank] = P.op("dve", (lambda q4=q4, bank=bank, hTt=hTt: (lambda e: e.tensor_copy(out=hTt[:, 4 * q4:4 * q4 + 4, :],
                                     in_=PS(bank).rearrange("p (a b) -> p a b", b=128))))(), waits=[tpe, hnT_free[tt % 2]], sig=True)
            else:
                ps_free[bank] = P.op("act", (lambda q4=q4, bank=bank, hTt=hTt: (lambda e: e.copy(out=hTt[:, 4 * q4:4 * q4 + 4, :],
                                     in_=PS(bank).rearrange("p (a b) -> p a b", b=128))))(), waits=[tpe, hnT_free[tt % 2]], sig=True)
        hnf_free[tt % 2] = tpe
        for kc in range(16):
            tpl = P.op("pe", (lambda kc=kc, hTt=hTt: (lambda e: e.matmul(PS(4)[:, 0:72], hTt[:, kc, :], wr[:, kc, :], start=(kc == 0), stop=(kc == 15))))(),
                       waits=[ps_free[0], ps_free[1], ps_free[2], ps_free[3], t_wr, ps_free[4]] if kc == 0 else [], sig=(kc == 15))
        hnT_free[tt % 2] = tpl
        ps_free[4] = P.op("dve", (lambda tt=tt: (lambda e: e.tensor_tensor(out=L[:, tt, :], in0=PS(4)[:, 0:72], in1=brb, op=ALU.add)))(),
                          waits=[tpl], sig=True)

    stage_S(0)
    for tt in range(8):
        if tt + 1 < 8:
            stage_S(tt + 1)
        stage_T(tt)
    t_hb = hn_tok[7][1]
    if debug:
        d_L = dbg("L", [128, 8, 72], F32)
        P.dma("sync", lambda e: e.dma_start(out=d_L, in_=L), "dbg", waits=[ps_free[4]])
    gl = L[:, :, 0:8]
    rlv = L[:, :, 8:72].rearrange("p t (g j) -> p t g j", j=8)

    def bc(ap, shape):
        return ap.to_broadcast(shape)

    dv = lambda fn, **kw: P.op("dve", fn, **kw)
    dv(lambda e: e.tensor_reduce(out=sm["gmax"], in_=gl, axis=AX.X, op=ALU.max))
    dv(lambda e: e.tensor_tensor(out=gd, in0=gl, in1=bc(sm["gmax"].unsqueeze(2), [128, 8, 8]), op=ALU.subtract))
    t_gd = dv(lambda e: e.tensor_tensor(out=gmask, in0=gl, in1=bc(sm["gmax"].unsqueeze(2), [128, 8, 8]), op=ALU.is_equal), sig=True)
    t_ge = P.op("act", lambda e: e.activation(out=gd, in_=gd, func=AF.Exp), waits=[t_gd], sig=True)
    dv(lambda e: e.tensor_reduce(out=sm["gsum"], in_=gd, axis=AX.X, op=ALU.add), waits=[t_ge])
    dv(lambda e: e.reciprocal(out=sm["pg"], in_=sm["gsum"]))
    dv(lambda e: e.tensor_tensor(out=rl4, in0=rlv, in1=bc(gmask.unsqueeze(3), [128, 8, 8, 8]), op=ALU.mult))
    dv(lambda e: e.tensor_reduce(out=sel, in_=rl4.rearrange("p t g j -> p t j g"), axis=AX.X, op=ALU.add))
    dv(lambda e: e.tensor_reduce(out=sm["m1"], in_=sel, axis=AX.X, op=ALU.max))
    dv(lambda e: e.tensor_tensor(out=mask1, in0=sel, in1=bc(sm["m1"].unsqueeze(2), [128, 8, 8]), op=ALU.is_equal))
    dv(lambda e: e.scalar_tensor_tensor(out=sel2, in0=mask1, scalar=-1e30, in1=sel, op0=ALU.mult, op1=ALU.add))
    dv(lambda e: e.tensor_reduce(out=sm["m2"], in_=sel2, axis=AX.X, op=ALU.max))
    dv(lambda e: e.tensor_tensor(out=mask2, in0=sel2, in1=bc(sm["m2"].unsqueeze(2), [128, 8, 8]), op=ALU.is_equal))
    t_w = dv(lambda e: e.tensor_tensor(out=sm["w1"], in0=sm["m2"], in1=sm["m1"], op=ALU.subtract), sig=True)
    t_we = P.op("act", lambda e: e.activation(out=sm["w1"], in_=sm["w1"], func=AF.Exp), waits=[t_w], sig=True)
    dv(lambda e: e.tensor_scalar(out=sm["w1"], in0=sm["w1"], scalar1=1.0, scalar2=None, op0=ALU.add), waits=[t_we])
    dv(lambda e: e.reciprocal(out=sm["w1"], in_=sm["w1"]))
    dv(lambda e: e.tensor_scalar(out=sm["w2"], in0=sm["w1"], scalar1=-1.0, scalar2=1.0, op0=ALU.mult, op1=ALU.add))
    dv(lambda e: e.tensor_tensor(out=A1, in0=bc(gmask.unsqueeze(3), [128, 8, 8, 8]), in1=bc(mask1.unsqueeze(2), [128, 8, 8, 8]), op=ALU.mult))
    dv(lambda e: e.tensor_tensor(out=A2, in0=bc(gmask.unsqueeze(3), [128, 8, 8, 8]), in1=bc(mask2.unsqueeze(2), [128, 8, 8, 8]), op=ALU.mult))
    A1f = A1.rearrange("p t g j -> p t (g j)"); A2f = A2.rearrange("p t g j -> p t (g j)")
    dv(lambda e: e.tensor_tensor(out=tmp64, in0=A1f, in1=A2f, op=ALU.add))
    t_A = dv(lambda e: e.tensor_copy(out=Abf, in_=tmp64), sig=True)
    for tt in range(8):
        bank = 5 + tt % 2
        tpe = P.op("pe", (lambda tt=tt, bank=bank: (lambda e: e.matmul(PS(bank)[:, 0:64], triu_b, Abf[:, tt, :], start=True, stop=(tt == 0))))(),
                   waits=[t_A, ps_free[bank]], sig=(tt == 0))
        for t2_ in range(tt):
            tpe = P.op("pe", (lambda t2_=t2_, tt=tt, bank=bank: (lambda e: e.matmul(PS(bank)[:, 0:64], ones_b, Abf[:, t2_, :], start=False,
                       stop=(t2_ == tt - 1))))(), sig=(t2_ == tt - 1))
        ps_free[bank] = dv((lambda tt=tt, bank=bank: (lambda e: e.tensor_copy(out=rank[:, tt, :], in_=PS(bank)[:, 0:64])))(), waits=[tpe], sig=True)
    for k, (Af, rk, vk, dk, wk) in enumerate(((A1f, "r1", "v1", "d1", "w1"), (A2f, "r2", "v2", "d2", "w2"))):
        dv((lambda Af=Af: (lambda e: e.tensor_tensor(out=tmp64, in0=Af, in1=rank, op=ALU.mult)))())
        dv((lambda rk=rk: (lambda e: e.tensor_reduce(out=sm[rk], in_=tmp64, axis=AX.X, op=ALU.add)))())
        dv((lambda Af=Af: (lambda e: e.tensor_tensor(out=tmp64, in0=Af, in1=bc(e128.unsqueeze(1), [128, 8, 64]), op=ALU.mult)))())
        dv((lambda dk=dk: (lambda e: e.tensor_reduce(out=sm[dk], in_=tmp64, axis=AX.X, op=ALU.add)))())
        dv((lambda dk=dk, rk=rk: (lambda e: e.tensor_tensor(out=sm[dk], in0=sm[dk], in1=sm[rk], op=ALU.add)))())
        dv((lambda vk=vk, rk=rk: (lambda e: e.tensor_scalar(out=sm[vk], in0=sm[rk], scalar1=float(CAP) - 0.5, scalar2=None, op0=ALU.is_lt)))())
        dv((lambda dk=dk, vk=vk: (lambda e: e.tensor_tensor(out=sm[dk], in0=sm[dk], in1=sm[vk], op=ALU.mult)))())
        dv((lambda rk=rk, vk=vk: (lambda e: e.tensor_scalar(out=sm[rk], in0=sm[vk], scalar1=-1.0e6, scalar2=1.0e6, op0=ALU.mult, op1=ALU.add)))())
        dv((lambda dk=dk, rk=rk: (lambda e: e.tensor_tensor(out=sm[dk], in0=sm[dk], in1=sm[rk], op=ALU.add)))())
        dv((lambda dk=dk, k=k: (lambda e: e.tensor_copy(out=dest_i[:, 8 * k:8 * k + 8], in_=sm[dk])))())
        dv((lambda wk=wk, k=k: (lambda e: e.tensor_tensor(out=gates[:, 8 * k:8 * k + 8], in0=sm[wk], in1=sm["pg"], op=ALU.mult)))())
        t_disp = dv((lambda vk=vk, k=k: (lambda e: e.tensor_tensor(out=gates[:, 8 * k:8 * k + 8], in0=gates[:, 8 * k:8 * k + 8], in1=sm[vk],
                    op=ALU.mult)))(), sig=True)
    if debug:
        d_dest = dbg("dest", [128, 16], I32); d_gates = dbg("gates", [128, 16], F32)
        P.dma("sync", lambda e: e.dma_start(out=d_dest, in_=dest_i), "dbg", waits=[t_disp])
        P.dma("sync", lambda e: e.dma_start(out=d_gates, in_=gates), "dbg", waits=[t_disp])
    t_z = P.dma_last["xdz"]
    for k in range(2):
        for tt in range(8):
            t_sc = P.dma("pool", (lambda k=k, tt=tt: (lambda e: e.indirect_dma_start(out=Xd, out_offset=bass.IndirectOffsetOnAxis(
                         ap=dest_i[:, 8 * k + tt:8 * k + tt + 1], axis=0), in_=hnb[:, tt, :], in_offset=None,
                         bounds_check=NE * CAP - 1, oob_is_err=False)))(), "scat", waits=[t_disp, t_z, t_hb])
    P.barrier()
    if stop <= 7:
        return finish()

    wg = [V("wg0", BF16, [16, 512]), V("wg1", BF16, [16, 512])]
    wu = [V("wu_0", BF16, [16, 512]), V("wu_1", BF16, [16, 512])]
    wd = [V("wd0", BF16, [4, D]), V("wd1", BF16, [4, D])]
    Xe = [V("Xe0", BF16, [D]), V("Xe1", BF16, [D])]
    XT = [V("XT0", BF16, [16, 128]), V("XT1", BF16, [16, 128])]
    sgt = [V("sgt0", F32, [512]), V("sgt1", F32, [512])]
    hb = [V("hb0", BF16, [512]), V("hb1", BF16, [512])]
    HT = [V("HT0", BF16, [4, 128]), V("HT1", BF16, [4, 128])]
    Ysb = [V("Ysb0", F32, [D]), V("Ysb1", F32, [D])]
    wdf = [V("wdf0", F32, [2, D]), V("wdf1", F32, [2, D])]
    wgv = w_gate.rearrange("e (kc p) c -> e p kc c", p=128)
    wuv = w_up.rearrange("e (kc p) c -> e p kc c", p=128)
    wdv = w_down.rearrange("e (kc p) c -> e p kc c", p=128)
    Ybuf_v = Ybuf.rearrange("(e p) d -> e p d", p=128)
    gu_done = [None, None]; dn_done = [None, None]; xtr_done = [None, None]; xt_used = [None, None]
    sgt_free = [None, None]; hb_free = [None, None]; ht_free = [None, None]; ysb_free = [None, None]
    bfree = [None] * 8
    ld = {}
    xt_rdy = {}
    mult_done = {}
    ht_rdy = {}

    xe_tok = {}

    def issue_xe(ex):
        b = ex % 2
        xe_tok[ex] = P.dma("sync", (lambda ex=ex, b=b: (lambda e: e.dma_start(out=Xe[b], in_=Xd_v[ex])))(), f"xe{b}", waits=[xtr_done[b]])

    def issue_loads(ex):
        b = ex % 2
        t_xe = xe_tok[ex]
        t_wg = P.dma("pool", (lambda ex=ex, b=b: (lambda e: e.dma_start(out=wg[b], in_=wgv[ex])))(), f"wg{b}", waits=[gu_done[b]])
        t_wu = P.dma("pool", (lambda ex=ex, b=b: (lambda e: e.dma_start(out=wu[b], in_=wuv[ex])))(), f"wu{b}", waits=[gu_done[b]])
        wdf_tok[ex] = [P.dma("sync", (lambda ex=ex, h=h: (lambda e: e.dma_start(out=wdf[h], in_=wdv[ex][:, 2 * h:2 * h + 2, :])))(), f"wdf{h}",
                             waits=[wdf_free[h]]) for h in range(2)]
        ld[ex] = (t_xe, t_wg, t_wu, None)

    wdf_tok = {}
    wdf_free = [None, None]
    wd_rdy = {}

    def cast_wd(ex):
        b = ex % 2
        t0 = P.op("act", (lambda b=b: (lambda e: e.copy(out=wd[b][:, 0:2, :], in_=wdf[0])))(), waits=[wdf_tok[ex][0], dn_done[b]], sig=True)
        t1 = P.op("dve", (lambda b=b: (lambda e: e.tensor_copy(out=wd[b][:, 2:4, :], in_=wdf[1])))(), waits=[wdf_tok[ex][1], dn_done[b]], sig=True)
        wdf_free[0], wdf_free[1] = t0, t1
        wd_rdy[ex] = [t0, t1]

    def stage_A1(ex):
        b = ex % 2
        t_xe = ld[ex][0]
        for half in range(2):
            for r in range(8):
                kc = 8 * half + r
                tpe = P.op("pe", (lambda b=b, kc=kc, half=half, r=r: (lambda e: e.transpose(out=PS(half, BF16)[:, 128 * r:128 * r + 128],
                           in_=Xe[b][:, 128 * kc:128 * kc + 128], identity=ident_b)))(), waits=[t_xe, bfree[half]] if r == 0 else [], sig=(r == 7))
            if half == 0:
                bfree[0] = P.op("act", (lambda b=b: (lambda e: e.copy(out=XT[b][:, 0:8, :], in_=PS(0, BF16).rearrange("p (a c) -> p a c", c=128))))(),
                                waits=[tpe, xt_used[b]], sig=True)
            else:
                bfree[1] = P.op("dve", (lambda b=b: (lambda e: e.tensor_copy(out=XT[b][:, 8:16, :], in_=PS(1, BF16).rearrange("p (a c) -> p a c", c=128))))(),
                                waits=[tpe, xt_used[b]], sig=True)
        xtr_done[b] = tpe
        xt_rdy[ex] = [bfree[0], bfree[1]]

    def stage_A2(ex):
        b = ex % 2
        _, t_wg, t_wu, _ = ld[ex]
        for which, (wt, tw, bank) in enumerate(((wg[b], t_wg, 2), (wu[b], t_wu, 3))):
            for kc in range(16):
                tpe = P.op("pe", (lambda b=b, wt=wt, kc=kc, bank=bank: (lambda e: e.matmul(PS(bank), XT[b][:, kc, :], wt[:, kc, :],
                           start=(kc == 0), stop=(kc == 15))))(), waits=xt_rdy[ex] + [tw, bfree[bank]] if kc == 0 else [], sig=(kc == 15))
            if which == 0:
                t_hg = tpe
        gu_done[b] = tpe
        xt_used[b] = tpe
        bfree[2] = P.op("act", (lambda b=b: (lambda e: e.activation(out=sgt[b], in_=PS(2), func=AF.Silu)))(), waits=[t_hg, sgt_free[b]], sig=True)
        bfree[3] = P.op("dve", (lambda b=b: (lambda e: e.tensor_tensor(out=hb[b], in0=sgt[b], in1=PS(3), op=ALU.mult)))(),
                        waits=[bfree[2], tpe, hb_free[b]], sig=True)
        sgt_free[b] = bfree[3]
        mult_done[ex] = bfree[3]

    def stage_B1(ex):
        b = ex % 2
        for fc in range(4):
            tpe = P.op("pe", (lambda b=b, fc=fc: (lambda e: e.transpose(out=PS(4, BF16)[:, 128 * fc:128 * fc + 128],
                       in_=hb[b][:, 128 * fc:128 * fc + 128], identity=ident_b)))(), waits=[mult_done[ex], bfree[4]] if fc == 0 else [], sig=(fc == 3))
        hb_free[b] = tpe
        bfree[4] = P.op("act", (lambda b=b: (lambda e: e.copy(out=HT[b], in_=PS(4, BF16)[:, 0:512].rearrange("p (a c) -> p a c", c=128))))(),
                        waits=[tpe, ht_free[b]], sig=True)
        ht_rdy[ex] = bfree[4]

    def stage_B2(ex):
        b = ex % 2
        ev = []
        for cb, bank in enumerate((5, 6, 7, 5)):
            for fc in range(4):
                tpe = P.op("pe", (lambda b=b, fc=fc, cb=cb, bank=bank: (lambda e: e.matmul(PS(bank), HT[b][:, fc, :],
                           wd[b][:, fc, 512 * cb:512 * cb + 512], start=(fc == 0), stop=(fc == 3))))(),
                           waits=([ht_rdy[ex], bfree[bank]] + wd_rdy[ex]) if fc == 0 else [], sig=(fc == 3))
            if cb % 2 == 0:
                bfree[bank] = P.op("dve", (lambda b=b, cb=cb, bank=bank: (lambda e: e.tensor_copy(out=Ysb[b][:, 512 * cb:512 * cb + 512],
                                   in_=PS(bank))))(), waits=[tpe, ysb_free[b]], sig=True)
            else:
                bfree[bank] = P.op("act", (lambda b=b, cb=cb, bank=bank: (lambda e: e.copy(out=Ysb[b][:, 512 * cb:512 * cb + 512],
                                   in_=PS(bank))))(), waits=[tpe, ysb_free[b]], sig=True)
            ev.append(bfree[bank])
        dn_done[b] = tpe
        ht_free[b] = tpe
        ysb_free[b] = P.dma("sync", (lambda ex=ex, b=b: (lambda e: e.dma_start(out=Ybuf_v[ex], in_=Ysb[b])))(), f"yout{b}", waits=ev)

    issue_xe(0)
    issue_xe(1)
    issue_loads(0)
    cast_wd(0)
    issue_loads(1)
    stage_A1(0)
    stage_A2(0)
    for ex in range(NE):
        if ex + 2 < NE:
            issue_xe(ex + 2)
        if ex + 1 < NE:
            stage_A1(ex + 1)
        stage_B1(ex)
        if ex + 1 < NE:
            stage_A2(ex + 1)
        stage_B2(ex)
        if ex + 1 < NE:
            cast_wd(ex + 1)
        if ex + 2 < NE:
            issue_loads(ex + 2)
    P.barrier()
    if stop <= 8:
        return finish()

    fgb = V("fgb", F32, [D])
    Y1 = [V("Y1_0", F32, [D]), V("Y1_1", F32, [D])]; Y2 = [V("Y2_0", F32, [D]), V("Y2_1", F32, [D])]
    hz = [V("hz0", F32, [D]), V("hz1", F32, [D])]; ss9 = [V("ss9_0", F32, [4]), V("ss9_1", F32, [4])]
    t_fg = P.dma("sync", lambda e: e.dma_start(out=fgb, in_=fgb_d), "fgb")
    buf_free = [None, None]
    out_v = out.rearrange("(t p) d -> t p d", p=128)
    hbuf_v = hbuf.rearrange("(t p) d -> t p d", p=128)
    t_out = None
    for tt in range(8):
        b = tt % 2
        t_h = P.dma("sync", (lambda tt=tt, b=b: (lambda e: e.dma_start(out=hz[b], in_=hbuf_v[tt])))(), f"hz{b}", waits=[buf_free[b], t_hs])
        t_m1 = P.op("pool", (lambda b=b: (lambda e: e.memset(Y1[b], 0.0)))(), waits=[buf_free[b]])
        t_m2 = P.op("pool", (lambda b=b: (lambda e: e.memset(Y2[b], 0.0)))(), sig=True)
        t_g1 = P.dma("pool", (lambda tt=tt, b=b: (lambda e: e.indirect_dma_start(out=Y1[b], out_offset=None, in_=Ybuf,
                     in_offset=bass.IndirectOffsetOnAxis(ap=dest_i[:, tt:tt + 1], axis=0), bounds_check=NE * CAP - 1, oob_is_err=False)))(),
                     f"ga{b}", waits=[t_m2])
        t_g2_ = P.dma("pool", (lambda tt=tt, b=b: (lambda e: e.indirect_dma_start(out=Y2[b], out_offset=None, in_=Ybuf,
                      in_offset=bass.IndirectOffsetOnAxis(ap=dest_i[:, 8 + tt:9 + tt], axis=0), bounds_check=NE * CAP - 1, oob_is_err=False)))(),
                      f"gb{b}", waits=[t_m2])
        P.op("dve", (lambda b=b: (lambda e: e.memset(ss9[b], 0.0)))(), waits=[buf_free[b]])
        P.op("dve", (lambda tt=tt, b=b: (lambda e: e.scalar_tensor_tensor(out=hz[b], in0=Y1[b], scalar=gates[:, tt:tt + 1], in1=hz[b],
             op0=ALU.mult, op1=ALU.add)))(), waits=[t_h, t_g1])
        t_z9 = P.op("dve", (lambda tt=tt, b=b: (lambda e: e.scalar_tensor_tensor(out=hz[b], in0=Y2[b], scalar=gates[:, 8 + tt:9 + tt], in1=hz[b],
                    op0=ALU.mult, op1=ALU.add)))(), waits=[t_g2_], sig=True)
        t_s9 = P.op("act", (lambda b=b: (lambda e: e.activation(out=Y1[b], in_=hz[b], func=AF.Square, accum_out=ss9[b][:, 0:1])))(),
                    waits=[t_z9], sig=True)
        _t, t_r9 = emit_rstd(ss9[b][:, 3:4], ss9[b][:, 0:1], 1.0 / D, waits=[t_s9])
        t_o = P.op("dve", (lambda b=b: (lambda e: e.scalar_tensor_tensor(out=hz[b], in0=hz[b], scalar=ss9[b][:, 3:4], in1=fgb,
                   op0=ALU.mult, op1=ALU.mult)))(), waits=[t_fg], sig=True)
        t_out = P.dma("sync", (lambda tt=tt, b=b: (lambda e: e.dma_start(out=out_v[tt], in_=hz[b])))(), f"out{b}", waits=[t_o])
        buf_free[b] = t_out
    P.wait_only("sync", [P.dma_last["out0"], P.dma_last["out1"]] + ([P.dma_last["dbg"]] if debug else []))
    P.emit(stack, ps_probe)
    stack.close()
    return nc, list(dbg_out.keys())


_CACHE = {}


def _host_inputs(inputs):
    f32 = np.float32
    x = np.asarray(inputs["x"], f32)
    B = x.shape[0]

    def vecT(v, n):
        return np.ascontiguousarray(np.asarray(v, f32).reshape(n, 128).T)

    com = {
        "ident": np.eye(128, dtype=f32),
        "triu": np.triu(np.ones((128, 128), f32), 1),
        "e128": np.broadcast_to((np.arange(NE, dtype=f32) * CAP)[None, :], (128, NE)).copy(),
        "g1t": vecT(inputs["ln1_g"][0], 16),
        "bglut": vecT(inputs["b_glu"][0], 16),
        "qgt": vecT(inputs["q_norm_g"][0], 6),
        "kvgt": vecT(inputs["kv_norm_g"][0], 4),
        "wdwt": np.ascontiguousarray(np.asarray(inputs["w_dw"][0], f32).T.reshape(8, 128, 31).transpose(1, 0, 2)),
        "bdwt": vecT(inputs["b_dw"][0], 8),
        "lngt": vecT(inputs["conv_ln_g"][0], 8),
        "lnbt": vecT(inputs["conv_ln_b"][0], 8),
        "g2b": np.broadcast_to(np.asarray(inputs["ln2_g"][0], f32)[None, :], (128, D)).copy(),
        "fgb": np.broadcast_to(np.asarray(inputs["final_g"], f32)[None, :], (128, D)).copy(),
        "brb": np.broadcast_to(np.concatenate([np.asarray(inputs["b_group"][0], f32), np.asarray(inputs["b_router"][0], f32)])[None, :],
                               (128, 72)).copy(),
        "w_in": np.asarray(inputs["w_in"][0], f32),
        "w_uq": np.asarray(inputs["w_uq"][0], f32),
        "w_ukv": np.asarray(inputs["w_ukv"][0], f32),
        "w_o": np.asarray(inputs["w_o"][0], f32),
        "w_r": np.ascontiguousarray(np.concatenate([np.asarray(inputs["w_group"][0], f32), np.asarray(inputs["w_router"][0], f32)], axis=1)),
        "w_gate": np.asarray(inputs["w_gate"][0], f32),
        "w_up": np.asarray(inputs["w_up"][0], f32),
        "w_down": np.asarray(inputs["w_down"][0], f32),
    }
    pos = np.arange(S, dtype=f32)
    inv_freq = (f32(10000.0) ** (-np.arange(0, 64, 2, dtype=f32) / f32(64))).astype(f32)
    ang = (pos[:, None] * inv_freq[None, :]).astype(f32)
    cos = np.cos(ang).astype(f32).T
    sin = np.sin(ang).astype(f32).T
    cos64 = np.concatenate([cos, cos], 0)
    sin64 = np.concatenate([-sin, sin], 0)
    in_maps = []
    for c in range(NCORES):
        b, hf = c // 2, c % 2
        own = np.arange(hf * TOWN, (hf + 1) * TOWN)
        oth = np.arange((1 - hf) * TOWN, (2 - hf) * TOWN)
        order = np.concatenate([own, oth])
        xb = x[b]
        xt = np.zeros((D, 2 * TOWN + 32), f32)
        xt[:, 0:2 * TOWN] = xb[order].T
        hm = np.zeros((32,), f32)
        lh = np.arange(own[0] - 16, own[0]); rh = np.arange(own[-1] + 1, own[-1] + 17)
        for i, p in enumerate(np.concatenate([lh, rh])):
            if 0 <= p < S:
                xt[:, 2 * TOWN + i] = xb[p]
                hm[i] = 1.0
        m = dict(com)
        m["xT"] = xt
        m["xown"] = np.ascontiguousarray(xb[own])
        m["cosk"] = np.ascontiguousarray(cos64[:, order])
        m["sink"] = np.ascontiguousarray(sin64[:, order])
        m["halo_mask"] = np.broadcast_to(hm[None, :], (128, 32)).copy()
        in_maps.append(m)
    return in_maps, B


def kernel(**inputs):
    debug = bool(inputs.pop("_debug", False))
    stop = int(inputs.pop("_stop", 99))
    key = ("prog", debug, stop)
    if key not in _CACHE:
        _CACHE[key] = build_program(debug, stop)
    nc, dbg_names = _CACHE[key]
    in_maps, B = _host_inputs(inputs)
    if stop < 8:
        for m in in_maps:
            for k in ("w_gate", "w_up", "w_down"):
                m[k] = m[k][0:1]
    res = run_bass_kernel_spmd(nc, in_maps, core_ids=list(range(NCORES)))
    outp = np.zeros((B, S, D), np.float32)
    for c in range(NCORES):
        b, hf = c // 2, c % 2
        if "out" in res.results[c]:
            outp[b, hf * TOWN:(hf + 1) * TOWN, :] = res.results[c]["out"]
    if debug:
        return outp, [{n: r["dbg_" + n] for n in dbg_names} for r in res.results]
    return outp
```

```python
import contextlib
import numpy as np
import ml_dtypes
import concourse.bass as bass
import concourse.mybir as mybir
from concourse.bass_utils import run_bass_kernel_spmd

F32 = mybir.dt.float32
BF16 = mybir.dt.bfloat16
I32 = mybir.dt.int32
U8 = mybir.dt.uint8
AF = mybir.ActivationFunctionType
ALU = mybir.AluOpType
AX = mybir.AxisListType

NCORES = 8
D = 2048
S = 2048
TOWN = 1024
NEXT = TOWN + 32
NE = 64
CAP = 128
EPS = 1e-6
SCALE = 192.0 ** -0.5
SBUF_BYTES = 206 * 1024

ENGS = ("sync", "act", "pe", "dve", "pool")


class Tok:
    __slots__ = ("sem", "val", "rec", "eng")

    def __init__(self, sem=None, val=None, rec=None, eng=None):
        self.sem, self.val, self.rec, self.eng = sem, val, rec, eng


class Rec:
    __slots__ = ("eng", "fn", "waits", "sig", "kind", "key", "tok", "seq")
    _seq = 0

    def __init__(self, eng, fn, waits, sig, kind, key=None):
        self.eng, self.fn, self.waits, self.sig, self.kind, self.key = eng, fn, list(waits), sig, kind, key
        self.tok = Tok(rec=self, eng=eng)
        Rec._seq += 1
        self.seq = Rec._seq


def _ap_range(v):
    esz = {F32: 4, BF16: 2, I32: 4, U8: 1}.get(v.dtype, 4)
    dims = v.ap
    pitch = dims[0][0]
    off = v.offset % pitch if pitch > 0 else v.offset
    ext = 1
    for st, sz in dims[1:]:
        ext += abs(st) * (sz - 1)
    return (v.tensor.name, off * esz, (off + ext) * esz)


class _Dummy:
    hit = False
    reads = []
    writes = []

    def __getattr__(self, name):
        def f(*a, **k):
            for key, v in [(None, x) for x in a] + list(k.items()):
                if not hasattr(v, "space") or not hasattr(v, "ap"):
                    continue
                if str(v.space) == "PSUM":
                    _Dummy.hit = True
                r = _ap_range(v)
                if key is None:
                    _Dummy.reads.append(r)
                    _Dummy.writes.append(r)
                elif key in ("out", "accum_out"):
                    _Dummy.writes.append(r)
                else:
                    _Dummy.reads.append(r)
            return _Dummy()
        return f


def _overlap(xs, ys):
    for (t0, a0, b0) in xs:
        for (t1, a1, b1) in ys:
            if t0 == t1 and a0 < b1 and a1 < b0:
                return True
    return False


FENCED = ("act", "dve", "pool")


class Prog:
    def __init__(self, nc):
        self.nc = nc
        self.q = {e: [] for e in ENGS}
        self.dma_last = {}
        self.dma_eng = {}

    def op(self, eng, fn, waits=(), sig=False):
        r = Rec(eng, fn, [w for w in waits if w is not None], sig, "op")
        self.q[eng].append(r)
        return r.tok

    def dma(self, eng, fn, key, waits=()):
        assert self.dma_eng.setdefault(key, eng) == eng, key
        r = Rec(eng, fn, [w for w in waits if w is not None], True, "dma", key)
        self.q[eng].append(r)
        self.dma_last[key] = r.tok
        return r.tok

    def wait_only(self, eng, waits):
        r = Rec(eng, None, [w for w in waits if w is not None], False, "wait")
        self.q[eng].append(r)

    def last_tok(self, eng):
        for r in reversed(self.q[eng]):
            if r.kind == "op":
                r.sig = True
                return r.tok
        return None

    def barrier(self):
        toks = [self.last_tok(e) for e in ("act", "pe", "dve", "pool")]
        toks += list(self.dma_last.values())
        for e in ENGS:
            self.wait_only(e, toks)

    def emit(self, stack, ps_probe=None):
        nc = self.nc
        sems = {}
        if ps_probe is not None:
            recs = sorted([r for e in ("act", "dve") for r in self.q[e] if r.kind == "op"], key=lambda r: r.seq)
            last = {"act": None, "dve": None}
            for r in recs:
                if ps_probe(r.fn):
                    other = "dve" if r.eng == "act" else "act"
                    if last[other] is not None:
                        last[other].rec.sig = True
                        r.waits.append(last[other])
                    last[r.eng] = r.tok
        self.fence = {}
        for e in FENCED:
            hist = []
            for r in self.q[e]:
                if r.kind != "op":
                    continue
                _Dummy.reads, _Dummy.writes = [], []
                r.fn(_Dummy())
                rd, wr = _Dummy.reads, _Dummy.writes
                for pr, pw in hist[-2:]:
                    if _overlap(pw, rd) or _overlap(pw, wr):
                        prev = hist[-1][0]
                        prev.sig = True
                        self.fence[id(r)] = prev
                        break
                hist.append((r, wr))

        def getsem(name):
            if name not in sems:
                sems[name] = stack.enter_context(nc.semaphore(name))
            return sems[name]

        for e in ENGS:
            cnt = 0
            dcnt = {}
            for r in self.q[e]:
                if r.kind == "op" and r.sig:
                    cnt += 1
                    r.tok.sem, r.tok.val = "c_" + e, cnt
                elif r.kind == "dma":
                    dcnt[r.key] = dcnt.get(r.key, 0) + 16
                    r.tok.sem, r.tok.val = "d_" + r.key, dcnt[r.key]
        block = stack.enter_context(nc.Block())
        q = self.q

        def run(eng_name, e):
            waited = {}
            prev_cnt = 0
            for r in q[eng_name]:
                for w in r.waits:
                    if w.rec.kind == "op" and w.eng == eng_name and r.kind == "op":
                        continue
                    assert w.sem is not None, "wait on unsignalled op"
                    if waited.get(w.sem, 0) >= w.val:
                        continue
                    waited[w.sem] = w.val
                    e.wait_ge(getsem(w.sem), w.val)
                if r.fn is None:
                    continue
                if r.kind == "op" and id(r) in self.fence:
                    pv = self.fence[id(r)].tok.val
                    if waited.get("c_" + eng_name, 0) < pv:
                        waited["c_" + eng_name] = pv
                        e.wait_ge(getsem("c_" + eng_name), pv)
                ins = r.fn(e)
                if r.kind == "dma":
                    ins.then_inc(getsem(r.tok.sem), 16)
                elif r.sig:
                    ins.then_inc(getsem(r.tok.sem), 1)
                    prev_cnt = r.tok.val

        @block.sync
        def _(e):
            run("sync", e)

        @block.scalar
        def _(e):
            run("act", e)

        @block.tensor
        def _(e):
            run("pe", e)

        @block.vector
        def _(e):
            run("dve", e)

        @block.gpsimd
        def _(e):
            run("pool", e)


class Arena:
    def __init__(self):
        self.items = []

    def add(self, name, nbytes, p0, p1):
        self.items.append((name, (nbytes + 63) // 64 * 64, p0, p1))

    def solve(self, limit):
        placed = {}
        for name, nb, p0, p1 in sorted(self.items, key=lambda t: -t[1]):
            conflicts = sorted((o, o + b) for (o, b, q0, q1) in placed.values() if not (q1 < p0 or p1 < q0))
            off = 0
            for a, b in conflicts:
                if off + nb <= a:
                    break
                off = max(off, b)
            assert off + nb <= limit, f"SBUF overflow placing {name}: {off + nb} > {limit}"
            placed[name] = (off, nb, p0, p1)
        return {k: v[0] for k, v in placed.items()}


def build_program(debug=False, stop=99):
    nc = bass.Bass("TRN2", target_bir_lowering=False)
    dbg_out = {}

    def din(name, shape, dt=F32):
        return nc.dram_tensor(name, list(shape), dt, kind="ExternalInput").ap()

    xT = din("xT", [D, 2 * TOWN + 32])
    xown = din("xown", [TOWN, D])
    cosk = din("cosk", [64, S])
    sink = din("sink", [64, S])
    halo_mask = din("halo_mask", [128, 32])
    ident_d = din("ident", [128, 128])
    triu_d = din("triu", [128, 128])
    e128_d = din("e128", [128, NE])
    g1_d = din("g1t", [128, 16])
    bglu_d = din("bglut", [128, 16])
    qg_d = din("qgt", [128, 6])
    kvg_d = din("kvgt", [128, 4])
    wdw_d = din("wdwt", [128, 8, 31])
    bdw_d = din("bdwt", [128, 8])
    lng_d = din("lngt", [128, 8])
    lnb_d = din("lnbt", [128, 8])
    g2b_d = din("g2b", [128, D])
    fgb_d = din("fgb", [128, D])
    brb_d = din("brb", [128, 72])
    w_in = din("w_in", [D, 3392])
    w_uq = din("w_uq", [768, 1536])
    w_ukv = din("w_ukv", [512, 2048])
    w_o = din("w_o", [D, D])
    w_r = din("w_r", [D, 72])
    NEW = NE if stop >= 8 else 1
    w_gate = din("w_gate", [NEW, D, 512])
    w_up = din("w_up", [NEW, D, 512])
    w_down = din("w_down", [NEW, 512, D])
    out = nc.dram_tensor("out", [TOWN, D], F32, kind="ExternalOutput").ap()
    Xd = nc.dram_tensor("Xd", [NE * CAP, D], BF16).ap()
    Ybuf = nc.dram_tensor("Ybuf", [NE * CAP, D], F32).ap()
    hbuf = nc.dram_tensor("hbuf", [TOWN, D], F32).ap()

    def dbg(name, shape, dt):
        if debug:
            dbg_out[name] = nc.dram_tensor("dbg_" + name, list(shape), dt, kind="ExternalOutput").ap()
            return dbg_out[name]
        return None

    ar = Arena()
    A = ar.add
    A("ident_f", 512, 0, 9); A("ident_b", 256, 0, 9); A("ones_b", 256, 0, 9); A("ones_f", 512, 0, 9)
    A("triu_b", 256, 0, 9); A("triu_f", 512, 0, 0); A("e128", 256, 0, 9)
    A("g1t", 64, 0, 9); A("bglut", 64, 0, 9); A("qgt", 24, 0, 9); A("kvgt", 16, 0, 9)
    A("wdwt", 8 * 31 * 4, 0, 9); A("bdwt", 32, 0, 9); A("lngt", 32, 0, 9); A("lnbt", 32, 0, 9)
    A("hmask", 128, 0, 9); A("brb", 288, 0, 9); A("zero_b", 4096, 0, 9)
    A("cosk", S * 4, 0, 1); A("sink", S * 4, 0, 1); A("cosq", TOWN * 4, 0, 3); A("sinq", TOWN * 4, 0, 3)
    A("xg_own", 16 * NEXT * 2, 1, 2); A("xoth", 16 * 512 * 2, 1, 1); A("xoth1", 16 * 512 * 2, 1, 1); A("sq", 16 * 512 * 2, 1, 1)
    A("rstd_all", (2 * TOWN + 32) * 4, 1, 2); A("wkv", 16 * 640 * 2, 1, 1)
    A("kvlat", 4 * 512 * 4, 1, 1); A("sq2", 4 * 512 * 2, 1, 1); A("kro", 512 * 4, 1, 1); A("krs", 512 * 4, 1, 1)
    A("rstd2", 512 * 4, 1, 1); A("tmpa", 512 * 4, 1, 1)
    A("kvn", 4 * S * 2, 1, 4); A("kr", S * 2, 1, 5)
    A("wq0", 16 * 384 * 2, 2, 2); A("wq1", 16 * 384 * 2, 2, 2); A("wu0", 16 * 256 * 2, 2, 2); A("wu1", 16 * 256 * 2, 2, 2)
    A("qlat", 6 * 512 * 4, 2, 2); A("sqq", 6 * 512 * 2, 2, 2); A("t1", 512 * 4, 2, 2); A("t2", 512 * 4, 2, 2)
    A("sg", 512 * 4, 2, 2); A("rstdq", 512 * 4, 2, 2)
    A("qn", 6 * TOWN * 2, 2, 3); A("c_ext", 8 * NEXT * 2, 2, 3)
    A("AT", 16 * TOWN * 2, 3, 6)
    A("sum1", TOWN * 4, 3, 3); A("sum2", TOWN * 4, 3, 3)
    A("convf0", 512 * 4, 3, 3); A("convf1", 512 * 4, 3, 3); A("convsq0", 512 * 4, 3, 3); A("convsq1", 512 * 4, 3, 3)
    A("diag0", 31 * 128 * 2, 3, 3); A("diag1", 31 * 128 * 2, 3, 3)
    A("lnmean", TOWN * 4, 3, 3); A("lnrstd", TOWN * 4, 3, 3)
    A("wuq", 6 * 1536 * 2, 3, 3); A("wqs", 6 * 512 * 2, 3, 3); A("QN", 8 * TOWN * 2, 3, 5); A("QR", 8 * TOWN * 2, 3, 5)
    A("rt1", 512 * 4, 3, 3); A("rt2", 512 * 4, 3, 3)
    A("wukv", 4 * 2048 * 2, 4, 4); A("KT", 8 * S * 2, 4, 5); A("Vaug", 16 * 8 * 129 * 2 + 64, 4, 5)
    A("PT0", 16 * 512 * 2, 5, 5); A("PT1", 16 * 512 * 2, 5, 5); A("rcp", 64, 5, 5)
    A("Otok0", 256, 5, 5); A("Otok1", 256, 5, 5)
    A("wo0", 16 * 512 * 2, 6, 6); A("wo1", 16 * 512 * 2, 6, 6); A("h", 8 * D * 4, 6, 7)
    A("g2b", D * 4, 7, 7); A("hnf", D * 4, 7, 7); A("hnb", 8 * D * 2, 7, 7); A("hnT", 16 * 128 * 4, 7, 7)
    A("hnf1", D * 4, 7, 7); A("hnT1", 16 * 128 * 4, 7, 7)
    A("wr", 16 * 72 * 4, 7, 7); A("ssq7", 64, 7, 7); A("L", 8 * 72 * 4, 7, 7)
    for nm in ("gmax", "gsum", "pg", "m1", "m2", "w1", "w2", "r1", "r2", "v1", "v2", "d1", "d2"):
        A(nm, 32, 7, 7)
    for nm in ("gd", "gmask"):
        A(nm, 8 * 8 * 4, 7, 7)
    for nm in ("sel", "sel2", "mask1", "mask2"):
        A(nm, 8 * 8 * 4, 7, 7)
    for nm in ("rl4", "A1", "A2", "rank", "tmp64"):
        A(nm, 8 * 64 * 4, 7, 7)
    A("Abf", 8 * 64 * 2, 7, 7)
    A("dest_i", 16 * 4, 7, 9); A("gates", 16 * 4, 7, 9)
    for i in range(2):
        A(f"wg{i}", 16 * 512 * 2, 8, 8); A(f"wu_{i}", 16 * 512 * 2, 8, 8); A(f"wd{i}", 4 * D * 2, 8, 8)
        A(f"Xe{i}", D * 2, 8, 8); A(f"XT{i}", 16 * 128 * 2, 8, 8); A(f"sgt{i}", 512 * 4, 8, 8)
        A(f"hb{i}", 512 * 2, 8, 8); A(f"HT{i}", 4 * 128 * 2, 8, 8); A(f"Ysb{i}", D * 4, 8, 8)
        A(f"wdf{i}", 2 * D * 4, 8, 8)
    A("fgb", D * 4, 9, 9)
    for i in range(2):
        A(f"Y1_{i}", D * 4, 9, 9); A(f"Y2_{i}", D * 4, 9, 9); A(f"hz{i}", D * 4, 9, 9); A(f"ss9_{i}", 64, 9, 9)
    offs = ar.solve(SBUF_BYTES)
    sizes = {n: b for (n, b, _, _) in ar.items}

    stack = contextlib.ExitStack()
    arena = stack.enter_context(nc.sbuf_tensor("arena", [128, SBUF_BYTES], U8))
    banks = [stack.enter_context(nc.psum_tensor(f"ps{i}", [128, 512], F32)) for i in range(8)]

    def V(name, dt, shape=None, parts=128):
        nb = sizes[name]
        esz = {F32: 4, BF16: 2, I32: 4}[dt]
        ap = arena[0:parts, offs[name]:offs[name] + nb].bitcast(dt)
        if shape is None:
            return ap
        n = int(np.prod(shape))
        ap = ap[:, 0:n]
        if len(shape) == 1:
            return ap
        if len(shape) == 2:
            return ap.rearrange("p (a b) -> p a b", b=shape[1])
        if len(shape) == 3:
            return ap.rearrange("p (a b c) -> p a b c", b=shape[1], c=shape[2])
        raise ValueError

    ps_flag = [False]

    def ps_probe(fn):
        _Dummy.hit = False
        fn(_Dummy())
        return _Dummy.hit

    def PS(i, dt=F32, parts=128):
        ps_flag[0] = True
        ap = banks[i][0:parts, :]
        return ap if dt == F32 else ap.bitcast(dt)

    def emit_rstd(dst, src, inv_n, waits=()):
        t_a = P.op("dve", lambda e: e.tensor_scalar(out=dst, in0=src, scalar1=inv_n, scalar2=EPS, op0=ALU.mult, op1=ALU.add),
                   waits=list(waits), sig=True)
        t_b = P.op("act", lambda e: e.activation(out=dst, in_=dst, func=AF.Sqrt), waits=[t_a], sig=True)
        t_c = P.op("dve", lambda e: e.reciprocal(out=dst, in_=dst), waits=[t_b], sig=True)
        return t_a, t_c

    P = Prog(nc)

    def finish():
        P.barrier()
        fin = [P.dma_last[k] for k in ("out0", "out1", "dbg") if k in P.dma_last]
        P.wait_only("sync", fin)
        P.emit(stack, ps_probe)
        stack.close()
        return nc, list(dbg_out.keys())

    def dump(name, ap, shape, dt):
        if debug:
            d = dbg(name, shape, dt)
            P.dma("sync", lambda e: e.dma_start(out=d, in_=ap), "dbg")

    ident_f = V("ident_f", F32, [128]); ident_b = V("ident_b", BF16, [128])
    ones_b = V("ones_b", BF16, [128]); ones_f = V("ones_f", F32, [128])
    triu_b = V("triu_b", BF16, [128]); triu_f = V("triu_f", F32, [128]); e128 = V("e128", F32, [NE])
    g1t = V("g1t", F32, [16]); bglut = V("bglut", F32, [16]); qgt = V("qgt", F32, [6]); kvgt = V("kvgt", F32, [4])
    wdwt = V("wdwt", F32, [8, 31]); bdwt = V("bdwt", F32, [8]); lngt = V("lngt", F32, [8]); lnbt = V("lnbt", F32, [8])
    hmask = V("hmask", F32, [32]); brb = V("brb", F32, [72]); zero_b = V("zero_b", BF16, [2048])
    cosk_t = V("cosk", F32, [S], parts=64); sink_t = V("sink", F32, [S], parts=64)
    cosq_t = V("cosq", F32, [TOWN], parts=64); sinq_t = V("sinq", F32, [TOWN], parts=64)

    cl = []
    for dst, src in ((ident_f, ident_d), (triu_f, triu_d), (e128, e128_d), (g1t, g1_d), (bglut, bglu_d), (qgt, qg_d),
                     (kvgt, kvg_d), (wdwt, wdw_d), (bdwt, bdw_d), (lngt, lng_d), (lnbt, lnb_d), (hmask, halo_mask),
                     (brb, brb_d), (cosk_t, cosk), (sink_t, sink), (cosq_t, cosk[:, 0:TOWN]), (sinq_t, sink[:, 0:TOWN])):
        cl.append(P.dma("sync", (lambda d, s: (lambda e: e.dma_start(out=d, in_=s)))(dst, src), "const"))
    tc0 = cl[-1]
    P.op("dve", lambda e: e.tensor_copy(out=ident_b, in_=ident_f), waits=[tc0])
    P.op("dve", lambda e: e.tensor_copy(out=triu_b, in_=triu_f))
    P.op("dve", lambda e: e.memset(ones_b, 1.0))
    P.op("dve", lambda e: e.memset(ones_f, 1.0))
    P.op("dve", lambda e: e.memset(zero_b, 0.0))
    P.barrier()
    if stop <= 0:
        return finish()

    xg_own = V("xg_own", BF16, [16, NEXT]); xoth = V("xoth", BF16, [16, 512]); sq = V("sq", BF16, [16, 512])
    rstd_all = V("rstd_all", F32, [2 * TOWN + 32]); wkv = V("wkv", BF16, [16, 640])
    kvlat = V("kvlat", F32, [4, 512]); sq2 = V("sq2", BF16, [4, 512])
    kro = V("kro", F32, [512], parts=64); krs = V("krs", F32, [512], parts=64)
    rstd2 = V("rstd2", F32, [512]); tmpa = V("tmpa", F32, [512])
    kvn = V("kvn", BF16, [4, S]); kr = V("kr", BF16, [S], parts=64)
    xTv = xT.rearrange("(kc p) t -> p kc t", p=128)
    w_in_v = w_in.rearrange("(kc p) c -> p kc c", p=128)

    t_wkv = P.dma("pool", lambda e: e.dma_start(out=wkv[:, :, 0:576], in_=w_in_v[:, :, 768:1344]), "wkv")
    t_sw = P.op("act", lambda e: e.copy(out=wkv[:, :, 576:608], in_=wkv[:, :, 544:576]), waits=[t_wkv])
    t_sw = P.op("act", lambda e: e.copy(out=wkv[:, :, 608:640], in_=wkv[:, :, 512:544]), sig=True)

    xoth1 = V("xoth1", BF16, [16, 512])
    blocks = [(0, 512, xg_own[:, :, 0:512], True), (512, 512, xg_own[:, :, 512:1024], True),
              (1024, 512, xoth, True), (1536, 512, xoth1, True), (2048, 32, xg_own[:, :, 1024:1056], False)]
    ps_free = [None] * 8
    st1 = {"sq_free": None}
    blk = {}

    def stage_Xa(bi):
        c0, nb, xb, is_kv = blocks[bi]
        tx = P.dma("pool", (lambda xb=xb, c0=c0, nb=nb: (lambda e: e.dma_start(out=xb, in_=xTv[:, :, c0:c0 + nb])))(), f"xblk{bi}")
        sqv = sq[:, :, 0:nb]
        tsq = P.op("act", (lambda xb=xb, sqv=sqv: (lambda e: e.activation(out=sqv, in_=xb, func=AF.Square)))(),
                   waits=[tx, st1["sq_free"]], sig=True)
        for kc in range(16):
            t_xg = P.op("dve", (lambda xb=xb, kc=kc: (lambda e: e.tensor_scalar(out=xb[:, kc, :], in0=xb[:, kc, :],
                        scalar1=g1t[:, kc:kc + 1], scalar2=None, op0=ALU.mult)))(), waits=[tsq] if kc == 0 else [], sig=(kc == 15))
        blk[bi] = {"tsq": tsq, "t_xg": t_xg, "sqv": sqv}

    def stage_Xb(bi):
        c0, nb, xb, is_kv = blocks[bi]
        sqv = blk[bi]["sqv"]
        for kc in range(16):
            tpe = P.op("pe", (lambda kc=kc, sqv=sqv, nb=nb: (lambda e: e.matmul(PS(0)[:, 0:nb], ones_b, sqv[:, kc, :],
                       start=(kc == 0), stop=(kc == 15))))(), waits=[blk[bi]["tsq"], ps_free[0]] if kc == 0 else [], sig=(kc == 15))
        st1["sq_free"] = tpe
        rs = rstd_all[:, c0:c0 + nb]
        ps_free[0], t_rs = emit_rstd(rs, PS(0)[:, 0:nb], 1.0 / D, waits=[tpe])
        blk[bi]["rs"] = rs
        blk[bi]["t_rs"] = t_rs

    def stage_K(bi):
        c0, nb, xb, is_kv = blocks[bi]
        rs, t_rs, t_xg = blk[bi]["rs"], blk[bi]["t_rs"], blk[bi]["t_xg"]
        for m in range(4):
            for kc in range(16):
                tpe = P.op("pe", (lambda m=m, kc=kc, xb=xb: (lambda e: e.matmul(PS(1 + m), wkv[:, kc, 128 * m:128 * m + 128],
                           xb[:, kc, :], start=(kc == 0), stop=(kc == 15))))(),
                           waits=[t_xg, t_sw, ps_free[1 + m]] if kc == 0 else [], sig=(kc == 15))
            ps_free[1 + m] = P.op("dve", (lambda m=m, rs=rs: (lambda e: e.tensor_tensor(out=kvlat[:, m, :], in0=PS(1 + m), in1=rs,
                                  op=ALU.mult)))(), waits=[tpe, t_rs], sig=True)
        t_kvl = ps_free[4]
        for j, (cc, dst) in enumerate(((512, kro), (576, krs))):
            for kc in range(16):
                tpe = P.op("pe", (lambda j=j, kc=kc, cc=cc, xb=xb: (lambda e: e.matmul(PS(5 + j, parts=64), wkv[:, kc, cc:cc + 64],
                           xb[:, kc, :], start=(kc == 0), stop=(kc == 15))))(),
                           waits=[ps_free[5 + j]] if kc == 0 else [], sig=(kc == 15))
            ps_free[5 + j] = P.op("dve", (lambda j=j, dst=dst, rs=rs: (lambda e: e.tensor_tensor(out=dst, in0=PS(5 + j, parts=64),
                                  in1=rs[0:64, :], op=ALU.mult)))(), waits=[tpe], sig=True)
        P.op("dve", (lambda c0=c0: (lambda e: e.tensor_tensor(out=kro, in0=kro, in1=cosk_t[:, c0:c0 + 512], op=ALU.mult)))())
        P.op("dve", (lambda c0=c0: (lambda e: e.tensor_tensor(out=krs, in0=krs, in1=sink_t[:, c0:c0 + 512], op=ALU.mult)))())
        P.op("dve", (lambda c0=c0: (lambda e: e.tensor_tensor(out=kr[:, c0:c0 + 512], in0=kro, in1=krs, op=ALU.add)))())
        tsq2 = P.op("act", lambda e: e.activation(out=sq2, in_=kvlat, func=AF.Square), waits=[t_kvl], sig=True)
        for m in range(4):
            tpe = P.op("pe", (lambda m=m: (lambda e: e.matmul(PS(7), ones_b, sq2[:, m, :], start=(m == 0), stop=(m == 3))))(),
                       waits=[tsq2, ps_free[7]] if m == 0 else [], sig=(m == 3))
        ps_free[7], _t = emit_rstd(rstd2, PS(7), 1.0 / 512, waits=[tpe])
        for m in range(4):
            P.op("dve", (lambda m=m, c0=c0: (lambda e: e.scalar_tensor_tensor(out=kvn[:, m, c0:c0 + 512], in0=kvlat[:, m, :],
                 scalar=kvgt[:, m:m + 1], in1=rstd2, op0=ALU.mult, op1=ALU.mult)))())

    stage_Xa(0)
    stage_Xb(0)
    for bi in range(5):
        if bi + 1 < 5:
            stage_Xa(bi + 1)
        if blocks[bi][3]:
            stage_K(bi)
        if bi + 1 < 5:
            stage_Xb(bi + 1)
    P.barrier()
    dump("kvn", kvn, [128, 4, S], BF16)
    dump("kr", kr, [64, S], BF16)
    dump("rstd_all", rstd_all, [128, 2 * TOWN + 32], F32)
    dump("xg_own", xg_own, [128, 16, NEXT], BF16)
    if stop <= 1:
        return finish()

    wqb = [V("wq0", BF16, [16, 384]), V("wq1", BF16, [16, 384])]
    wub = [V("wu0", BF16, [16, 256]), V("wu1", BF16, [16, 256])]
    qlat = V("qlat", F32, [6, 512]); sqq = V("sqq", BF16, [6, 512])
    t1 = V("t1", F32, [512]); t2 = V("t2", F32, [512]); sgv = V("sg", F32, [512]); rstdq = V("rstdq", F32, [512])
    qn = V("qn", BF16, [6, TOWN]); c_ext = V("c_ext", BF16, [8, NEXT])
    ps_free = [None] * 8
    t_wq = [P.dma("pool", (lambda i=i: (lambda e: e.dma_start(out=wqb[i], in_=w_in_v[:, :, 384 * i:384 * i + 384])))(), f"wq{i}")
            for i in range(2)]
    t_qn_done = None
    for blk in range(2):
        tk = slice(512 * blk, 512 * blk + 512)
        for m in range(6):
            bank = m % 4
            for kc in range(16):
                tpe = P.op("pe", (lambda m=m, kc=kc, bank=bank, tk=tk: (lambda e: e.matmul(PS(bank), wqb[m // 3][:, kc, 128 * (m % 3):128 * (m % 3) + 128],
                           xg_own[:, kc, tk], start=(kc == 0), stop=(kc == 15))))(),
                           waits=[t_wq[m // 3], ps_free[bank]] if kc == 0 else [], sig=(kc == 15))
            ps_free[bank] = P.op("dve", (lambda m=m, bank=bank, tk=tk: (lambda e: e.tensor_tensor(out=qlat[:, m, :], in0=PS(bank),
                                 in1=rstd_all[:, tk], op=ALU.mult)))(), waits=[tpe, t_qn_done], sig=True)
        tsq = P.op("act", lambda e: e.activation(out=sqq, in_=qlat, func=AF.Square), waits=[ps_free[1]], sig=True)
        for m in range(6):
            tpe = P.op("pe", (lambda m=m: (lambda e: e.matmul(PS(4), ones_b, sqq[:, m, :], start=(m == 0), stop=(m == 5))))(),
                       waits=[tsq, ps_free[4]] if m == 0 else [], sig=(m == 5))
        ps_free[4], _t = emit_rstd(rstdq, PS(4), 1.0 / 768, waits=[tpe])
        for m in range(6):
            t_qn_done = P.op("dve", (lambda m=m, tk=tk: (lambda e: e.scalar_tensor_tensor(out=qn[:, m, tk], in0=qlat[:, m, :],
                             scalar=qgt[:, m:m + 1], in1=rstdq, op0=ALU.mult, op1=ALU.mult)))(), sig=(m == 5))
    wu_free = [None, None]
    ublocks = [(0, 512, 16), (512, 512, 16 + 512), (1024, 16, 0), (1040, 16, 16 + 1024)]
    for j in range(8):
        wb = wub[j % 2]
        ta = P.dma("pool", (lambda wb=wb, j=j: (lambda e: e.dma_start(out=wb[:, :, 0:128], in_=w_in_v[:, :, 1344 + 128 * j:1344 + 128 * j + 128])))(),
                   f"wua{j % 2}", waits=[wu_free[j % 2]])
        tg = P.dma("pool", (lambda wb=wb, j=j: (lambda e: e.dma_start(out=wb[:, :, 128:256], in_=w_in_v[:, :, 2368 + 128 * j:2368 + 128 * j + 128])))(),
                   f"wug{j % 2}", waits=[wu_free[j % 2]])
        for ui, (xc, n, cc) in enumerate(ublocks):
            ba, bg = (5, 6) if ui % 2 == 0 else (7, 0)
            for half, bank in ((0, ba), (1, bg)):
                for kc in range(16):
                    tpe = P.op("pe", (lambda wb=wb, half=half, bank=bank, kc=kc, xc=xc, n=n: (lambda e: e.matmul(PS(bank)[:, 0:n],
                               wb[:, kc, 128 * half:128 * half + 128], xg_own[:, kc, xc:xc + n], start=(kc == 0), stop=(kc == 15))))(),
                               waits=[ta, tg, ps_free[bank]] if kc == 0 else [], sig=(kc == 15))
                if half == 0:
                    tpa = tpe
            rsl = rstd_all[:, xc:xc + n] if xc < 1024 else rstd_all[:, 2048 + (xc - 1024):2048 + (xc - 1024) + n]
            ps_free[ba] = P.op("dve", (lambda ba=ba, n=n, rsl=rsl: (lambda e: e.tensor_tensor(out=t1[:, 0:n], in0=PS(ba)[:, 0:n], in1=rsl,
                               op=ALU.mult)))(), waits=[tpa], sig=True)
            ps_free[bg] = P.op("dve", (lambda bg=bg, n=n, rsl=rsl: (lambda e: e.tensor_tensor(out=t2[:, 0:n], in0=PS(bg)[:, 0:n], in1=rsl,
                               op=ALU.mult)))(), waits=[tpe], sig=True)
            tsg = P.op("act", (lambda n=n, j=j: (lambda e: e.activation(out=sgv[:, 0:n], in_=t2[:, 0:n], func=AF.Sigmoid,
                       bias=bglut[:, 8 + j:9 + j], scale=1.0)))(), waits=[ps_free[bg]], sig=True)
            P.op("dve", (lambda n=n, j=j, cc=cc: (lambda e: e.scalar_tensor_tensor(out=c_ext[:, j, cc:cc + n], in0=t1[:, 0:n],
                 scalar=bglut[:, j:j + 1], in1=sgv[:, 0:n], op0=ALU.add, op1=ALU.mult)))(), waits=[tsg])
            if ui >= 2:
                hm = hmask[:, 0:16] if ui == 2 else hmask[:, 16:32]
                P.op("dve", (lambda n=n, j=j, cc=cc, hm=hm: (lambda e: e.tensor_tensor(out=c_ext[:, j, cc:cc + n],
                     in0=c_ext[:, j, cc:cc + n], in1=hm, op=ALU.mult)))())
        wu_free[j % 2] = tpe
    P.barrier()
    dump("qn", qn, [128, 6, TOWN], BF16)
    dump("c_ext", c_ext, [128, 8, NEXT], BF16)
    if stop <= 2:
        return finish()

    AT = V("AT", BF16, [16, TOWN])
    sum1 = V("sum1", F32, [TOWN]); sum2 = V("sum2", F32, [TOWN])
    convf = [V("convf0", F32, [512]), V("convf1", F32, [512])]
    convsq = [V("convsq0", F32, [512]), V("convsq1", F32, [512])]
    diag = [V("diag0", BF16, [31, 128]), V("diag1", BF16, [31, 128])]
    lnmean = V("lnmean", F32, [TOWN]); lnrstd = V("lnrstd", F32, [TOWN])
    wuq = V("wuq", BF16, [6, 1536]); wqs = V("wqs", BF16, [6, 512])
    QN = V("QN", BF16, [8, TOWN]); QR = V("QR", BF16, [8, TOWN], parts=64)
    rt1 = V("rt1", F32, [512], parts=64); rt2 = V("rt2", F32, [512], parts=64)
    w_uq_v = w_uq.rearrange("(kc p) c -> p kc c", p=128)
    KDC = ""
    if "w" in KDC:
        t_wuq = None; t_wqs = None
    else:
        t_wuq = P.dma("pool", lambda e: e.dma_start(out=wuq, in_=w_uq_v), "wuq")
    wuq4 = wuq.rearrange("p k (h c) -> p k h c", c=192)
    wqs4 = wqs.rearrange("p k (h c) -> p k h c", c=64)
    for kc in range(6):
        if "w" in KDC:
            break
        P.op("act", (lambda kc=kc: (lambda e: e.copy(out=wqs4[:, kc, :, 0:32], in_=wuq4[:, kc, :, 160:192])))(), waits=[t_wuq])
        t_wqs = P.op("act", (lambda kc=kc: (lambda e: e.copy(out=wqs4[:, kc, :, 32:64], in_=wuq4[:, kc, :, 128:160])))(), sig=True)
    qp_free = [None] * 4

    def emit_qproj(u):
        h, blk = u // 2, u % 2
        tk = slice(512 * blk, 512 * blk + 512)
        bank = u % 2
        for kc in range(6):
            tpe = P.op("pe", (lambda h=h, kc=kc, tk=tk, bank=bank: (lambda e: e.matmul(PS(bank), wuq[:, kc, 192 * h:192 * h + 128],
                       qn[:, kc, tk], start=(kc == 0), stop=(kc == 5))))(), waits=[t_wuq, qp_free[bank]] if kc == 0 else [], sig=(kc == 5))
        qp_free[bank] = P.op("act", (lambda h=h, tk=tk, bank=bank: (lambda e: e.copy(out=QN[:, h, tk], in_=PS(bank))))(),
                             waits=[tpe], sig=True)
        for kc in range(6):
            tpr = P.op("pe", (lambda h=h, kc=kc, tk=tk: (lambda e: e.matmul(PS(2, parts=64), wuq[:, kc, 192 * h + 128:192 * h + 192],
                       qn[:, kc, tk], start=(kc == 0), stop=(kc == 5))))(), waits=[qp_free[2]] if kc == 0 else [], sig=(kc == 5))
        for kc in range(6):
            tps = P.op("pe", (lambda h=h, kc=kc, tk=tk: (lambda e: e.matmul(PS(3, parts=64), wqs[:, kc, 64 * h:64 * h + 64],
                       qn[:, kc, tk], start=(kc == 0), stop=(kc == 5))))(), waits=[t_wqs, qp_free[3]] if kc == 0 else [], sig=(kc == 5))
        qp_free[2] = P.op("dve", (lambda tk=tk: (lambda e: e.tensor_tensor(out=rt1, in0=PS(2, parts=64), in1=cosq_t[:, tk], op=ALU.mult)))(),
                          waits=[tpr], sig=True)
        qp_free[3] = P.op("dve", (lambda tk=tk: (lambda e: e.tensor_tensor(out=rt2, in0=PS(3, parts=64), in1=sinq_t[:, tk], op=ALU.mult)))(),
                          waits=[tps], sig=True)
        P.op("dve", (lambda h=h, tk=tk: (lambda e: e.tensor_tensor(out=QR[:, h, tk], in0=rt1, in1=rt2, op=ALU.add)))())

    PENG = "pool"
    P.op(PENG, lambda e: e.memset(sum1, 0.0))
    P.op(PENG, lambda e: e.memset(sum2, 0.0))
    diag_free = [None, None]
    cv_free = [None, None]
    cbank_free = [[], [], [], []]
    ci = 0
    for j in range(8):
        db = diag[j % 2]
        t_dg = P.op(PENG, (lambda db=db, j=j: (lambda e: e.tensor_tensor(out=db, in0=ident_f.unsqueeze(1).to_broadcast([128, 31, 128]),
                    in1=wdwt[:, j, :].unsqueeze(2).to_broadcast([128, 31, 128]), op=ALU.mult)))(), waits=[diag_free[j % 2]], sig=True)
        for blk in range(2):
            bank = 4 + ci % 4
            cb = ci % 2
            ci += 1
            sl = slice(512 * blk, 512 * blk + 512)
            for k in range(31):
                if "m" in KDC:
                    tpe = None
                    break
                c0 = k + 1 + 512 * blk
                if None:
                    c0 = (c0 // 2) * 2
                tpe = P.op("pe", (lambda db=db, j=j, k=k, c0=c0, bank=bank: (lambda e: e.matmul(PS(bank), db[:, k, :], c_ext[:, j, c0:c0 + 512],
                           start=(k == 0), stop=(k == 30))))(), waits=([t_dg] + cbank_free[bank - 4]) if k == 0 else [], sig=(k == 30))
            t_cf = None if "d" in KDC else P.op("dve", (lambda j=j, cb=cb, bank=bank: (lambda e: e.tensor_scalar(out=convf[cb], in0=PS(bank), scalar1=bdwt[:, j:j + 1],
                        scalar2=None, op0=ALU.add)))(), waits=[tpe, cv_free[cb]], sig=True)
            if "a" in KDC:
                cbank_free[bank - 4] = [t_cf]
                continue
            t_at = P.op("act", (lambda j=j, sl=sl, bank=bank: (lambda e: e.activation(out=AT[:, 8 + j, sl], in_=PS(bank), func=AF.Identity,
                        bias=bdwt[:, j:j + 1], scale=1.0)))(), waits=[tpe])
            t_sq = P.op("act", (lambda j=j, cb=cb, bank=bank: (lambda e: e.activation(out=convsq[cb], in_=PS(bank), func=AF.Square,
                        bias=bdwt[:, j:j + 1], scale=1.0)))(), waits=[cv_free[cb]], sig=True)
            cbank_free[bank - 4] = [t_cf, t_sq]
            if "p" in KDC:
                continue
            P.op(PENG, (lambda cb=cb, sl=sl: (lambda e: e.tensor_tensor(out=sum1[:, sl], in0=sum1[:, sl], in1=convf[cb], op=ALU.add)))(),
                 waits=[t_cf])
            cv_free[cb] = P.op(PENG, (lambda cb=cb, sl=sl: (lambda e: e.tensor_tensor(out=sum2[:, sl], in0=sum2[:, sl], in1=convsq[cb],
                               op=ALU.add)))(), waits=[t_sq], sig=True)
        diag_free[j % 2] = tpe
        if not None:
            emit_qproj(2 * j)
            emit_qproj(2 * j + 1)
    if None == "1":
        return finish()
    for blk in range(2):
        for which, src in ((0, sum1), (1, sum2)):
            bank = 4 + 2 * which + blk
            pe_ln = P.op("pe", (lambda blk=blk, src=src, bank=bank: (lambda e: e.matmul(PS(bank), ones_f,
                         src[:, 512 * blk:512 * blk + 512], start=True, stop=True)))(),
                         waits=[cv_free[0], cv_free[1]] + cbank_free[bank - 4], sig=True)
    if None == "2":
        return finish()
    for blk in range(2):
        sl = slice(512 * blk, 512 * blk + 512)
        P.op("dve", (lambda blk=blk, sl=sl: (lambda e: e.tensor_scalar(out=lnmean[:, sl], in0=PS(4 + blk), scalar1=1.0 / 1024, scalar2=None,
             op0=ALU.mult)))(), waits=[pe_ln])
        P.op("dve", (lambda blk=blk, sl=sl: (lambda e: e.tensor_scalar(out=lnrstd[:, sl], in0=PS(6 + blk), scalar1=1.0 / 1024, scalar2=None,
             op0=ALU.mult)))())
    P.op("dve", lambda e: e.tensor_tensor(out=sum1, in0=lnmean, in1=lnmean, op=ALU.mult))
    P.op("dve", lambda e: e.tensor_tensor(out=sum2, in0=lnrstd, in1=sum1, op=ALU.subtract))
    _t, t_lr = emit_rstd(lnrstd, sum2, 1.0)
    ybufs = [sum1, sum2]
    y_free = [None, None]
    for j in range(8):
        yb = ybufs[j % 2]
        P.op("dve", (lambda j=j, yb=yb: (lambda e: e.tensor_tensor(out=yb, in0=AT[:, 8 + j, :], in1=lnmean, op=ALU.subtract)))(),
             waits=[y_free[j % 2], t_lr])
        ty = P.op("dve", (lambda yb=yb: (lambda e: e.tensor_tensor(out=yb, in0=yb, in1=lnrstd, op=ALU.mult)))(), sig=True)
        y_free[j % 2] = P.op("act", (lambda j=j, yb=yb: (lambda e: e.activation(out=AT[:, 8 + j, :], in_=yb, func=AF.Silu,
                             bias=lnbt[:, j:j + 1], scale=lngt[:, j:j + 1])))(), waits=[ty], sig=True)
    P.barrier()
    dump("AT3", AT, [128, 16, TOWN], BF16)
    dump("QN", QN, [128, 8, TOWN], BF16)
    dump("QR", QR, [64, 8, TOWN], BF16)
    if stop <= 3:
        return finish()

    wukv = V("wukv", BF16, [4, 2048]); KT = V("KT", BF16, [8, S])
    Vaug = V("Vaug", BF16, [16 * 8 * 129 + 32])[:, 0:16 * 8 * 129].rearrange("p (k h c) -> p k h c", h=8, c=129)
    w_ukv_v = w_ukv.rearrange("(kc p) c -> p kc c", p=128)
    t_wukv = P.dma("pool", lambda e: e.dma_start(out=wukv, in_=w_ukv_v), "wukv")
    for kc16 in range(16):
        P.op("pool", (lambda kc16=kc16: (lambda e: e.memset(Vaug[:, kc16, :, 128:129], 1.0)))())
    ps_free = [None] * 8
    i = 0
    for h in range(8):
        for kb in range(4):
            bank = i % 4
            i += 1
            for c in range(4):
                tpe = P.op("pe", (lambda h=h, kb=kb, c=c, bank=bank: (lambda e: e.matmul(PS(bank), wukv[:, c, 256 * h:256 * h + 128],
                           kvn[:, c, 512 * kb:512 * kb + 512], start=(c == 0), stop=(c == 3))))(),
                           waits=[t_wukv, ps_free[bank]] if c == 0 else [], sig=(c == 3))
            eng = "act" if i % 2 == 0 else "dve"
            if eng == "act":
                ps_free[bank] = P.op("act", (lambda h=h, kb=kb, bank=bank: (lambda e: e.copy(out=KT[:, h, 512 * kb:512 * kb + 512],
                                     in_=PS(bank))))(), waits=[tpe], sig=True)
            else:
                ps_free[bank] = P.op("dve", (lambda h=h, kb=kb, bank=bank: (lambda e: e.tensor_copy(out=KT[:, h, 512 * kb:512 * kb + 512],
                                     in_=PS(bank))))(), waits=[tpe], sig=True)
    wukv4 = wukv.rearrange("p k (h c) -> p k h c", c=256)
    i = 0
    for kc16 in range(16):
        for hg in range(2):
            bank = 4 + i % 4
            i += 1
            for c in range(4):
                tpe = P.op("pe", (lambda kc16=kc16, hg=hg, c=c, bank=bank: (lambda e: e.matmul(
                           PS(bank).rearrange("p (h c) -> p h c", c=128), kvn[:, c, 128 * kc16:128 * kc16 + 128],
                           wukv4[:, c, 4 * hg:4 * hg + 4, 128:256], start=(c == 0), stop=(c == 3))))(),
                           waits=[t_wukv, ps_free[bank]] if c == 0 else [], sig=(c == 3))
            if i % 2 == 0:
                ps_free[bank] = P.op("act", (lambda kc16=kc16, hg=hg, bank=bank: (lambda e: e.copy(out=Vaug[:, kc16, 4 * hg:4 * hg + 4, 0:128],
                                     in_=PS(bank).rearrange("p (h c) -> p h c", c=128))))(), waits=[tpe], sig=True)
            else:
                ps_free[bank] = P.op("dve", (lambda kc16=kc16, hg=hg, bank=bank: (lambda e: e.tensor_copy(out=Vaug[:, kc16, 4 * hg:4 * hg + 4, 0:128],
                                     in_=PS(bank).rearrange("p (h c) -> p h c", c=128))))(), waits=[tpe], sig=True)
    P.barrier()
    dump("KT", KT, [128, 8, S], BF16)
    dump("Vaug", Vaug, [128, 16, 8, 129], BF16)
    if stop <= 4:
        return finish()

    PT = [V("PT0", BF16, [16, 512]), V("PT1", BF16, [16, 512])]
    rcp = V("rcp", F32, [16]); Otok = [V("Otok0", BF16, [128]), V("Otok1", BF16, [128])]
    Xd_v = Xd.rearrange("(e p) d -> e p d", p=128)
    for e_ in range(NE):
        P.dma("sync", (lambda e_=e_: (lambda e: e.dma_start(out=Xd_v[e_], in_=zero_b)))(), "xdz")
    steps = [(h, qb) for h in range(8) for qb in range(2)]
    st_bank_free = [None] * 4
    exp_done = {}
    pv_done = {}
    o_bank_free = [None, None]
    otok_free = [None, None]
    tile_i = 0

    def emit_ST(s):
        nonlocal tile_i
        h, qb = steps[s]
        qs = slice(512 * qb, 512 * qb + 512)
        for kc in range(16):
            bank = tile_i % 4
            tile_i += 1
            P.op("pe", (lambda h=h, kc=kc, qs=qs, bank=bank: (lambda e: e.matmul(PS(bank), KT[:, h, 128 * kc:128 * kc + 128], QN[:, h, qs],
                 start=True, stop=False)))(), waits=[st_bank_free[bank]])
            tpe = P.op("pe", (lambda h=h, kc=kc, qs=qs, bank=bank: (lambda e: e.matmul(PS(bank), kr[:, 128 * kc:128 * kc + 128], QR[:, h, qs],
                       start=False, stop=True)))(), sig=True)
            w = [tpe]
            if kc == 0 and s >= 2:
                w.append(pv_done[s - 2])
            tex = P.op("act", (lambda s=s, kc=kc, bank=bank: (lambda e: e.activation(out=PT[s % 2][:, kc, :], in_=PS(bank), func=AF.Exp,
                       scale=SCALE)))(), waits=w, sig=True)
            st_bank_free[bank] = tex
        exp_done[s] = tex

    pending_tr = []

    def emit_PV(s):
        h, qb = steps[s]
        for qc in range(4):
            ob = qc % 2
            for kc in range(16):
                tpe = P.op("pe", (lambda s=s, h=h, kc=kc, qc=qc, ob=ob: (lambda e: e.matmul(PS(4 + ob)[:, 0:129],
                           PT[s % 2][:, kc, 128 * qc:128 * qc + 128], Vaug[:, kc, h, :], start=(kc == 0), stop=(kc == 15))))(),
                           waits=[exp_done[s], o_bank_free[ob]] if kc == 0 else [], sig=(kc == 15))
            P.op("dve", (lambda ob=ob, qc=qc: (lambda e: e.reciprocal(out=rcp[:, qc:qc + 1], in_=PS(4 + ob)[:, 128:129])))(), waits=[tpe])
            tn = P.op("dve", (lambda ob=ob, qc=qc: (lambda e: e.tensor_scalar(out=Otok[ob], in0=PS(4 + ob)[:, 0:128],
                      scalar1=rcp[:, qc:qc + 1], scalar2=None, op0=ALU.mult)))(), waits=[otok_free[ob]], sig=True)
            o_bank_free[ob] = tn
            pending_tr.append((s, qc, ob, tn))
            if len(pending_tr) >= 2:
                emit_TR(pending_tr.pop(0))
        pv_done[s] = tpe

    tr_i = 0

    tr_bank_free = [None, None]

    def emit_TR(item):
        s, qc, ob, tn = item
        h, qb = steps[s]
        bank = 6 + s % 2
        tt = P.op("pe", (lambda ob=ob, bank=bank, qc=qc: (lambda e: e.transpose(out=PS(bank, BF16)[:, 128 * qc:128 * qc + 128], in_=Otok[ob],
                  identity=ident_b)))(), waits=[tn] + ([tr_bank_free[s % 2]] if qc == 0 else []), sig=True)
        otok_free[ob] = tt
        if qc == 3:
            tr_bank_free[s % 2] = P.op("dve", (lambda h=h, qb=qb, bank=bank: (lambda e: e.tensor_copy(out=AT[:, h, 512 * qb:512 * qb + 512],
                                       in_=PS(bank, BF16)[:, 0:512])))(), waits=[tt], sig=True)

    KD5 = ""
    if "t" in KD5:
        emit_TR = lambda item: None
    if "z" in KD5:
        steps = steps[:2]
    emit_ST(0)
    for s in range(len(steps)):
        if s + 1 < len(steps):
            emit_ST(s + 1)
        if "p" not in KD5:
            emit_PV(s)
    while pending_tr:
        emit_TR(pending_tr.pop(0))
    if debug:
        d_at = dbg("AT", [128, 16, TOWN], BF16)
        P.barrier()
        P.dma("sync", lambda e: e.dma_start(out=d_at, in_=AT), "dbg")
    P.barrier()
    if stop <= 5:
        return finish()

    wob = [V("wo0", BF16, [16, 512]), V("wo1", BF16, [16, 512])]
    hT = V("h", F32, [8, D])
    w_o_v = w_o.rearrange("(kc p) c -> p kc c", p=128)
    t_x = P.dma("sync", lambda e: e.dma_start(out=hT, in_=xown.rearrange("(t p) d -> p t d", p=128)), "xown")
    wo_free = [None, None]
    ps_free = [None] * 8
    i = 0
    for g in range(4):
        two = P.dma("pool", (lambda g=g: (lambda e: e.dma_start(out=wob[g % 2], in_=w_o_v[:, :, 512 * g:512 * g + 512])))(),
                    f"wo{g % 2}", waits=[wo_free[g % 2]])
        for tt in range(8):
            bank = i % 4
            i += 1
            for f in range(16):
                tpe = P.op("pe", (lambda g=g, tt=tt, f=f, bank=bank: (lambda e: e.matmul(PS(bank), AT[:, f, 128 * tt:128 * tt + 128],
                           wob[g % 2][:, f, :], start=(f == 0), stop=(f == 15))))(), waits=[two, ps_free[bank]] if f == 0 else [], sig=(f == 15))
            ps_free[bank] = P.op("dve", (lambda g=g, tt=tt, bank=bank: (lambda e: e.tensor_tensor(out=hT[:, tt, 512 * g:512 * g + 512],
                                 in0=hT[:, tt, 512 * g:512 * g + 512], in1=PS(bank), op=ALU.add)))(), waits=[tpe, t_x], sig=True)
        wo_free[g % 2] = tpe
    P.barrier()
    if stop <= 6:
        return finish()
    if debug:
        d_h = dbg("h", [TOWN, D], F32)
        P.dma("sync", lambda e: e.dma_start(out=d_h.rearrange("(t p) d -> p t d", p=128), in_=hT), "dbg")

    g2b = V("g2b", F32, [D]); hnf = V("hnf", F32, [D]); hnb = V("hnb", BF16, [8, D]); hnT = V("hnT", F32, [16, 128])
    wr = V("wr", F32, [16, 72]); ssq7 = V("ssq7", F32, [16]); L = V("L", F32, [8, 72])
    sm = {nm: V(nm, F32, [8]) for nm in ("gmax", "gsum", "pg", "m1", "m2", "w1", "w2", "r1", "r2", "v1", "v2", "d1", "d2")}
    gd = V("gd", F32, [8, 8]); gmask = V("gmask", F32, [8, 8])
    sel = V("sel", F32, [8, 8]); sel2 = V("sel2", F32, [8, 8]); mask1 = V("mask1", F32, [8, 8]); mask2 = V("mask2", F32, [8, 8])
    rl4 = V("rl4", F32, [8, 8, 8]); A1 = V("A1", F32, [8, 8, 8]); A2 = V("A2", F32, [8, 8, 8])
    rank = V("rank", F32, [8, 64]); tmp64 = V("tmp64", F32, [8, 64]); Abf = V("Abf", BF16, [8, 64])
    dest_i = V("dest_i", I32, [16]); gates = V("gates", F32, [16])
    t_g2 = P.dma("sync", lambda e: e.dma_start(out=g2b, in_=g2b_d), "g2b")
    t_wr = P.dma("sync", lambda e: e.dma_start(out=wr, in_=w_r.rearrange("(kc p) c -> p kc c", p=128)), "wr")
    t_hs = P.dma("sync", lambda e: e.dma_start(out=hbuf.rearrange("(t p) d -> p t d", p=128), in_=hT), "hbuf")
    ps_free = [None] * 8
    hnfb = [hnf, V("hnf1", F32, [D])]
    hnTb = [hnT, V("hnT1", F32, [16, 128])]
    hnf_free = [None, None]
    hnT_free = [None, None]
    hn_tok = {}
    P.op("dve", lambda e: e.memset(ssq7, 0.0))
    t_ms = P.op("dve", lambda e: e.memset(hnb[:, 0, 0:2], 0.0), sig=True)

    def stage_S(tt):
        hf = hnfb[tt % 2]
        t_sq = P.op("act", (lambda tt=tt, hf=hf: (lambda e: e.activation(out=hf, in_=hT[:, tt, :], func=AF.Square, accum_out=ssq7[:, tt:tt + 1])))(),
                    waits=[hnf_free[tt % 2], t_ms], sig=True)
        _t, t_r = emit_rstd(ssq7[:, 8 + tt:9 + tt], ssq7[:, tt:tt + 1], 1.0 / D, waits=[t_sq])
        t_hn = P.op("dve", (lambda tt=tt, hf=hf: (lambda e: e.scalar_tensor_tensor(out=hf, in0=hT[:, tt, :], scalar=ssq7[:, 8 + tt:9 + tt], in1=g2b,
                    op0=ALU.mult, op1=ALU.mult)))(), waits=[t_g2], sig=True)
        t_hb_ = P.op("act", (lambda tt=tt, hf=hf: (lambda e: e.copy(out=hnb[:, tt, :], in_=hf)))(), waits=[t_hn], sig=True)
        hn_tok[tt] = (t_hn, t_hb_)

    def stage_T(tt):
        hf = hnfb[tt % 2]
        hTt = hnTb[tt % 2]
        t_hn = hn_tok[tt][0]
        for q4 in range(4):
            bank = q4
            for r in range(4):
                kc = 4 * q4 + r
                tpe = P.op("pe", (lambda kc=kc, bank=bank, r=r, hf=hf: (lambda e: e.transpose(out=PS(bank)[:, 128 * r:128 * r + 128],
                           in_=hf[:, 128 * kc:128 * kc + 128], identity=ident_f)))(), waits=[t_hn, ps_free[bank]] if r == 0 else [], sig=(r == 3))
            if q4 % 2 == 0:
                ps_free[bank] = P.op("dve", (lambda q4=q4, bank=bank, hTt=hTt: (lambda e: e.tensor_copy(out=hTt[:, 4 * q4:4 * q4 + 4, :],
                                     in_=PS(bank).rearrange("p (a b) -> p a b", b=128))))(), waits=[tpe, hnT_free[tt % 2]], sig=True)
            else:
                ps_free[bank] = P.op("act", (lambda q4=q4, bank=bank, hTt=hTt: (lambda e: e.copy(out=hTt[:, 4 * q4:4 * q4 + 4, :],
                                     in_=PS(bank).rearrange("p (a b) -> p a b", b=128))))(), waits=[tpe, hnT_free[tt % 2]], sig=True)
        hnf_free[tt % 2] = tpe
        for kc in range(16):
            tpl = P.op("pe", (lambda kc=kc, hTt=hTt: (lambda e: e.matmul(PS(4)[:, 0:72], hTt[:, kc, :], wr[:, kc, :], start=(kc == 0), stop=(kc == 15))))(),
                       waits=[ps_free[0], ps_free[1], ps_free[2], ps_free[3], t_wr, ps_free[4]] if kc == 0 else [], sig=(kc == 15))
        hnT_free[tt % 2] = tpl
        ps_free[4] = P.op("dve", (lambda tt=tt: (lambda e: e.tensor_tensor(out=L[:, tt, :], in0=PS(4)[:, 0:72], in1=brb, op=ALU.add)))(),
                          waits=[tpl], sig=True)

    stage_S(0)
    for tt in range(8):
        if tt + 1 < 8:
            stage_S(tt + 1)
        stage_T(tt)
    t_hb = hn_tok[7][1]
    if debug:
        d_L = dbg("L", [128, 8, 72], F32)
        P.dma("sync", lambda e: e.dma_start(out=d_L, in_=L), "dbg", waits=[ps_free[4]])
    gl = L[:, :, 0:8]
    rlv = L[:, :, 8:72].rearrange("p t (g j) -> p t g j", j=8)

    def bc(ap, shape):
        return ap.to_broadcast(shape)

    dv = lambda fn, **kw: P.op("dve", fn, **kw)
    dv(lambda e: e.tensor_reduce(out=sm["gmax"], in_=gl, axis=AX.X, op=ALU.max))
    dv(lambda e: e.tensor_tensor(out=gd, in0=gl, in1=bc(sm["gmax"].unsqueeze(2), [128, 8, 8]), op=ALU.subtract))
    t_gd = dv(lambda e: e.tensor_tensor(out=gmask, in0=gl, in1=bc(sm["gmax"].unsqueeze(2), [128, 8, 8]), op=ALU.is_equal), sig=True)
    t_ge = P.op("act", lambda e: e.activation(out=gd, in_=gd, func=AF.Exp), waits=[t_gd], sig=True)
    dv(lambda e: e.tensor_reduce(out=sm["gsum"], in_=gd, axis=AX.X, op=ALU.add), waits=[t_ge])
    dv(lambda e: e.reciprocal(out=sm["pg"], in_=sm["gsum"]))
    dv(lambda e: e.tensor_tensor(out=rl4, in0=rlv, in1=bc(gmask.unsqueeze(3), [128, 8, 8, 8]), op=ALU.mult))
    dv(lambda e: e.tensor_reduce(out=sel, in_=rl4.rearrange("p t g j -> p t j g"), axis=AX.X, op=ALU.add))
    dv(lambda e: e.tensor_reduce(out=sm["m1"], in_=sel, axis=AX.X, op=ALU.max))
    dv(lambda e: e.tensor_tensor(out=mask1, in0=sel, in1=bc(sm["m1"].unsqueeze(2), [128, 8, 8]), op=ALU.is_equal))
    dv(lambda e: e.scalar_tensor_tensor(out=sel2, in0=mask1, scalar=-1e30, in1=sel, op0=ALU.mult, op1=ALU.add))
    dv(lambda e: e.tensor_reduce(out=sm["m2"], in_=sel2, axis=AX.X, op=ALU.max))
    dv(lambda e: e.tensor_tensor(out=mask2, in0=sel2, in1=bc(sm["m2"].unsqueeze(2), [128, 8, 8]), op=ALU.is_equal))
    t_w = dv(lambda e: e.tensor_tensor(out=sm["w1"], in0=sm["m2"], in1=sm["m1"], op=ALU.subtract), sig=True)
    t_we = P.op("act", lambda e: e.activation(out=sm["w1"], in_=sm["w1"], func=AF.Exp), waits=[t_w], sig=True)
    dv(lambda e: e.tensor_scalar(out=sm["w1"], in0=sm["w1"], scalar1=1.0, scalar2=None, op0=ALU.add), waits=[t_we])
    dv(lambda e: e.reciprocal(out=sm["w1"], in_=sm["w1"]))
    dv(lambda e: e.tensor_scalar(out=sm["w2"], in0=sm["w1"], scalar1=-1.0, scalar2=1.0, op0=ALU.mult, op1=ALU.add))
    dv(lambda e: e.tensor_tensor(out=A1, in0=bc(gmask.unsqueeze(3), [128, 8, 8, 8]), in1=bc(mask1.unsqueeze(2), [128, 8, 8, 8]), op=ALU.mult))
    dv(lambda e: e.tensor_tensor(out=A2, in0=bc(gmask.unsqueeze(3), [128, 8, 8, 8]), in1=bc(mask2.unsqueeze(2), [128, 8, 8, 8]), op=ALU.mult))
    A1f = A1.rearrange("p t g j -> p t (g j)"); A2f = A2.rearrange("p t g j -> p t (g j)")
    dv(lambda e: e.tensor_tensor(out=tmp64, in0=A1f, in1=A2f, op=ALU.add))
    t_A = dv(lambda e: e.tensor_copy(out=Abf, in_=tmp64), sig=True)
    for tt in range(8):
        bank = 5 + tt % 2
        tpe = P.op("pe", (lambda tt=tt, bank=bank: (lambda e: e.matmul(PS(bank)[:, 0:64], triu_b, Abf[:, tt, :], start=True, stop=(tt == 0))))(),
                   waits=[t_A, ps_free[bank]], sig=(tt == 0))
        for t2_ in range(tt):
            tpe = P.op("pe", (lambda t2_=t2_, tt=tt, bank=bank: (lambda e: e.matmul(PS(bank)[:, 0:64], ones_b, Abf[:, t2_, :], start=False,
                       stop=(t2_ == tt - 1))))(), sig=(t2_ == tt - 1))
        ps_free[bank] = dv((lambda tt=tt, bank=bank: (lambda e: e.tensor_copy(out=rank[:, tt, :], in_=PS(bank)[:, 0:64])))(), waits=[tpe], sig=True)
    for k, (Af, rk, vk, dk, wk) in enumerate(((A1f, "r1", "v1", "d1", "w1"), (A2f, "r2", "v2", "d2", "w2"))):
        dv((lambda Af=Af: (lambda e: e.tensor_tensor(out=tmp64, in0=Af, in1=rank, op=ALU.mult)))())
        dv((lambda rk=rk: (lambda e: e.tensor_reduce(out=sm[rk], in_=tmp64, axis=AX.X, op=ALU.add)))())
        dv((lambda Af=Af: (lambda e: e.tensor_tensor(out=tmp64, in0=Af, in1=bc(e128.unsqueeze(1), [128, 8, 64]), op=ALU.mult)))())
        dv((lambda dk=dk: (lambda e: e.tensor_reduce(out=sm[dk], in_=tmp64, axis=AX.X, op=ALU.add)))())
        dv((lambda dk=dk, rk=rk: (lambda e: e.tensor_tensor(out=sm[dk], in0=sm[dk], in1=sm[rk], op=ALU.add)))())
        dv((lambda vk=vk, rk=rk: (lambda e: e.tensor_scalar(out=sm[vk], in0=sm[rk], scalar1=float(CAP) - 0.5, scalar2=None, op0=ALU.is_lt)))())
        dv((lambda dk=dk, vk=vk: (lambda e: e.tensor_tensor(out=sm[dk], in0=sm[dk], in1=sm[vk], op=ALU.mult)))())
        dv((lambda rk=rk, vk=vk: (lambda e: e.tensor_scalar(out=sm[rk], in0=sm[vk], scalar1=-1.0e6, scalar2=1.0e6, op0=ALU.mult, op1=ALU.add)))())
        dv((lambda dk=dk, rk=rk: (lambda e: e.tensor_tensor(out=sm[dk], in0=sm[dk], in1=sm[rk], op=ALU.add)))())
        dv((lambda dk=dk, k=k: (lambda e: e.tensor_copy(out=dest_i[:, 8 * k:8 * k + 8], in_=sm[dk])))())
        dv((lambda wk=wk, k=k: (lambda e: e.tensor_tensor(out=gates[:, 8 * k:8 * k + 8], in0=sm[wk], in1=sm["pg"], op=ALU.mult)))())
        t_disp = dv((lambda vk=vk, k=k: (lambda e: e.tensor_tensor(out=gates[:, 8 * k:8 * k + 8], in0=gates[:, 8 * k:8 * k + 8], in1=sm[vk],
                    op=ALU.mult)))(), sig=True)
    if debug:
        d_dest = dbg("dest", [128, 16], I32); d_gates = dbg("gates", [128, 16], F32)
        P.dma("sync", lambda e: e.dma_start(out=d_dest, in_=dest_i), "dbg", waits=[t_disp])
        P.dma("sync", lambda e: e.dma_start(out=d_gates, in_=gates), "dbg", waits=[t_disp])
    t_z = P.dma_last["xdz"]
    for k in range(2):
        for tt in range(8):
            t_sc = P.dma("pool", (lambda k=k, tt=tt: (lambda e: e.indirect_dma_start(out=Xd, out_offset=bass.IndirectOffsetOnAxis(
                         ap=dest_i[:, 8 * k + tt:8 * k + tt + 1], axis=0), in_=hnb[:, tt, :], in_offset=None,
                         bounds_check=NE * CAP - 1, oob_is_err=False)))(), "scat", waits=[t_disp, t_z, t_hb])
    P.barrier()
    if stop <= 7:
        return finish()

    wg = [V("wg0", BF16, [16, 512]), V("wg1", BF16, [16, 512])]
    wu = [V("wu_0", BF16, [16, 512]), V("wu_1", BF16, [16, 512])]
    wd = [V("wd0", BF16, [4, D]), V("wd1", BF16, [4, D])]
    Xe = [V("Xe0", BF16, [D]), V("Xe1", BF16, [D])]
    XT = [V("XT0", BF16, [16, 128]), V("XT1", BF16, [16, 128])]
    sgt = [V("sgt0", F32, [512]), V("sgt1", F32, [512])]
    hb = [V("hb0", BF16, [512]), V("hb1", BF16, [512])]
    HT = [V("HT0", BF16, [4, 128]), V("HT1", BF16, [4, 128])]
    Ysb = [V("Ysb0", F32, [D]), V("Ysb1", F32, [D])]
    wdf = [V("wdf0", F32, [2, D]), V("wdf1", F32, [2, D])]
    wgv = w_gate.rearrange("e (kc p) c -> e p kc c", p=128)
    wuv = w_up.rearrange("e (kc p) c -> e p kc c", p=128)
    wdv = w_down.rearrange("e (kc p) c -> e p kc c", p=128)
    Ybuf_v = Ybuf.rearrange("(e p) d -> e p d", p=128)
    gu_done = [None, None]; dn_done = [None, None]; xtr_done = [None, None]; xt_used = [None, None]
    sgt_free = [None, None]; hb_free = [None, None]; ht_free = [None, None]; ysb_free = [None, None]
    bfree = [None] * 8
    ld = {}
    xt_rdy = {}
    mult_done = {}
    ht_rdy = {}

    xe_tok = {}

    def issue_xe(ex):
        b = ex % 2
        xe_tok[ex] = P.dma("sync", (lambda ex=ex, b=b: (lambda e: e.dma_start(out=Xe[b], in_=Xd_v[ex])))(), f"xe{b}", waits=[xtr_done[b]])

    def issue_loads(ex):
        b = ex % 2
        t_xe = xe_tok[ex]
        t_wg = P.dma("pool", (lambda ex=ex, b=b: (lambda e: e.dma_start(out=wg[b], in_=wgv[ex])))(), f"wg{b}", waits=[gu_done[b]])
        t_wu = P.dma("pool", (lambda ex=ex, b=b: (lambda e: e.dma_start(out=wu[b], in_=wuv[ex])))(), f"wu{b}", waits=[gu_done[b]])
        wdf_tok[ex] = [P.dma("sync", (lambda ex=ex, h=h: (lambda e: e.dma_start(out=wdf[h], in_=wdv[ex][:, 2 * h:2 * h + 2, :])))(), f"wdf{h}",
                             waits=[wdf_free[h]]) for h in range(2)]
        ld[ex] = (t_xe, t_wg, t_wu, None)

    wdf_tok = {}
    wdf_free = [None, None]
    wd_rdy = {}

    def cast_wd(ex):
        b = ex % 2
        t0 = P.op("act", (lambda b=b: (lambda e: e.copy(out=wd[b][:, 0:2, :], in_=wdf[0])))(), waits=[wdf_tok[ex][0], dn_done[b]], sig=True)
        t1 = P.op("dve", (lambda b=b: (lambda e: e.tensor_copy(out=wd[b][:, 2:4, :], in_=wdf[1])))(), waits=[wdf_tok[ex][1], dn_done[b]], sig=True)
        wdf_free[0], wdf_free[1] = t0, t1
        wd_rdy[ex] = [t0, t1]

    def stage_A1(ex):
        b = ex % 2
        t_xe = ld[ex][0]
        for half in range(2):
            for r in range(8):
                kc = 8 * half + r
                tpe = P.op("pe", (lambda b=b, kc=kc, half=half, r=r: (lambda e: e.transpose(out=PS(half, BF16)[:, 128 * r:128 * r + 128],
                           in_=Xe[b][:, 128 * kc:128 * kc + 128], identity=ident_b)))(), waits=[t_xe, bfree[half]] if r == 0 else [], sig=(r == 7))
            if half == 0:
                bfree[0] = P.op("act", (lambda b=b: (lambda e: e.copy(out=XT[b][:, 0:8, :], in_=PS(0, BF16).rearrange("p (a c) -> p a c", c=128))))(),
                                waits=[tpe, xt_used[b]], sig=True)
            else:
                bfree[1] = P.op("dve", (lambda b=b: (lambda e: e.tensor_copy(out=XT[b][:, 8:16, :], in_=PS(1, BF16).rearrange("p (a c) -> p a c", c=128))))(),
                                waits=[tpe, xt_used[b]], sig=True)
        xtr_done[b] = tpe
        xt_rdy[ex] = [bfree[0], bfree[1]]

    def stage_A2(ex):
        b = ex % 2
        _, t_wg, t_wu, _ = ld[ex]
        for which, (wt, tw, bank) in enumerate(((wg[b], t_wg, 2), (wu[b], t_wu, 3))):
            for kc in range(16):
                tpe = P.op("pe", (lambda b=b, wt=wt, kc=kc, bank=bank: (lambda e: e.matmul(PS(bank), XT[b][:, kc, :], wt[:, kc, :],
                           start=(kc == 0), stop=(kc == 15))))(), waits=xt_rdy[ex] + [tw, bfree[bank]] if kc == 0 else [], sig=(kc == 15))
            if which == 0:
                t_hg = tpe
        gu_done[b] = tpe
        xt_used[b] = tpe
        bfree[2] = P.op("act", (lambda b=b: (lambda e: e.activation(out=sgt[b], in_=PS(2), func=AF.Silu)))(), waits=[t_hg, sgt_free[b]], sig=True)
        bfree[3] = P.op("dve", (lambda b=b: (lambda e: e.tensor_tensor(out=hb[b], in0=sgt[b], in1=PS(3), op=ALU.mult)))(),
                        waits=[bfree[2], tpe, hb_free[b]], sig=True)
        sgt_free[b] = bfree[3]
        mult_done[ex] = bfree[3]

    def stage_B1(ex):
        b = ex % 2
        for fc in range(4):
            tpe = P.op("pe", (lambda b=b, fc=fc: (lambda e: e.transpose(out=PS(4, BF16)[:, 128 * fc:128 * fc + 128],
                       in_=hb[b][:, 128 * fc:128 * fc + 128], identity=ident_b)))(), waits=[mult_done[ex], bfree[4]] if fc == 0 else [], sig=(fc == 3))
        hb_free[b] = tpe
        bfree[4] = P.op("act", (lambda b=b: (lambda e: e.copy(out=HT[b], in_=PS(4, BF16)[:, 0:512].rearrange("p (a c) -> p a c", c=128))))(),
                        waits=[tpe, ht_free[b]], sig=True)
        ht_rdy[ex] = bfree[4]

    def stage_B2(ex):
        b = ex % 2
        ev = []
        for cb, bank in enumerate((5, 6, 7, 5)):
            for fc in range(4):
                tpe = P.op("pe", (lambda b=b, fc=fc, cb=cb, bank=bank: (lambda e: e.matmul(PS(bank), HT[b][:, fc, :],
                           wd[b][:, fc, 512 * cb:512 * cb + 512], start=(fc == 0), stop=(fc == 3))))(),
                           waits=([ht_rdy[ex], bfree[bank]] + wd_rdy[ex]) if fc == 0 else [], sig=(fc == 3))
            if cb % 2 == 0:
                bfree[bank] = P.op("dve", (lambda b=b, cb=cb, bank=bank: (lambda e: e.tensor_copy(out=Ysb[b][:, 512 * cb:512 * cb + 512],
                                   in_=PS(bank))))(), waits=[tpe, ysb_free[b]], sig=True)
            else:
                bfree[bank] = P.op("act", (lambda b=b, cb=cb, bank=bank: (lambda e: e.copy(out=Ysb[b][:, 512 * cb:512 * cb + 512],
                                   in_=PS(bank))))(), waits=[tpe, ysb_free[b]], sig=True)
            ev.append(bfree[bank])
        dn_done[b] = tpe
        ht_free[b] = tpe
        ysb_free[b] = P.dma("sync", (lambda ex=ex, b=b: (lambda e: e.dma_start(out=Ybuf_v[ex], in_=Ysb[b])))(), f"yout{b}", waits=ev)

    issue_xe(0)
    issue_xe(1)
    issue_loads(0)
    cast_wd(0)
    issue_loads(1)
    stage_A1(0)
    stage_A2(0)
    for ex in range(NE):
        if ex + 2 < NE:
            issue_xe(ex + 2)
        if ex + 1 < NE:
            stage_A1(ex + 1)
        stage_B1(ex)
        if ex + 1 < NE:
            stage_A2(ex + 1)
        stage_B2(ex)
        if ex + 1 < NE:
            cast_wd(ex + 1)
        if ex + 2 < NE:
            issue_loads(ex + 2)
    P.barrier()
    if stop <= 8:
        return finish()

    fgb = V("fgb", F32, [D])
    Y1 = [V("Y1_0", F32, [D]), V("Y1_1", F32, [D])]; Y2 = [V("Y2_0", F32, [D]), V("Y2_1", F32, [D])]
    hz = [V("hz0", F32, [D]), V("hz1", F32, [D])]; ss9 = [V("ss9_0", F32, [4]), V("ss9_1", F32, [4])]
    t_fg = P.dma("sync", lambda e: e.dma_start(out=fgb, in_=fgb_d), "fgb")
    buf_free = [None, None]
    out_v = out.rearrange("(t p) d -> t p d", p=128)
    hbuf_v = hbuf.rearrange("(t p) d -> t p d", p=128)
    t_out = None
    for tt in range(8):
        b = tt % 2
        t_h = P.dma("sync", (lambda tt=tt, b=b: (lambda e: e.dma_start(out=hz[b], in_=hbuf_v[tt])))(), f"hz{b}", waits=[buf_free[b], t_hs])
        t_m1 = P.op("pool", (lambda b=b: (lambda e: e.memset(Y1[b], 0.0)))(), waits=[buf_free[b]])
        t_m2 = P.op("pool", (lambda b=b: (lambda e: e.memset(Y2[b], 0.0)))(), sig=True)
        t_g1 = P.dma("pool", (lambda tt=tt, b=b: (lambda e: e.indirect_dma_start(out=Y1[b], out_offset=None, in_=Ybuf,
                     in_offset=bass.IndirectOffsetOnAxis(ap=dest_i[:, tt:tt + 1], axis=0), bounds_check=NE * CAP - 1, oob_is_err=False)))(),
                     f"ga{b}", waits=[t_m2])
        t_g2_ = P.dma("pool", (lambda tt=tt, b=b: (lambda e: e.indirect_dma_start(out=Y2[b], out_offset=None, in_=Ybuf,
                      in_offset=bass.IndirectOffsetOnAxis(ap=dest_i[:, 8 + tt:9 + tt], axis=0), bounds_check=NE * CAP - 1, oob_is_err=False)))(),
                      f"gb{b}", waits=[t_m2])
        P.op("dve", (lambda b=b: (lambda e: e.memset(ss9[b], 0.0)))(), waits=[buf_free[b]])
        P.op("dve", (lambda tt=tt, b=b: (lambda e: e.scalar_tensor_tensor(out=hz[b], in0=Y1[b], scalar=gates[:, tt:tt + 1], in1=hz[b],
             op0=ALU.mult, op1=ALU.add)))(), waits=[t_h, t_g1])
        t_z9 = P.op("dve", (lambda tt=tt, b=b: (lambda e: e.scalar_tensor_tensor(out=hz[b], in0=Y2[b], scalar=gates[:, 8 + tt:9 + tt], in1=hz[b],
                    op0=ALU.mult, op1=ALU.add)))(), waits=[t_g2_], sig=True)
        t_s9 = P.op("act", (lambda b=b: (lambda e: e.activation(out=Y1[b], in_=hz[b], func=AF.Square, accum_out=ss9[b][:, 0:1])))(),
                    waits=[t_z9], sig=True)
        _t, t_r9 = emit_rstd(ss9[b][:, 3:4], ss9[b][:, 0:1], 1.0 / D, waits=[t_s9])
        t_o = P.op("dve", (lambda b=b: (lambda e: e.scalar_tensor_tensor(out=hz[b], in0=hz[b], scalar=ss9[b][:, 3:4], in1=fgb,
                   op0=ALU.mult, op1=ALU.mult)))(), waits=[t_fg], sig=True)
        t_out = P.dma("sync", (lambda tt=tt, b=b: (lambda e: e.dma_start(out=out_v[tt], in_=hz[b])))(), f"out{b}", waits=[t_o])
        buf_free[b] = t_out
    P.wait_only("sync", [P.dma_last["out0"], P.dma_last["out1"]] + ([P.dma_last["dbg"]] if debug else []))
    P.emit(stack, ps_probe)
    stack.close()
    return nc, list(dbg_out.keys())


_CACHE = {}


def _host_inputs(inputs):
    f32 = np.float32
    x = np.asarray(inputs["x"], f32)
    B = x.shape[0]

    def vecT(v, n):
        return np.ascontiguousarray(np.asarray(v, f32).reshape(n, 128).T)

    com = {
        "ident": np.eye(128, dtype=f32),
        "triu": np.triu(np.ones((128, 128), f32), 1),
        "e128": np.broadcast_to((np.arange(NE, dtype=f32) * CAP)[None, :], (128, NE)).copy(),
        "g1t": vecT(inputs["ln1_g"][0], 16),
        "bglut": vecT(inputs["b_glu"][0], 16),
        "qgt": vecT(inputs["q_norm_g"][0], 6),
        "kvgt": vecT(inputs["kv_norm_g"][0], 4),
        "wdwt": np.ascontiguousarray(np.asarray(inputs["w_dw"][0], f32).T.reshape(8, 128, 31).transpose(1, 0, 2)),
        "bdwt": vecT(inputs["b_dw"][0], 8),
        "lngt": vecT(inputs["conv_ln_g"][0], 8),
        "lnbt": vecT(inputs["conv_ln_b"][0], 8),
        "g2b": np.broadcast_to(np.asarray(inputs["ln2_g"][0], f32)[None, :], (128, D)).copy(),
        "fgb": np.broadcast_to(np.asarray(inputs["final_g"], f32)[None, :], (128, D)).copy(),
        "brb": np.broadcast_to(np.concatenate([np.asarray(inputs["b_group"][0], f32), np.asarray(inputs["b_router"][0], f32)])[None, :],
                               (128, 72)).copy(),
        "w_in": np.asarray(inputs["w_in"][0], f32),
        "w_uq": np.asarray(inputs["w_uq"][0], f32),
        "w_ukv": np.asarray(inputs["w_ukv"][0], f32),
        "w_o": np.asarray(inputs["w_o"][0], f32),
        "w_r": np.ascontiguousarray(np.concatenate([np.asarray(inputs["w_group"][0], f32), np.asarray(inputs["w_router"][0], f32)], axis=1)),
        "w_gate": np.asarray(inputs["w_gate"][0], f32),
        "w_up": np.asarray(inputs["w_up"][0], f32),
        "w_down": np.asarray(inputs["w_down"][0], f32),
    }
    pos = np.arange(S, dtype=f32)
    inv_freq = (f32(10000.0) ** (-np.arange(0, 64, 2, dtype=f32) / f32(64))).astype(f32)
    ang = (pos[:, None] * inv_freq[None, :]).astype(f32)
    cos = np.cos(ang).astype(f32).T
    sin = np.sin(ang).astype(f32).T
    cos64 = np.concatenate([cos, cos], 0)
    sin64 = np.concatenate([-sin, sin], 0)
    in_maps = []
    for c in range(NCORES):
        b, hf = c // 2, c % 2
        own = np.arange(hf * TOWN, (hf + 1) * TOWN)
        oth = np.arange((1 - hf) * TOWN, (2 - hf) * TOWN)
        order = np.concatenate([own, oth])
        xb = x[b]
        xt = np.zeros((D, 2 * TOWN + 32), f32)
        xt[:, 0:2 * TOWN] = xb[order].T
        hm = np.zeros((32,), f32)
        lh = np.arange(own[0] - 16, own[0]); rh = np.arange(own[-1] + 1, own[-1] + 17)
        for i, p in enumerate(np.concatenate([lh, rh])):
            if 0 <= p < S:
                xt[:, 2 * TOWN + i] = xb[p]
                hm[i] = 1.0
        m = dict(com)
        m["xT"] = xt
        m["xown"] = np.ascontiguousarray(xb[own])
        m["cosk"] = np.ascontiguousarray(cos64[:, order])
        m["sink"] = np.ascontiguousarray(sin64[:, order])
        m["halo_mask"] = np.broadcast_to(hm[None, :], (128, 32)).copy()
        in_maps.append(m)
    return in_maps, B


def kernel(**inputs):
    debug = bool(inputs.pop("_debug", False))
    stop = int(inputs.pop("_stop", 99))
    key = ("prog", debug, stop)
    if key not in _CACHE:
        _CACHE[key] = build_program(debug, stop)
    nc, dbg_names = _CACHE[key]
    in_maps, B = _host_inputs(inputs)
    if stop < 8:
        for m in in_maps:
            for k in ("w_gate", "w_up", "w_down"):
                m[k] = m[k][0:1]
    res = run_bass_kernel_spmd(nc, in_maps, core_ids=list(range(NCORES)))
    outp = np.zeros((B, S, D), np.float32)
    for c in range(NCORES):
        b, hf = c // 2, c % 2
        if "out" in res.results[c]:
            outp[b, hf * TOWN:(hf + 1) * TOWN, :] = res.results[c]["out"]
    if debug:
        return outp, [{n: r["dbg_" + n] for n in dbg_names} for r in res.results]
    return outp
```

```python
import contextlib
import numpy as np
import ml_dtypes
import concourse.bass as bass
import concourse.mybir as mybir
from concourse.bass_utils import run_bass_kernel_spmd

F32 = mybir.dt.float32
BF16 = mybir.dt.bfloat16
I32 = mybir.dt.int32
U8 = mybir.dt.uint8
AF = mybir.ActivationFunctionType
ALU = mybir.AluOpType
AX = mybir.AxisListType

NCORES = 8
D = 2048
S = 2048
TOWN = 1024
NEXT = TOWN + 32
NE = 64
CAP = 128
EPS = 1e-6
SCALE = 192.0 ** -0.5
SBUF_BYTES = 206 * 1024

ENGS = ("sync", "act", "pe", "dve", "pool")


class Tok:
    __slots__ = ("sem", "val", "rec", "eng")

    def __init__(self, sem=None, val=None, rec=None, eng=None):
        self.sem, self.val, self.rec, self.eng = sem, val, rec, eng


class Rec:
    __slots__ = ("eng", "fn", "waits", "sig", "kind", "key", "tok", "seq")
    _seq = 0

    def __init__(self, eng, fn, waits, sig, kind, key=None):
        self.eng, self.fn, self.waits, self.sig, self.kind, self.key = eng, fn, list(waits), sig, kind, key
        self.tok = Tok(rec=self, eng=eng)
        Rec._seq += 1
        self.seq = Rec._seq


def _ap_range(v):
    esz = {F32: 4, BF16: 2, I32: 4, U8: 1}.get(v.dtype, 4)
    dims = v.ap
    pitch = dims[0][0]
    off = v.offset % pitch if pitch > 0 else v.offset
    ext = 1
    for st, sz in dims[1:]:
        ext += abs(st) * (sz - 1)
    return (v.tensor.name, off * esz, (off + ext) * esz)


class _Dummy:
    hit = False
    reads = []
    writes = []

    def __getattr__(self, name):
        def f(*a, **k):
            for key, v in [(None, x) for x in a] + list(k.items()):
                if not hasattr(v, "space") or not hasattr(v, "ap"):
                    continue
                if str(v.space) == "PSUM":
                    _Dummy.hit = True
                r = _ap_range(v)
                if key is None:
                    _Dummy.reads.append(r)
                    _Dummy.writes.append(r)
                elif key in ("out", "accum_out"):
                    _Dummy.writes.append(r)
                else:
                    _Dummy.reads.append(r)
            return _Dummy()
        return f


def _overlap(xs, ys):
    for (t0, a0, b0) in xs:
        for (t1, a1, b1) in ys:
            if t0 == t1 and a0 < b1 and a1 < b0:
                return True
    return False


FENCED = ("act", "dve", "pool")


class Prog:
    def __init__(self, nc):
        self.nc = nc
        self.q = {e: [] for e in ENGS}
        self.dma_last = {}
        self.dma_eng = {}

    def op(self, eng, fn, waits=(), sig=False):
        r = Rec(eng, fn, [w for w in waits if w is not None], sig, "op")
        self.q[eng].append(r)
        return r.tok

    def dma(self, eng, fn, key, waits=()):
        assert self.dma_eng.setdefault(key, eng) == eng, key
        r = Rec(eng, fn, [w for w in waits if w is not None], True, "dma", key)
        self.q[eng].append(r)
        self.dma_last[key] = r.tok
        return r.tok

    def wait_only(self, eng, waits):
        r = Rec(eng, None, [w for w in waits if w is not None], False, "wait")
        self.q[eng].append(r)

    def last_tok(self, eng):
        for r in reversed(self.q[eng]):
            if r.kind == "op":
                r.sig = True
                return r.tok
        return None

    def barrier(self):
        toks = [self.last_tok(e) for e in ("act", "pe", "dve", "pool")]
        toks += list(self.dma_last.values())
        for e in ENGS:
            self.wait_only(e, toks)

    def emit(self, stack, ps_probe=None):
        nc = self.nc
        sems = {}
        if ps_probe is not None:
            recs = sorted([r for e in ("act", "dve") for r in self.q[e] if r.kind == "op"], key=lambda r: r.seq)
            last = {"act": None, "dve": None}
            for r in recs:
                if ps_probe(r.fn):
                    other = "dve" if r.eng == "act" else "act"
                    if last[other] is not None:
                        last[other].rec.sig = True
                        r.waits.append(last[other])
                    last[r.eng] = r.tok
        self.fence = {}
        for e in FENCED:
            hist = []
            for r in self.q[e]:
                if r.kind != "op":
                    continue
                _Dummy.reads, _Dummy.writes = [], []
                r.fn(_Dummy())
                rd, wr = _Dummy.reads, _Dummy.writes
                if hist:
                    prev = hist[-1][0]
                    prev.sig = True
                    self.fence[id(r)] = prev
                hist.append((r, wr))

        def getsem(name):
            if name not in sems:
                sems[name] = stack.enter_context(nc.semaphore(name))
            return sems[name]

        for e in ENGS:
            cnt = 0
            dcnt = {}
            for r in self.q[e]:
                if r.kind == "op" and r.sig:
                    cnt += 1
                    r.tok.sem, r.tok.val = "c_" + e, cnt
                elif r.kind == "dma":
                    dcnt[r.key] = dcnt.get(r.key, 0) + 16
                    r.tok.sem, r.tok.val = "d_" + r.key, dcnt[r.key]
        block = stack.enter_context(nc.Block())
        q = self.q

        def run(eng_name, e):
            waited = {}
            prev_cnt = 0
            for r in q[eng_name]:
                for w in r.waits:
                    if w.rec.kind == "op" and w.eng == eng_name and r.kind == "op":
                        continue
                    assert w.sem is not None, "wait on unsignalled op"
                    if waited.get(w.sem, 0) >= w.val:
                        continue
                    waited[w.sem] = w.val
                    e.wait_ge(getsem(w.sem), w.val)
                if r.fn is None:
                    continue
                if r.kind == "op" and id(r) in self.fence:
                    pv = self.fence[id(r)].tok.val
                    if waited.get("c_" + eng_name, 0) < pv:
                        waited["c_" + eng_name] = pv
                        e.wait_ge(getsem("c_" + eng_name), pv)
                ins = r.fn(e)
                if r.kind == "dma":
                    ins.then_inc(getsem(r.tok.sem), 16)
                elif r.sig:
                    ins.then_inc(getsem(r.tok.sem), 1)
                    prev_cnt = r.tok.val

        @block.sync
        def _(e):
            run("sync", e)

        @block.scalar
        def _(e):
            run("act", e)

        @block.tensor
        def _(e):
            run("pe", e)

        @block.vector
        def _(e):
            run("dve", e)

        @block.gpsimd
        def _(e):
            run("pool", e)


class Arena:
    def __init__(self):
        self.items = []

    def add(self, name, nbytes, p0, p1):
        self.items.append((name, (nbytes + 63) // 64 * 64, p0, p1))

    def solve(self, limit):
        placed = {}
        for name, nb, p0, p1 in sorted(self.items, key=lambda t: -t[1]):
            conflicts = sorted((o, o + b) for (o, b, q0, q1) in placed.values() if not (q1 < p0 or p1 < q0))
            off = 0
            for a, b in conflicts:
                if off + nb <= a:
                    break
                off = max(off, b)
            assert off + nb <= limit, f"SBUF overflow placing {name}: {off + nb} > {limit}"
            placed[name] = (off, nb, p0, p1)
        return {k: v[0] for k, v in placed.items()}


def build_program(debug=False, stop=99):
    nc = bass.Bass("TRN2", target_bir_lowering=False)
    dbg_out = {}

    def din(name, shape, dt=F32):
        return nc.dram_tensor(name, list(shape), dt, kind="ExternalInput").ap()

    xT = din("xT", [D, 2 * TOWN + 32])
    xown = din("xown", [TOWN, D])
    cosk = din("cosk", [64, S])
    sink = din("sink", [64, S])
    halo_mask = din("halo_mask", [128, 32])
    ident_d = din("ident", [128, 128])
    triu_d = din("triu", [128, 128])
    e128_d = din("e128", [128, NE])
    g1_d = din("g1t", [128, 16])
    bglu_d = din("bglut", [128, 16])
    qg_d = din("qgt", [128, 6])
    kvg_d = din("kvgt", [128, 4])
    wdw_d = din("wdwt", [128, 8, 31])
    bdw_d = din("bdwt", [128, 8])
    lng_d = din("lngt", [128, 8])
    lnb_d = din("lnbt", [128, 8])
    g2b_d = din("g2b", [128, D])
    fgb_d = din("fgb", [128, D])
    brb_d = din("brb", [128, 72])
    w_in = din("w_in", [D, 3392])
    w_uq = din("w_uq", [768, 1536])
    w_ukv = din("w_ukv", [512, 2048])
    w_o = din("w_o", [D, D])
    w_r = din("w_r", [D, 72])
    NEW = NE if stop >= 8 else 1
    w_gate = din("w_gate", [NEW, D, 512])
    w_up = din("w_up", [NEW, D, 512])
    w_down = din("w_down", [NEW, 512, D])
    out = nc.dram_tensor("out", [TOWN, D], F32, kind="ExternalOutput").ap()
    Xd = nc.dram_tensor("Xd", [NE * CAP, D], BF16).ap()
    Ybuf = nc.dram_tensor("Ybuf", [NE * CAP, D], F32).ap()
    hbuf = nc.dram_tensor("hbuf", [TOWN, D], F32).ap()

    def dbg(name, shape, dt):
        if debug:
            dbg_out[name] = nc.dram_tensor("dbg_" + name, list(shape), dt, kind="ExternalOutput").ap()
            return dbg_out[name]
        return None

    ar = Arena()
    A = ar.add
    A("ident_f", 512, 0, 9); A("ident_b", 256, 0, 9); A("ones_b", 256, 0, 9); A("ones_f", 512, 0, 9)
    A("triu_b", 256, 0, 9); A("triu_f", 512, 0, 0); A("e128", 256, 0, 9)
    A("g1t", 64, 0, 9); A("bglut", 64, 0, 9); A("qgt", 24, 0, 9); A("kvgt", 16, 0, 9)
    A("wdwt", 8 * 31 * 4, 0, 9); A("bdwt", 32, 0, 9); A("lngt", 32, 0, 9); A("lnbt", 32, 0, 9)
    A("hmask", 128, 0, 9); A("brb", 288, 0, 9); A("zero_b", 4096, 0, 9)
    A("cosk", S * 4, 0, 1); A("sink", S * 4, 0, 1); A("cosq", TOWN * 4, 0, 3); A("sinq", TOWN * 4, 0, 3)
    A("xg_own", 16 * NEXT * 2, 1, 2); A("xoth", 16 * 512 * 2, 1, 1); A("xoth1", 16 * 512 * 2, 1, 1); A("sq", 16 * 512 * 2, 1, 1)
    A("rstd_all", (2 * TOWN + 32) * 4, 1, 2); A("wkv", 16 * 640 * 2, 1, 1)
    A("kvlat", 4 * 512 * 4, 1, 1); A("sq2", 4 * 512 * 2, 1, 1); A("kro", 512 * 4, 1, 1); A("krs", 512 * 4, 1, 1)
    A("rstd2", 512 * 4, 1, 1); A("tmpa", 512 * 4, 1, 1)
    A("kvn", 4 * S * 2, 1, 4); A("kr", S * 2, 1, 5)
    A("wq0", 16 * 384 * 2, 2, 2); A("wq1", 16 * 384 * 2, 2, 2); A("wu0", 16 * 256 * 2, 2, 2); A("wu1", 16 * 256 * 2, 2, 2)
    A("qlat", 6 * 512 * 4, 2, 2); A("sqq", 6 * 512 * 2, 2, 2); A("t1", 512 * 4, 2, 2); A("t2", 512 * 4, 2, 2)
    A("sg", 512 * 4, 2, 2); A("rstdq", 512 * 4, 2, 2)
    A("qn", 6 * TOWN * 2, 2, 3); A("c_ext", 8 * NEXT * 2, 2, 3)
    A("AT", 16 * TOWN * 2, 3, 6)
    A("sum1", TOWN * 4, 3, 3); A("sum2", TOWN * 4, 3, 3)
    A("convf0", 512 * 4, 3, 3); A("convf1", 512 * 4, 3, 3); A("convsq0", 512 * 4, 3, 3); A("convsq1", 512 * 4, 3, 3)
    A("diag0", 31 * 128 * 2, 3, 3); A("diag1", 31 * 128 * 2, 3, 3)
    A("lnmean", TOWN * 4, 3, 3); A("lnrstd", TOWN * 4, 3, 3)
    A("wuq", 6 * 1536 * 2, 3, 3); A("wqs", 6 * 512 * 2, 3, 3); A("QN", 8 * TOWN * 2, 3, 5); A("QR", 8 * TOWN * 2, 3, 5)
    A("rt1", 512 * 4, 3, 3); A("rt2", 512 * 4, 3, 3)
    A("wukv", 4 * 2048 * 2, 4, 4); A("KT", 8 * S * 2, 4, 5); A("Vaug", 16 * 8 * 129 * 2 + 64, 4, 5)
    A("PT0", 16 * 512 * 2, 5, 5); A("PT1", 16 * 512 * 2, 5, 5); A("rcp", 64, 5, 5)
    A("Otok0", 256, 5, 5); A("Otok1", 256, 5, 5)
    A("wo0", 16 * 512 * 2, 6, 6); A("wo1", 16 * 512 * 2, 6, 6); A("h", 8 * D * 4, 6, 7)
    A("g2b", D * 4, 7, 7); A("hnf", D * 4, 7, 7); A("hnb", 8 * D * 2, 7, 7); A("hnT", 16 * 128 * 4, 7, 7)
    A("hnf1", D * 4, 7, 7); A("hnT1", 16 * 128 * 4, 7, 7)
    A("wr", 16 * 72 * 4, 7, 7); A("ssq7", 64, 7, 7); A("L", 8 * 72 * 4, 7, 7)
    for nm in ("gmax", "gsum", "pg", "m1", "m2", "w1", "w2", "r1", "r2", "v1", "v2", "d1", "d2"):
        A(nm, 32, 7, 7)
    for nm in ("gd", "gmask"):
        A(nm, 8 * 8 * 4, 7, 7)
    for nm in ("sel", "sel2", "mask1", "mask2"):
        A(nm, 8 * 8 * 4, 7, 7)
    for nm in ("rl4", "A1", "A2", "rank", "tmp64"):
        A(nm, 8 * 64 * 4, 7, 7)
    A("Abf", 8 * 64 * 2, 7, 7)
    A("dest_i", 16 * 4, 7, 9); A("gates", 16 * 4, 7, 9)
    for i in range(2):
        A(f"wg{i}", 16 * 512 * 2, 8, 8); A(f"wu_{i}", 16 * 512 * 2, 8, 8); A(f"wd{i}", 4 * D * 2, 8, 8)
        A(f"Xe{i}", D * 2, 8, 8); A(f"XT{i}", 16 * 128 * 2, 8, 8); A(f"sgt{i}", 512 * 4, 8, 8)
        A(f"hb{i}", 512 * 2, 8, 8); A(f"HT{i}", 4 * 128 * 2, 8, 8); A(f"Ysb{i}", D * 4, 8, 8)
        A(f"wdf{i}", 2 * D * 4, 8, 8)
    A("fgb", D * 4, 9, 9)
    for i in range(2):
        A(f"Y1_{i}", D * 4, 9, 9); A(f"Y2_{i}", D * 4, 9, 9); A(f"hz{i}", D * 4, 9, 9); A(f"ss9_{i}", 64, 9, 9)
    offs = ar.solve(SBUF_BYTES)
    sizes = {n: b for (n, b, _, _) in ar.items}

    stack = contextlib.ExitStack()
    arena = stack.enter_context(nc.sbuf_tensor("arena", [128, SBUF_BYTES], U8))
    banks = [stack.enter_context(nc.psum_tensor(f"ps{i}", [128, 512], F32)) for i in range(8)]

    def V(name, dt, shape=None, parts=128):
        nb = sizes[name]
        esz = {F32: 4, BF16: 2, I32: 4}[dt]
        ap = arena[0:parts, offs[name]:offs[name] + nb].bitcast(dt)
        if shape is None:
            return ap
        n = int(np.prod(shape))
        ap = ap[:, 0:n]
        if len(shape) == 1:
            return ap
        if len(shape) == 2:
            return ap.rearrange("p (a b) -> p a b", b=shape[1])
        if len(shape) == 3:
            return ap.rearrange("p (a b c) -> p a b c", b=shape[1], c=shape[2])
        raise ValueError

    ps_flag = [False]

    def ps_probe(fn):
        _Dummy.hit = False
        fn(_Dummy())
        return _Dummy.hit

    def PS(i, dt=F32, parts=128):
        ps_flag[0] = True
        ap = banks[i][0:parts, :]
        return ap if dt == F32 else ap.bitcast(dt)

    def emit_rstd(dst, src, inv_n, waits=()):
        t_a = P.op("dve", lambda e: e.tensor_scalar(out=dst, in0=src, scalar1=inv_n, scalar2=EPS, op0=ALU.mult, op1=ALU.add),
                   waits=list(waits), sig=True)
        t_b = P.op("act", lambda e: e.activation(out=dst, in_=dst, func=AF.Sqrt), waits=[t_a], sig=True)
        t_c = P.op("dve", lambda e: e.reciprocal(out=dst, in_=dst), waits=[t_b], sig=True)
        return t_a, t_c

    P = Prog(nc)

    def finish():
        P.barrier()
        fin = [P.dma_last[k] for k in ("out0", "out1", "dbg") if k in P.dma_last]
        P.wait_only("sync", fin)
        P.emit(stack, ps_probe)
        stack.close()
        return nc, list(dbg_out.keys())

    def dump(name, ap, shape, dt):
        if debug:
            d = dbg(name, shape, dt)
            P.dma("sync", lambda e: e.dma_start(out=d, in_=ap), "dbg")

    ident_f = V("ident_f", F32, [128]); ident_b = V("ident_b", BF16, [128])
    ones_b = V("ones_b", BF16, [128]); ones_f = V("ones_f", F32, [128])
    triu_b = V("triu_b", BF16, [128]); triu_f = V("triu_f", F32, [128]); e128 = V("e128", F32, [NE])
    g1t = V("g1t", F32, [16]); bglut = V("bglut", F32, [16]); qgt = V("qgt", F32, [6]); kvgt = V("kvgt", F32, [4])
    wdwt = V("wdwt", F32, [8, 31]); bdwt = V("bdwt", F32, [8]); lngt = V("lngt", F32, [8]); lnbt = V("lnbt", F32, [8])
    hmask = V("hmask", F32, [32]); brb = V("brb", F32, [72]); zero_b = V("zero_b", BF16, [2048])
    cosk_t = V("cosk", F32, [S], parts=64); sink_t = V("sink", F32, [S], parts=64)
    cosq_t = V("cosq", F32, [TOWN], parts=64); sinq_t = V("sinq", F32, [TOWN], parts=64)

    cl = []
    for dst, src in ((ident_f, ident_d), (triu_f, triu_d), (e128, e128_d), (g1t, g1_d), (bglut, bglu_d), (qgt, qg_d),
                     (kvgt, kvg_d), (wdwt, wdw_d), (bdwt, bdw_d), (lngt, lng_d), (lnbt, lnb_d), (hmask, halo_mask),
                     (brb, brb_d), (cosk_t, cosk), (sink_t, sink), (cosq_t, cosk[:, 0:TOWN]), (sinq_t, sink[:, 0:TOWN])):
        cl.append(P.dma("sync", (lambda d, s: (lambda e: e.dma_start(out=d, in_=s)))(dst, src), "const"))
    tc0 = cl[-1]
    P.op("dve", lambda e: e.tensor_copy(out=ident_b, in_=ident_f), waits=[tc0])
    P.op("dve", lambda e: e.tensor_copy(out=triu_b, in_=triu_f))
    P.op("dve", lambda e: e.memset(ones_b, 1.0))
    P.op("dve", lambda e: e.memset(ones_f, 1.0))
    P.op("dve", lambda e: e.memset(zero_b, 0.0))
    P.barrier()
    if stop <= 0:
        return finish()

    xg_own = V("xg_own", BF16, [16, NEXT]); xoth = V("xoth", BF16, [16, 512]); sq = V("sq", BF16, [16, 512])
    rstd_all = V("rstd_all", F32, [2 * TOWN + 32]); wkv = V("wkv", BF16, [16, 640])
    kvlat = V("kvlat", F32, [4, 512]); sq2 = V("sq2", BF16, [4, 512])
    kro = V("kro", F32, [512], parts=64); krs = V("krs", F32, [512], parts=64)
    rstd2 = V("rstd2", F32, [512]); tmpa = V("tmpa", F32, [512])
    kvn = V("kvn", BF16, [4, S]); kr = V("kr", BF16, [S], parts=64)
    xTv = xT.rearrange("(kc p) t -> p kc t", p=128)
    w_in_v = w_in.rearrange("(kc p) c -> p kc c", p=128)

    t_wkv = P.dma("pool", lambda e: e.dma_start(out=wkv[:, :, 0:576], in_=w_in_v[:, :, 768:1344]), "wkv")
    t_sw = P.op("act", lambda e: e.copy(out=wkv[:, :, 576:608], in_=wkv[:, :, 544:576]), waits=[t_wkv])
    t_sw = P.op("act", lambda e: e.copy(out=wkv[:, :, 608:640], in_=wkv[:, :, 512:544]), sig=True)

    xoth1 = V("xoth1", BF16, [16, 512])
    blocks = [(0, 512, xg_own[:, :, 0:512], True), (512, 512, xg_own[:, :, 512:1024], True),
              (1024, 512, xoth, True), (1536, 512, xoth1, True), (2048, 32, xg_own[:, :, 1024:1056], False)]
    ps_free = [None] * 8
    st1 = {"sq_free": None}
    blk = {}

    def stage_Xa(bi):
        c0, nb, xb, is_kv = blocks[bi]
        tx = P.dma("pool", (lambda xb=xb, c0=c0, nb=nb: (lambda e: e.dma_start(out=xb, in_=xTv[:, :, c0:c0 + nb])))(), f"xblk{bi}")
        sqv = sq[:, :, 0:nb]
        tsq = P.op("act", (lambda xb=xb, sqv=sqv: (lambda e: e.activation(out=sqv, in_=xb, func=AF.Square)))(),
                   waits=[tx, st1["sq_free"]], sig=True)
        for kc in range(16):
            t_xg = P.op("dve", (lambda xb=xb, kc=kc: (lambda e: e.tensor_scalar(out=xb[:, kc, :], in0=xb[:, kc, :],
                        scalar1=g1t[:, kc:kc + 1], scalar2=None, op0=ALU.mult)))(), waits=[tsq] if kc == 0 else [], sig=(kc == 15))
        blk[bi] = {"tsq": tsq, "t_xg": t_xg, "sqv": sqv}

    def stage_Xb(bi):
        c0, nb, xb, is_kv = blocks[bi]
        sqv = blk[bi]["sqv"]
        for kc in range(16):
            tpe = P.op("pe", (lambda kc=kc, sqv=sqv, nb=nb: (lambda e: e.matmul(PS(0)[:, 0:nb], ones_b, sqv[:, kc, :],
                       start=(kc == 0), stop=(kc == 15))))(), waits=[blk[bi]["tsq"], ps_free[0]] if kc == 0 else [], sig=(kc == 15))
        st1["sq_free"] = tpe
        rs = rstd_all[:, c0:c0 + nb]
        ps_free[0], t_rs = emit_rstd(rs, PS(0)[:, 0:nb], 1.0 / D, waits=[tpe])
        blk[bi]["rs"] = rs
        blk[bi]["t_rs"] = t_rs

    def stage_K(bi):
        c0, nb, xb, is_kv = blocks[bi]
        rs, t_rs, t_xg = blk[bi]["rs"], blk[bi]["t_rs"], blk[bi]["t_xg"]
        for m in range(4):
            for kc in range(16):
                tpe = P.op("pe", (lambda m=m, kc=kc, xb=xb: (lambda e: e.matmul(PS(1 + m), wkv[:, kc, 128 * m:128 * m + 128],
                           xb[:, kc, :], start=(kc == 0), stop=(kc == 15))))(),
                           waits=[t_xg, t_sw, ps_free[1 + m]] if kc == 0 else [], sig=(kc == 15))
            ps_free[1 + m] = P.op("dve", (lambda m=m, rs=rs: (lambda e: e.tensor_tensor(out=kvlat[:, m, :], in0=PS(1 + m), in1=rs,
                                  op=ALU.mult)))(), waits=[tpe, t_rs], sig=True)
        t_kvl = ps_free[4]
        for j, (cc, dst) in enumerate(((512, kro), (576, krs))):
            for kc in range(16):
                tpe = P.op("pe", (lambda j=j, kc=kc, cc=cc, xb=xb: (lambda e: e.matmul(PS(5 + j, parts=64), wkv[:, kc, cc:cc + 64],
                           xb[:, kc, :], start=(kc == 0), stop=(kc == 15))))(),
                           waits=[ps_free[5 + j]] if kc == 0 else [], sig=(kc == 15))
            ps_free[5 + j] = P.op("dve", (lambda j=j, dst=dst, rs=rs: (lambda e: e.tensor_tensor(out=dst, in0=PS(5 + j, parts=64),
                                  in1=rs[0:64, :], op=ALU.mult)))(), waits=[tpe], sig=True)
        P.op("dve", (lambda c0=c0: (lambda e: e.tensor_tensor(out=kro, in0=kro, in1=cosk_t[:, c0:c0 + 512], op=ALU.mult)))())
        P.op("dve", (lambda c0=c0: (lambda e: e.tensor_tensor(out=krs, in0=krs, in1=sink_t[:, c0:c0 + 512], op=ALU.mult)))())
        P.op("dve", (lambda c0=c0: (lambda e: e.tensor_tensor(out=kr[:, c0:c0 + 512], in0=kro, in1=krs, op=ALU.add)))())
        tsq2 = P.op("act", lambda e: e.activation(out=sq2, in_=kvlat, func=AF.Square), waits=[t_kvl], sig=True)
        for m in range(4):
            tpe = P.op("pe", (lambda m=m: (lambda e: e.matmul(PS(7), ones_b, sq2[:, m, :], start=(m == 0), stop=(m == 3))))(),
                       waits=[tsq2, ps_free[7]] if m == 0 else [], sig=(m == 3))
        ps_free[7], _t = emit_rstd(rstd2, PS(7), 1.0 / 512, waits=[tpe])
        for m in range(4):
            P.op("dve", (lambda m=m, c0=c0: (lambda e: e.scalar_tensor_tensor(out=kvn[:, m, c0:c0 + 512], in0=kvlat[:, m, :],
                 scalar=kvgt[:, m:m + 1], in1=rstd2, op0=ALU.mult, op1=ALU.mult)))())

    stage_Xa(0)
    stage_Xb(0)
    for bi in range(5):
        if bi + 1 < 5:
            stage_Xa(bi + 1)
        if blocks[bi][3]:
            stage_K(bi)
        if bi + 1 < 5:
            stage_Xb(bi + 1)
    P.barrier()
    dump("kvn", kvn, [128, 4, S], BF16)
    dump("kr", kr, [64, S], BF16)
    dump("rstd_all", rstd_all, [128, 2 * TOWN + 32], F32)
    dump("xg_own", xg_own, [128, 16, NEXT], BF16)
    if stop <= 1:
        return finish()

    wqb = [V("wq0", BF16, [16, 384]), V("wq1", BF16, [16, 384])]
    wub = [V("wu0", BF16, [16, 256]), V("wu1", BF16, [16, 256])]
    qlat = V("qlat", F32, [6, 512]); sqq = V("sqq", BF16, [6, 512])
    t1 = V("t1", F32, [512]); t2 = V("t2", F32, [512]); sgv = V("sg", F32, [512]); rstdq = V("rstdq", F32, [512])
    qn = V("qn", BF16, [6, TOWN]); c_ext = V("c_ext", BF16, [8, NEXT])
    ps_free = [None] * 8
    t_wq = [P.dma("pool", (lambda i=i: (lambda e: e.dma_start(out=wqb[i], in_=w_in_v[:, :, 384 * i:384 * i + 384])))(), f"wq{i}")
            for i in range(2)]
    t_qn_done = None
    for blk in range(2):
        tk = slice(512 * blk, 512 * blk + 512)
        for m in range(6):
            bank = m % 4
            for kc in range(16):
                tpe = P.op("pe", (lambda m=m, kc=kc, bank=bank, tk=tk: (lambda e: e.matmul(PS(bank), wqb[m // 3][:, kc, 128 * (m % 3):128 * (m % 3) + 128],
                           xg_own[:, kc, tk], start=(kc == 0), stop=(kc == 15))))(),
                           waits=[t_wq[m // 3], ps_free[bank]] if kc == 0 else [], sig=(kc == 15))
            ps_free[bank] = P.op("dve", (lambda m=m, bank=bank, tk=tk: (lambda e: e.tensor_tensor(out=qlat[:, m, :], in0=PS(bank),
                                 in1=rstd_all[:, tk], op=ALU.mult)))(), waits=[tpe, t_qn_done], sig=True)
        tsq = P.op("act", lambda e: e.activation(out=sqq, in_=qlat, func=AF.Square), waits=[ps_free[1]], sig=True)
        for m in range(6):
            tpe = P.op("pe", (lambda m=m: (lambda e: e.matmul(PS(4), ones_b, sqq[:, m, :], start=(m == 0), stop=(m == 5))))(),
                       waits=[tsq, ps_free[4]] if m == 0 else [], sig=(m == 5))
        ps_free[4], _t = emit_rstd(rstdq, PS(4), 1.0 / 768, waits=[tpe])
        for m in range(6):
            t_qn_done = P.op("dve", (lambda m=m, tk=tk: (lambda e: e.scalar_tensor_tensor(out=qn[:, m, tk], in0=qlat[:, m, :],
                             scalar=qgt[:, m:m + 1], in1=rstdq, op0=ALU.mult, op1=ALU.mult)))(), sig=(m == 5))
    wu_free = [None, None]
    ublocks = [(0, 512, 16), (512, 512, 16 + 512), (1024, 16, 0), (1040, 16, 16 + 1024)]
    for j in range(8):
        wb = wub[j % 2]
        ta = P.dma("pool", (lambda wb=wb, j=j: (lambda e: e.dma_start(out=wb[:, :, 0:128], in_=w_in_v[:, :, 1344 + 128 * j:1344 + 128 * j + 128])))(),
                   f"wua{j % 2}", waits=[wu_free[j % 2]])
        tg = P.dma("pool", (lambda wb=wb, j=j: (lambda e: e.dma_start(out=wb[:, :, 128:256], in_=w_in_v[:, :, 2368 + 128 * j:2368 + 128 * j + 128])))(),
                   f"wug{j % 2}", waits=[wu_free[j % 2]])
        for ui, (xc, n, cc) in enumerate(ublocks):
            ba, bg = (5, 6) if ui % 2 == 0 else (7, 0)
            for half, bank in ((0, ba), (1, bg)):
                for kc in range(16):
                    tpe = P.op("pe", (lambda wb=wb, half=half, bank=bank, kc=kc, xc=xc, n=n: (lambda e: e.matmul(PS(bank)[:, 0:n],
                               wb[:, kc, 128 * half:128 * half + 128], xg_own[:, kc, xc:xc + n], start=(kc == 0), stop=(kc == 15))))(),
                               waits=[ta, tg, ps_free[bank]] if kc == 0 else [], sig=(kc == 15))
                if half == 0:
                    tpa = tpe
            rsl = rstd_all[:, xc:xc + n] if xc < 1024 else rstd_all[:, 2048 + (xc - 1024):2048 + (xc - 1024) + n]
            ps_free[ba] = P.op("dve", (lambda ba=ba, n=n, rsl=rsl: (lambda e: e.tensor_tensor(out=t1[:, 0:n], in0=PS(ba)[:, 0:n], in1=rsl,
                               op=ALU.mult)))(), waits=[tpa], sig=True)
            ps_free[bg] = P.op("dve", (lambda bg=bg, n=n, rsl=rsl: (lambda e: e.tensor_tensor(out=t2[:, 0:n], in0=PS(bg)[:, 0:n], in1=rsl,
                               op=ALU.mult)))(), waits=[tpe], sig=True)
            tsg = P.op("act", (lambda n=n, j=j: (lambda e: e.activation(out=sgv[:, 0:n], in_=t2[:, 0:n], func=AF.Sigmoid,
                       bias=bglut[:, 8 + j:9 + j], scale=1.0)))(), waits=[ps_free[bg]], sig=True)
            P.op("dve", (lambda n=n, j=j, cc=cc: (lambda e: e.scalar_tensor_tensor(out=c_ext[:, j, cc:cc + n], in0=t1[:, 0:n],
                 scalar=bglut[:, j:j + 1], in1=sgv[:, 0:n], op0=ALU.add, op1=ALU.mult)))(), waits=[tsg])
            if ui >= 2:
                hm = hmask[:, 0:16] if ui == 2 else hmask[:, 16:32]
                P.op("dve", (lambda n=n, j=j, cc=cc, hm=hm: (lambda e: e.tensor_tensor(out=c_ext[:, j, cc:cc + n],
                     in0=c_ext[:, j, cc:cc + n], in1=hm, op=ALU.mult)))())
        wu_free[j % 2] = tpe
    P.barrier()
    dump("qn", qn, [128, 6, TOWN], BF16)
    dump("c_ext", c_ext, [128, 8, NEXT], BF16)
    if stop <= 2:
        return finish()

    AT = V("AT", BF16, [16, TOWN])
    sum1 = V("sum1", F32, [TOWN]); sum2 = V("sum2", F32, [TOWN])
    convf = [V("convf0", F32, [512]), V("convf1", F32, [512])]
    convsq = [V("convsq0", F32, [512]), V("convsq1", F32, [512])]
    diag = [V("diag0", BF16, [31, 128]), V("diag1", BF16, [31, 128])]
    lnmean = V("lnmean", F32, [TOWN]); lnrstd = V("lnrstd", F32, [TOWN])
    wuq = V("wuq", BF16, [6, 1536]); wqs = V("wqs", BF16, [6, 512])
    QN = V("QN", BF16, [8, TOWN]); QR = V("QR", BF16, [8, TOWN], parts=64)
    rt1 = V("rt1", F32, [512], parts=64); rt2 = V("rt2", F32, [512], parts=64)
    w_uq_v = w_uq.rearrange("(kc p) c -> p kc c", p=128)
    KDC = ""
    if "w" in KDC:
        t_wuq = None; t_wqs = None
    else:
        t_wuq = P.dma("pool", lambda e: e.dma_start(out=wuq, in_=w_uq_v), "wuq")
    wuq4 = wuq.rearrange("p k (h c) -> p k h c", c=192)
    wqs4 = wqs.rearrange("p k (h c) -> p k h c", c=64)
    for kc in range(6):
        if "w" in KDC:
            break
        P.op("act", (lambda kc=kc: (lambda e: e.copy(out=wqs4[:, kc, :, 0:32], in_=wuq4[:, kc, :, 160:192])))(), waits=[t_wuq])
        t_wqs = P.op("act", (lambda kc=kc: (lambda e: e.copy(out=wqs4[:, kc, :, 32:64], in_=wuq4[:, kc, :, 128:160])))(), sig=True)
    qp_free = [None] * 4

    def emit_qproj(u):
        h, blk = u // 2, u % 2
        tk = slice(512 * blk, 512 * blk + 512)
        bank = u % 2
        for kc in range(6):
            tpe = P.op("pe", (lambda h=h, kc=kc, tk=tk, bank=bank: (lambda e: e.matmul(PS(bank), wuq[:, kc, 192 * h:192 * h + 128],
                       qn[:, kc, tk], start=(kc == 0), stop=(kc == 5))))(), waits=[t_wuq, qp_free[bank]] if kc == 0 else [], sig=(kc == 5))
        qp_free[bank] = P.op("act", (lambda h=h, tk=tk, bank=bank: (lambda e: e.copy(out=QN[:, h, tk], in_=PS(bank))))(),
                             waits=[tpe], sig=True)
        for kc in range(6):
            tpr = P.op("pe", (lambda h=h, kc=kc, tk=tk: (lambda e: e.matmul(PS(2, parts=64), wuq[:, kc, 192 * h + 128:192 * h + 192],
                       qn[:, kc, tk], start=(kc == 0), stop=(kc == 5))))(), waits=[qp_free[2]] if kc == 0 else [], sig=(kc == 5))
        for kc in range(6):
            tps = P.op("pe", (lambda h=h, kc=kc, tk=tk: (lambda e: e.matmul(PS(3, parts=64), wqs[:, kc, 64 * h:64 * h + 64],
                       qn[:, kc, tk], start=(kc == 0), stop=(kc == 5))))(), waits=[t_wqs, qp_free[3]] if kc == 0 else [], sig=(kc == 5))
        qp_free[2] = P.op("dve", (lambda tk=tk: (lambda e: e.tensor_tensor(out=rt1, in0=PS(2, parts=64), in1=cosq_t[:, tk], op=ALU.mult)))(),
                          waits=[tpr], sig=True)
        qp_free[3] = P.op("dve", (lambda tk=tk: (lambda e: e.tensor_tensor(out=rt2, in0=PS(3, parts=64), in1=sinq_t[:, tk], op=ALU.mult)))(),
                          waits=[tps], sig=True)
        P.op("dve", (lambda h=h, tk=tk: (lambda e: e.tensor_tensor(out=QR[:, h, tk], in0=rt1, in1=rt2, op=ALU.add)))())

    PENG = "pool"
    P.op(PENG, lambda e: e.memset(sum1, 0.0))
    P.op(PENG, lambda e: e.memset(sum2, 0.0))
    diag_free = [None, None]
    cv_free = [None, None]
    cbank_free = [[], [], [], []]
    ci = 0
    for j in range(8):
        db = diag[j % 2]
        t_dg = P.op(PENG, (lambda db=db, j=j: (lambda e: e.tensor_tensor(out=db, in0=ident_f.unsqueeze(1).to_broadcast([128, 31, 128]),
                    in1=wdwt[:, j, :].unsqueeze(2).to_broadcast([128, 31, 128]), op=ALU.mult)))(), waits=[diag_free[j % 2]], sig=True)
        for blk in range(2):
            bank = 4 + ci % 4
            cb = ci % 2
            ci += 1
            sl = slice(512 * blk, 512 * blk + 512)
            for k in range(31):
                if "m" in KDC:
                    tpe = None
                    break
                c0 = k + 1 + 512 * blk
                if None:
                    c0 = (c0 // 2) * 2
                tpe = P.op("pe", (lambda db=db, j=j, k=k, c0=c0, bank=bank: (lambda e: e.matmul(PS(bank), db[:, k, :], c_ext[:, j, c0:c0 + 512],
                           start=(k == 0), stop=(k == 30))))(), waits=([t_dg] + cbank_free[bank - 4]) if k == 0 else [], sig=(k == 30))
            t_cf = None if "d" in KDC else P.op("dve", (lambda j=j, cb=cb, bank=bank: (lambda e: e.tensor_scalar(out=convf[cb], in0=PS(bank), scalar1=bdwt[:, j:j + 1],
                        scalar2=None, op0=ALU.add)))(), waits=[tpe, cv_free[cb]], sig=True)
            if "a" in KDC:
                cbank_free[bank - 4] = [t_cf]
                continue
            t_at = P.op("act", (lambda j=j, sl=sl, bank=bank: (lambda e: e.activation(out=AT[:, 8 + j, sl], in_=PS(bank), func=AF.Identity,
                        bias=bdwt[:, j:j + 1], scale=1.0)))(), waits=[tpe])
            t_sq = P.op("act", (lambda j=j, cb=cb, bank=bank: (lambda e: e.activation(out=convsq[cb], in_=PS(bank), func=AF.Square,
                        bias=bdwt[:, j:j + 1], scale=1.0)))(), waits=[cv_free[cb]], sig=True)
            cbank_free[bank - 4] = [t_cf, t_sq]
            if "p" in KDC:
                continue
            P.op(PENG, (lambda cb=cb, sl=sl: (lambda e: e.tensor_tensor(out=sum1[:, sl], in0=sum1[:, sl], in1=convf[cb], op=ALU.add)))(),
                 waits=[t_cf])
            cv_free[cb] = P.op(PENG, (lambda cb=cb, sl=sl: (lambda e: e.tensor_tensor(out=sum2[:, sl], in0=sum2[:, sl], in1=convsq[cb],
                               op=ALU.add)))(), waits=[t_sq], sig=True)
        diag_free[j % 2] = tpe
        if not None:
            emit_qproj(2 * j)
            emit_qproj(2 * j + 1)
    if None == "1":
        return finish()
    for blk in range(2):
        for which, src in ((0, sum1), (1, sum2)):
            bank = 4 + 2 * which + blk
            pe_ln = P.op("pe", (lambda blk=blk, src=src, bank=bank: (lambda e: e.matmul(PS(bank), ones_f,
                         src[:, 512 * blk:512 * blk + 512], start=True, stop=True)))(),
                         waits=[cv_free[0], cv_free[1]] + cbank_free[bank - 4], sig=True)
    if None == "2":
        return finish()
    for blk in range(2):
        sl = slice(512 * blk, 512 * blk + 512)
        P.op("dve", (lambda blk=blk, sl=sl: (lambda e: e.tensor_scalar(out=lnmean[:, sl], in0=PS(4 + blk), scalar1=1.0 / 1024, scalar2=None,
             op0=ALU.mult)))(), waits=[pe_ln])
        P.op("dve", (lambda blk=blk, sl=sl: (lambda e: e.tensor_scalar(out=lnrstd[:, sl], in0=PS(6 + blk), scalar1=1.0 / 1024, scalar2=None,
             op0=ALU.mult)))())
    P.op("dve", lambda e: e.tensor_tensor(out=sum1, in0=lnmean, in1=lnmean, op=ALU.mult))
    P.op("dve", lambda e: e.tensor_tensor(out=sum2, in0=lnrstd, in1=sum1, op=ALU.subtract))
    _t, t_lr = emit_rstd(lnrstd, sum2, 1.0)
    ybufs = [sum1, sum2]
    y_free = [None, None]
    for j in range(8):
        yb = ybufs[j % 2]
        P.op("dve", (lambda j=j, yb=yb: (lambda e: e.tensor_tensor(out=yb, in0=AT[:, 8 + j, :], in1=lnmean, op=ALU.subtract)))(),
             waits=[y_free[j % 2], t_lr])
        ty = P.op("dve", (lambda yb=yb: (lambda e: e.tensor_tensor(out=yb, in0=yb, in1=lnrstd, op=ALU.mult)))(), sig=True)
        y_free[j % 2] = P.op("act", (lambda j=j, yb=yb: (lambda e: e.activation(out=AT[:, 8 + j, :], in_=yb, func=AF.Silu,
                             bias=lnbt[:, j:j + 1], scale=lngt[:, j:j + 1])))(), waits=[ty], sig=True)
    P.barrier()
    dump("AT3", AT, [128, 16, TOWN], BF16)
    dump("QN", QN, [128, 8, TOWN], BF16)
    dump("QR", QR, [64, 8, TOWN], BF16)
    if stop <= 3:
        return finish()

    wukv = V("wukv", BF16, [4, 2048]); KT = V("KT", BF16, [8, S])
    Vaug = V("Vaug", BF16, [16 * 8 * 129 + 32])[:, 0:16 * 8 * 129].rearrange("p (k h c) -> p k h c", h=8, c=129)
    w_ukv_v = w_ukv.rearrange("(kc p) c -> p kc c", p=128)
    t_wukv = P.dma("pool", lambda e: e.dma_start(out=wukv, in_=w_ukv_v), "wukv")
    for kc16 in range(16):
        P.op("pool", (lambda kc16=kc16: (lambda e: e.memset(Vaug[:, kc16, :, 128:129], 1.0)))())
    ps_free = [None] * 8
    i = 0
    for h in range(8):
        for kb in range(4):
            bank = i % 4
            i += 1
            for c in range(4):
                tpe = P.op("pe", (lambda h=h, kb=kb, c=c, bank=bank: (lambda e: e.matmul(PS(bank), wukv[:, c, 256 * h:256 * h + 128],
                           kvn[:, c, 512 * kb:512 * kb + 512], start=(c == 0), stop=(c == 3))))(),
                           waits=[t_wukv, ps_free[bank]] if c == 0 else [], sig=(c == 3))
            eng = "act" if i % 2 == 0 else "dve"
            if eng == "act":
                ps_free[bank] = P.op("act", (lambda h=h, kb=kb, bank=bank: (lambda e: e.copy(out=KT[:, h, 512 * kb:512 * kb + 512],
                                     in_=PS(bank))))(), waits=[tpe], sig=True)
            else:
                ps_free[bank] = P.op("dve", (lambda h=h, kb=kb, bank=bank: (lambda e: e.tensor_copy(out=KT[:, h, 512 * kb:512 * kb + 512],
                                     in_=PS(bank))))(), waits=[tpe], sig=True)
    wukv4 = wukv.rearrange("p k (h c) -> p k h c", c=256)
    i = 0
    for kc16 in range(16):
        for hg in range(2):
            bank = 4 + i % 4
            i += 1
            for c in range(4):
                tpe = P.op("pe", (lambda kc16=kc16, hg=hg, c=c, bank=bank: (lambda e: e.matmul(
                           PS(bank).rearrange("p (h c) -> p h c", c=128), kvn[:, c, 128 * kc16:128 * kc16 + 128],
                           wukv4[:, c, 4 * hg:4 * hg + 4, 128:256], start=(c == 0), stop=(c == 3))))(),
                           waits=[t_wukv, ps_free[bank]] if c == 0 else [], sig=(c == 3))
            if i % 2 == 0:
                ps_free[bank] = P.op("act", (lambda kc16=kc16, hg=hg, bank=bank: (lambda e: e.copy(out=Vaug[:, kc16, 4 * hg:4 * hg + 4, 0:128],
                                     in_=PS(bank).rearrange("p (h c) -> p h c", c=128))))(), waits=[tpe], sig=True)
            else:
                ps_free[bank] = P.op("dve", (lambda kc16=kc16, hg=hg, bank=bank: (lambda e: e.tensor_copy(out=Vaug[:, kc16, 4 * hg:4 * hg + 4, 0:128],
                                     in_=PS(bank).rearrange("p (h c) -> p h c", c=128))))(), waits=[tpe], sig=True)
    P.barrier()
    dump("KT", KT, [128, 8, S], BF16)
    dump("Vaug", Vaug, [128, 16, 8, 129], BF16)
    if stop <= 4:
        return finish()

    PT = [V("PT0", BF16, [16, 512]), V("PT1", BF16, [16, 512])]
    rcp = V("rcp", F32, [16]); Otok = [V("Otok0", BF16, [128]), V("Otok1", BF16, [128])]
    Xd_v = Xd.rearrange("(e p) d -> e p d", p=128)
    for e_ in range(NE):
        P.dma("sync", (lambda e_=e_: (lambda e: e.dma_start(out=Xd_v[e_], in_=zero_b)))(), "xdz")
    steps = [(h, qb) for h in range(8) for qb in range(2)]
    st_bank_free = [None] * 4
    exp_done = {}
    pv_done = {}
    o_bank_free = [None, None]
    otok_free = [None, None]
    tile_i = 0

    def emit_ST(s):
        nonlocal tile_i
        h, qb = steps[s]
        qs = slice(512 * qb, 512 * qb + 512)
        for kc in range(16):
            bank = tile_i % 4
            tile_i += 1
            P.op("pe", (lambda h=h, kc=kc, qs=qs, bank=bank: (lambda e: e.matmul(PS(bank), KT[:, h, 128 * kc:128 * kc + 128], QN[:, h, qs],
                 start=True, stop=False)))(), waits=[st_bank_free[bank]])
            tpe = P.op("pe", (lambda h=h, kc=kc, qs=qs, bank=bank: (lambda e: e.matmul(PS(bank), kr[:, 128 * kc:128 * kc + 128], QR[:, h, qs],
                       start=False, stop=True)))(), sig=True)
            w = [tpe]
            if kc == 0 and s >= 2:
                w.append(pv_done[s - 2])
            tex = P.op("act", (lambda s=s, kc=kc, bank=bank: (lambda e: e.activation(out=PT[s % 2][:, kc, :], in_=PS(bank), func=AF.Exp,
                       scale=SCALE)))(), waits=w, sig=True)
            st_bank_free[bank] = tex
        exp_done[s] = tex

    pending_tr = []

    def emit_PV(s):
        h, qb = steps[s]
        for qc in range(4):
            ob = qc % 2
            for kc in range(16):
                tpe = P.op("pe", (lambda s=s, h=h, kc=kc, qc=qc, ob=ob: (lambda e: e.matmul(PS(4 + ob)[:, 0:129],
                           PT[s % 2][:, kc, 128 * qc:128 * qc + 128], Vaug[:, kc, h, :], start=(kc == 0), stop=(kc == 15))))(),
                           waits=[exp_done[s], o_bank_free[ob]] if kc == 0 else [], sig=(kc == 15))
            P.op("dve", (lambda ob=ob, qc=qc: (lambda e: e.reciprocal(out=rcp[:, qc:qc + 1], in_=PS(4 + ob)[:, 128:129])))(), waits=[tpe])
            tn = P.op("dve", (lambda ob=ob, qc=qc: (lambda e: e.tensor_scalar(out=Otok[ob], in0=PS(4 + ob)[:, 0:128],
                      scalar1=rcp[:, qc:qc + 1], scalar2=None, op0=ALU.mult)))(), waits=[otok_free[ob]], sig=True)
            o_bank_free[ob] = tn
            pending_tr.append((s, qc, ob, tn))
            if len(pending_tr) >= 2:
                emit_TR(pending_tr.pop(0))
        pv_done[s] = tpe

    tr_i = 0

    tr_bank_free = [None, None]

    def emit_TR(item):
        s, qc, ob, tn = item
        h, qb = steps[s]
        bank = 6 + s % 2
        tt = P.op("pe", (lambda ob=ob, bank=bank, qc=qc: (lambda e: e.transpose(out=PS(bank, BF16)[:, 128 * qc:128 * qc + 128], in_=Otok[ob],
                  identity=ident_b)))(), waits=[tn] + ([tr_bank_free[s % 2]] if qc == 0 else []), sig=True)
        otok_free[ob] = tt
        if qc == 3:
            tr_bank_free[s % 2] = P.op("dve", (lambda h=h, qb=qb, bank=bank: (lambda e: e.tensor_copy(out=AT[:, h, 512 * qb:512 * qb + 512],
                                       in_=PS(bank, BF16)[:, 0:512])))(), waits=[tt], sig=True)

    KD5 = ""
    if "t" in KD5:
        emit_TR = lambda item: None
    if "z" in KD5:
        steps = steps[:2]
    emit_ST(0)
    for s in range(len(steps)):
        if s + 1 < len(steps):
            emit_ST(s + 1)
        if "p" not in KD5:
            emit_PV(s)
    while pending_tr:
        emit_TR(pending_tr.pop(0))
    if debug:
        d_at = dbg("AT", [128, 16, TOWN], BF16)
        P.barrier()
        P.dma("sync", lambda e: e.dma_start(out=d_at, in_=AT), "dbg")
    P.barrier()
    if stop <= 5:
        return finish()

    wob = [V("wo0", BF16, [16, 512]), V("wo1", BF16, [16, 512])]
    hT = V("h", F32, [8, D])
    w_o_v = w_o.rearrange("(kc p) c -> p kc c", p=128)
    t_x = P.dma("sync", lambda e: e.dma_start(out=hT, in_=xown.rearrange("(t p) d -> p t d", p=128)), "xown")
    wo_free = [None, None]
    ps_free = [None] * 8
    i = 0
    for g in range(4):
        two = P.dma("pool", (lambda g=g: (lambda e: e.dma_start(out=wob[g % 2], in_=w_o_v[:, :, 512 * g:512 * g + 512])))(),
                    f"wo{g % 2}", waits=[wo_free[g % 2]])
        for tt in range(8):
            bank = i % 4
            i += 1
            for f in range(16):
                tpe = P.op("pe", (lambda g=g, tt=tt, f=f, bank=bank: (lambda e: e.matmul(PS(bank), AT[:, f, 128 * tt:128 * tt + 128],
                           wob[g % 2][:, f, :], start=(f == 0), stop=(f == 15))))(), waits=[two, ps_free[bank]] if f == 0 else [], sig=(f == 15))
            ps_free[bank] = P.op("dve", (lambda g=g, tt=tt, bank=bank: (lambda e: e.tensor_tensor(out=hT[:, tt, 512 * g:512 * g + 512],
                                 in0=hT[:, tt, 512 * g:512 * g + 512], in1=PS(bank), op=ALU.add)))(), waits=[tpe, t_x], sig=True)
        wo_free[g % 2] = tpe
    P.barrier()
    if stop <= 6:
        return finish()
    if debug:
        d_h = dbg("h", [TOWN, D], F32)
        P.dma("sync", lambda e: e.dma_start(out=d_h.rearrange("(t p) d -> p t d", p=128), in_=hT), "dbg")

    g2b = V("g2b", F32, [D]); hnf = V("hnf", F32, [D]); hnb = V("hnb", BF16, [8, D]); hnT = V("hnT", F32, [16, 128])
    wr = V("wr", F32, [16, 72]); ssq7 = V("ssq7", F32, [16]); L = V("L", F32, [8, 72])
    sm = {nm: V(nm, F32, [8]) for nm in ("gmax", "gsum", "pg", "m1", "m2", "w1", "w2", "r1", "r2", "v1", "v2", "d1", "d2")}
    gd = V("gd", F32, [8, 8]); gmask = V("gmask", F32, [8, 8])
    sel = V("sel", F32, [8, 8]); sel2 = V("sel2", F32, [8, 8]); mask1 = V("mask1", F32, [8, 8]); mask2 = V("mask2", F32, [8, 8])
    rl4 = V("rl4", F32, [8, 8, 8]); A1 = V("A1", F32, [8, 8, 8]); A2 = V("A2", F32, [8, 8, 8])
    rank = V("rank", F32, [8, 64]); tmp64 = V("tmp64", F32, [8, 64]); Abf = V("Abf", BF16, [8, 64])
    dest_i = V("dest_i", I32, [16]); gates = V("gates", F32, [16])
    t_g2 = P.dma("sync", lambda e: e.dma_start(out=g2b, in_=g2b_d), "g2b")
    t_wr = P.dma("sync", lambda e: e.dma_start(out=wr, in_=w_r.rearrange("(kc p) c -> p kc c", p=128)), "wr")
    t_hs = P.dma("sync", lambda e: e.dma_start(out=hbuf.rearrange("(t p) d -> p t d", p=128), in_=hT), "hbuf")
    ps_free = [None] * 8
    hnfb = [hnf, V("hnf1", F32, [D])]
    hnTb = [hnT, V("hnT1", F32, [16, 128])]
    hnf_free = [None, None]
    hnT_free = [None, None]
    hn_tok = {}
    P.op("dve", lambda e: e.memset(ssq7, 0.0))
    t_ms = P.op("dve", lambda e: e.memset(hnb[:, 0, 0:2], 0.0), sig=True)

    def stage_S(tt):
        hf = hnfb[tt % 2]
        t_sq = P.op("act", (lambda tt=tt, hf=hf: (lambda e: e.activation(out=hf, in_=hT[:, tt, :], func=AF.Square, accum_out=ssq7[:, tt:tt + 1])))(),
                    waits=[hnf_free[tt % 2], t_ms], sig=True)
        _t, t_r = emit_rstd(ssq7[:, 8 + tt:9 + tt], ssq7[:, tt:tt + 1], 1.0 / D, waits=[t_sq])
        t_hn = P.op("dve", (lambda tt=tt, hf=hf: (lambda e: e.scalar_tensor_tensor(out=hf, in0=hT[:, tt, :], scalar=ssq7[:, 8 + tt:9 + tt], in1=g2b,
                    op0=ALU.mult, op1=ALU.mult)))(), waits=[t_g2], sig=True)
        t_hb_ = P.op("act", (lambda tt=tt, hf=hf: (lambda e: e.copy(out=hnb[:, tt, :], in_=hf)))(), waits=[t_hn], sig=True)
        hn_tok[tt] = (t_hn, t_hb_)

    def stage_T(tt):
        hf = hnfb[tt % 2]
        hTt = hnTb[tt % 2]
        t_hn = hn_tok[tt][0]
        for q4 in range(4):
            bank = q4
            for r in range(4):
                kc = 4 * q4 + r
                tpe = P.op("pe", (lambda kc=kc, bank=bank, r=r, hf=hf: (lambda e: e.transpose(out=PS(bank)[:, 128 * r:128 * r + 128],
                           in_=hf[:, 128 * kc:128 * kc + 128], identity=ident_f)))(), waits=[t_hn, ps_free[bank]] if r == 0 else [], sig=(r == 3))
            if q4 % 2 == 0:
                ps_free[bank] = P.op("dve", (lambda q4=q4, bank=bank, hTt=hTt: (lambda e: e.tensor_copy(out=hTt[:, 4 * q4:4 * q4 + 4, :],
                                     in_=PS(bank).rearrange("p (a b) -> p a b", b=128))))(), waits=[tpe, hnT_free[tt % 2]], sig=True)
            else:
                ps_free[bank] = P.op("act", (lambda q4=q4, bank=bank, hTt=hTt: (lambda e: e.copy(out=hTt[:, 4 * q4:4 * q4 + 4, :],
                                     in_=PS(bank).rearrange("p (a b) -> p a b", b=128))))(), waits=[tpe, hnT_free[tt % 2]], sig=True)
        hnf_free[tt % 2] = tpe
        for kc in range(16):
            tpl = P.op("pe", (lambda kc=kc, hTt=hTt: (lambda e: e.matmul(PS(4)[:, 0:72], hTt[:, kc, :], wr[:, kc, :], start=(kc == 0), stop=(kc == 15))))(),
                       waits=[ps_free[0], ps_free[1], ps_free[2], ps_free[3], t_wr, ps_free[4]] if kc == 0 else [], sig=(kc == 15))
        hnT_free[tt % 2] = tpl
        ps_free[4] = P.op("dve", (lambda tt=tt: (lambda e: e.tensor_tensor(out=L[:, tt, :], in0=PS(4)[:, 0:72], in1=brb, op=ALU.add)))(),
                          waits=[tpl], sig=True)

    stage_S(0)
    for tt in range(8):
        if tt + 1 < 8:
            stage_S(tt + 1)
        stage_T(tt)
    t_hb = hn_tok[7][1]
    if debug:
        d_L = dbg("L", [128, 8, 72], F32)
        P.dma("sync", lambda e: e.dma_start(out=d_L, in_=L), "dbg", waits=[ps_free[4]])
    gl = L[:, :, 0:8]
    rlv = L[:, :, 8:72].rearrange("p t (g j) -> p t g j", j=8)

    def bc(ap, shape):
        return ap.to_broadcast(shape)

    dv = lambda fn, **kw: P.op("dve", fn, **kw)
    dv(lambda e: e.tensor_reduce(out=sm["gmax"], in_=gl, axis=AX.X, op=ALU.max))
    dv(lambda e: e.tensor_tensor(out=gd, in0=gl, in1=bc(sm["gmax"].unsqueeze(2), [128, 8, 8]), op=ALU.subtract))
    t_gd = dv(lambda e: e.tensor_tensor(out=gmask, in0=gl, in1=bc(sm["gmax"].unsqueeze(2), [128, 8, 8]), op=ALU.is_equal), sig=True)
    t_ge = P.op("act", lambda e: e.activation(out=gd, in_=gd, func=AF.Exp), waits=[t_gd], sig=True)
    dv(lambda e: e.tensor_reduce(out=sm["gsum"], in_=gd, axis=AX.X, op=ALU.add), waits=[t_ge])
    dv(lambda e: e.reciprocal(out=sm["pg"], in_=sm["gsum"]))
    dv(lambda e: e.tensor_tensor(out=rl4, in0=rlv, in1=bc(gmask.unsqueeze(3), [128, 8, 8, 8]), op=ALU.mult))
    dv(lambda e: e.tensor_reduce(out=sel, in_=rl4.rearrange("p t g j -> p t j g"), axis=AX.X, op=ALU.add))
    dv(lambda e: e.tensor_reduce(out=sm["m1"], in_=sel, axis=AX.X, op=ALU.max))
    dv(lambda e: e.tensor_tensor(out=mask1, in0=sel, in1=bc(sm["m1"].unsqueeze(2), [128, 8, 8]), op=ALU.is_equal))
    dv(lambda e: e.scalar_tensor_tensor(out=sel2, in0=mask1, scalar=-1e30, in1=sel, op0=ALU.mult, op1=ALU.add))
    dv(lambda e: e.tensor_reduce(out=sm["m2"], in_=sel2, axis=AX.X, op=ALU.max))
    dv(lambda e: e.tensor_tensor(out=mask2, in0=sel2, in1=bc(sm["m2"].unsqueeze(2), [128, 8, 8]), op=ALU.is_equal))
    t_w = dv(lambda e: e.tensor_tensor(out=sm["w1"], in0=sm["m2"], in1=sm["m1"], op=ALU.subtract), sig=True)
    t_we = P.op("act", lambda e: e.activation(out=sm["w1"], in_=sm["w1"], func=AF.Exp), waits=[t_w], sig=True)
    dv(lambda e: e.tensor_scalar(out=sm["w1"], in0=sm["w1"], scalar1=1.0, scalar2=None, op0=ALU.add), waits=[t_we])
    dv(lambda e: e.reciprocal(out=sm["w1"], in_=sm["w1"]))
    dv(lambda e: e.tensor_scalar(out=sm["w2"], in0=sm["w1"], scalar1=-1.0, scalar2=1.0, op0=ALU.mult, op1=ALU.add))
    dv(lambda e: e.tensor_tensor(out=A1, in0=bc(gmask.unsqueeze(3), [128, 8, 8, 8]), in1=bc(mask1.unsqueeze(2), [128, 8, 8, 8]), op=ALU.mult))
    dv(lambda e: e.tensor_tensor(out=A2, in0=bc(gmask.unsqueeze(3), [128, 8, 8, 8]), in1=bc(mask2.unsqueeze(2), [128, 8, 8, 8]), op=ALU.mult))
    A1f = A1.rearrange("p t g j -> p t (g j)"); A2f = A2.rearrange("p t g j -> p t (g j)")
    dv(lambda e: e.tensor_tensor(out=tmp64, in0=A1f, in1=A2f, op=ALU.add))
    t_A = dv(lambda e: e.tensor_copy(out=Abf, in_=tmp64), sig=True)
    for tt in range(8):
        bank = 5 + tt % 2
        tpe = P.op("pe", (lambda tt=tt, bank=bank: (lambda e: e.matmul(PS(bank)[:, 0:64], triu_b, Abf[:, tt, :], start=True, stop=(tt == 0))))(),
                   waits=[t_A, ps_free[bank]], sig=(tt == 0))
        for t2_ in range(tt):
            tpe = P.op("pe", (lambda t2_=t2_, tt=tt, bank=bank: (lambda e: e.matmul(PS(bank)[:, 0:64], ones_b, Abf[:, t2_, :], start=False,
                       stop=(t2_ == tt - 1))))(), sig=(t2_ == tt - 1))
        ps_free[bank] = dv((lambda tt=tt, bank=bank: (lambda e: e.tensor_copy(out=rank[:, tt, :], in_=PS(bank)[:, 0:64])))(), waits=[tpe], sig=True)
    for k, (Af, rk, vk, dk, wk) in enumerate(((A1f, "r1", "v1", "d1", "w1"), (A2f, "r2", "v2", "d2", "w2"))):
        dv((lambda Af=Af: (lambda e: e.tensor_tensor(out=tmp64, in0=Af, in1=rank, op=ALU.mult)))())
        dv((lambda rk=rk: (lambda e: e.tensor_reduce(out=sm[rk], in_=tmp64, axis=AX.X, op=ALU.add)))())
        dv((lambda Af=Af: (lambda e: e.tensor_tensor(out=tmp64, in0=Af, in1=bc(e128.unsqueeze(1), [128, 8, 64]), op=ALU.mult)))())
        dv((lambda dk=dk: (lambda e: e.tensor_reduce(out=sm[dk], in_=tmp64, axis=AX.X, op=ALU.add)))())
        dv((lambda dk=dk, rk=rk: (lambda e: e.tensor_tensor(out=sm[dk], in0=sm[dk], in1=sm[rk], op=ALU.add)))())
        dv((lambda vk=vk, rk=rk: (lambda e: e.tensor_scalar(out=sm[vk], in0=sm[rk], scalar1=float(CAP) - 0.5, scalar2=None, op0=ALU.is_lt)))())
        dv((lambda dk=dk, vk=vk: (lambda e: e.tensor_tensor(out=sm[dk], in0=sm[dk], in1=sm[vk], op=ALU.mult)))())
        dv((lambda rk=rk, vk=vk: (lambda e: e.tensor_scalar(out=sm[rk], in0=sm[vk], scalar1=-1.0e6, scalar2=1.0e6, op0=ALU.mult, op1=ALU.add)))())
        dv((lambda dk=dk, rk=rk: (lambda e: e.tensor_tensor(out=sm[dk], in0=sm[dk], in1=sm[rk], op=ALU.add)))())
        dv((lambda dk=dk, k=k: (lambda e: e.tensor_copy(out=dest_i[:, 8 * k:8 * k + 8], in_=sm[dk])))())
        dv((lambda wk=wk, k=k: (lambda e: e.tensor_tensor(out=gates[:, 8 * k:8 * k + 8], in0=sm[wk], in1=sm["pg"], op=ALU.mult)))())
        t_disp = dv((lambda vk=vk, k=k: (lambda e: e.tensor_tensor(out=gates[:, 8 * k:8 * k + 8], in0=gates[:, 8 * k:8 * k + 8], in1=sm[vk],
                    op=ALU.mult)))(), sig=True)
    if debug:
        d_dest = dbg("dest", [128, 16], I32); d_gates = dbg("gates", [128, 16], F32)
        P.dma("sync", lambda e: e.dma_start(out=d_dest, in_=dest_i), "dbg", waits=[t_disp])
        P.dma("sync", lambda e: e.dma_start(out=d_gates, in_=gates), "dbg", waits=[t_disp])
    t_z = P.dma_last["xdz"]
    for k in range(2):
        for tt in range(8):
            t_sc = P.dma("pool", (lambda k=k, tt=tt: (lambda e: e.indirect_dma_start(out=Xd, out_offset=bass.IndirectOffsetOnAxis(
                         ap=dest_i[:, 8 * k + tt:8 * k + tt + 1], axis=0), in_=hnb[:, tt, :], in_offset=None,
                         bounds_check=NE * CAP - 1, oob_is_err=False)))(), "scat", waits=[t_disp, t_z, t_hb])
    P.barrier()
    if stop <= 7:
        return finish()

    wg = [V("wg0", BF16, [16, 512]), V("wg1", BF16, [16, 512])]
    wu = [V("wu_0", BF16, [16, 512]), V("wu_1", BF16, [16, 512])]
    wd = [V("wd0", BF16, [4, D]), V("wd1", BF16, [4, D])]
    Xe = [V("Xe0", BF16, [D]), V("Xe1", BF16, [D])]
    XT = [V("XT0", BF16, [16, 128]), V("XT1", BF16, [16, 128])]
    sgt = [V("sgt0", F32, [512]), V("sgt1", F32, [512])]
    hb = [V("hb0", BF16, [512]), V("hb1", BF16, [512])]
    HT = [V("HT0", BF16, [4, 128]), V("HT1", BF16, [4, 128])]
    Ysb = [V("Ysb0", F32, [D]), V("Ysb1", F32, [D])]
    wdf = [V("wdf0", F32, [2, D]), V("wdf1", F32, [2, D])]
    wgv = w_gate.rearrange("e (kc p) c -> e p kc c", p=128)
    wuv = w_up.rearrange("e (kc p) c -> e p kc c", p=128)
    wdv = w_down.rearrange("e (kc p) c -> e p kc c", p=128)
    Ybuf_v = Ybuf.rearrange("(e p) d -> e p d", p=128)
    gu_done = [None, None]; dn_done = [None, None]; xtr_done = [None, None]; xt_used = [None, None]
    sgt_free = [None, None]; hb_free = [None, None]; ht_free = [None, None]; ysb_free = [None, None]
    bfree = [None] * 8
    ld = {}
    xt_rdy = {}
    mult_done = {}
    ht_rdy = {}

    xe_tok = {}

    def issue_xe(ex):
        b = ex % 2
        xe_tok[ex] = P.dma("sync", (lambda ex=ex, b=b: (lambda e: e.dma_start(out=Xe[b], in_=Xd_v[ex])))(), f"xe{b}", waits=[xtr_done[b]])

    def issue_loads(ex):
        b = ex % 2
        t_xe = xe_tok[ex]
        t_wg = P.dma("pool", (lambda ex=ex, b=b: (lambda e: e.dma_start(out=wg[b], in_=wgv[ex])))(), f"wg{b}", waits=[gu_done[b]])
        t_wu = P.dma("pool", (lambda ex=ex, b=b: (lambda e: e.dma_start(out=wu[b], in_=wuv[ex])))(), f"wu{b}", waits=[gu_done[b]])
        wdf_tok[ex] = [P.dma("sync", (lambda ex=ex, h=h: (lambda e: e.dma_start(out=wdf[h], in_=wdv[ex][:, 2 * h:2 * h + 2, :])))(), f"wdf{h}",
                             waits=[wdf_free[h]]) for h in range(2)]
        ld[ex] = (t_xe, t_wg, t_wu, None)

    wdf_tok = {}
    wdf_free = [None, None]
    wd_rdy = {}

    def cast_wd(ex):
        b = ex % 2
        t0 = P.op("act", (lambda b=b: (lambda e: e.copy(out=wd[b][:, 0:2, :], in_=wdf[0])))(), waits=[wdf_tok[ex][0], dn_done[b]], sig=True)
        t1 = P.op("dve", (lambda b=b: (lambda e: e.tensor_copy(out=wd[b][:, 2:4, :], in_=wdf[1])))(), waits=[wdf_tok[ex][1], dn_done[b]], sig=True)
        wdf_free[0], wdf_free[1] = t0, t1
        wd_rdy[ex] = [t0, t1]

    def stage_A1(ex):
        b = ex % 2
        t_xe = ld[ex][0]
        for half in range(2):
            for r in range(8):
                kc = 8 * half + r
                tpe = P.op("pe", (lambda b=b, kc=kc, half=half, r=r: (lambda e: e.transpose(out=PS(half, BF16)[:, 128 * r:128 * r + 128],
                           in_=Xe[b][:, 128 * kc:128 * kc + 128], identity=ident_b)))(), waits=[t_xe, bfree[half]] if r == 0 else [], sig=(r == 7))
            if half == 0:
                bfree[0] = P.op("act", (lambda b=b: (lambda e: e.copy(out=XT[b][:, 0:8, :], in_=PS(0, BF16).rearrange("p (a c) -> p a c", c=128))))(),
                                waits=[tpe, xt_used[b]], sig=True)
            else:
                bfree[1] = P.op("dve", (lambda b=b: (lambda e: e.tensor_copy(out=XT[b][:, 8:16, :], in_=PS(1, BF16).rearrange("p (a c) -> p a c", c=128))))(),
                                waits=[tpe, xt_used[b]], sig=True)
        xtr_done[b] = tpe
        xt_rdy[ex] = [bfree[0], bfree[1]]

    def stage_A2(ex):
        b = ex % 2
        _, t_wg, t_wu, _ = ld[ex]
        for which, (wt, tw, bank) in enumerate(((wg[b], t_wg, 2), (wu[b], t_wu, 3))):
            for kc in range(16):
                tpe = P.op("pe", (lambda b=b, wt=wt, kc=kc, bank=bank: (lambda e: e.matmul(PS(bank), XT[b][:, kc, :], wt[:, kc, :],
                           start=(kc == 0), stop=(kc == 15))))(), waits=xt_rdy[ex] + [tw, bfree[bank]] if kc == 0 else [], sig=(kc == 15))
            if which == 0:
                t_hg = tpe
        gu_done[b] = tpe
        xt_used[b] = tpe
        bfree[2] = P.op("act", (lambda b=b: (lambda e: e.activation(out=sgt[b], in_=PS(2), func=AF.Silu)))(), waits=[t_hg, sgt_free[b]], sig=True)
        bfree[3] = P.op("dve", (lambda b=b: (lambda e: e.tensor_tensor(out=hb[b], in0=sgt[b], in1=PS(3), op=ALU.mult)))(),
                        waits=[bfree[2], tpe, hb_free[b]], sig=True)
        sgt_free[b] = bfree[3]
        mult_done[ex] = bfree[3]

    def stage_B1(ex):
        b = ex % 2
        for fc in range(4):
            tpe = P.op("pe", (lambda b=b, fc=fc: (lambda e: e.transpose(out=PS(4, BF16)[:, 128 * fc:128 * fc + 128],
                       in_=hb[b][:, 128 * fc:128 * fc + 128], identity=ident_b)))(), waits=[mult_done[ex], bfree[4]] if fc == 0 else [], sig=(fc == 3))
        hb_free[b] = tpe
        bfree[4] = P.op("act", (lambda b=b: (lambda e: e.copy(out=HT[b], in_=PS(4, BF16)[:, 0:512].rearrange("p (a c) -> p a c", c=128))))(),
                        waits=[tpe, ht_free[b]], sig=True)
        ht_rdy[ex] = bfree[4]

    def stage_B2(ex):
        b = ex % 2
        ev = []
        for cb, bank in enumerate((5, 6, 7, 5)):
            for fc in range(4):
                tpe = P.op("pe", (lambda b=b, fc=fc, cb=cb, bank=bank: (lambda e: e.matmul(PS(bank), HT[b][:, fc, :],
                           wd[b][:, fc, 512 * cb:512 * cb + 512], start=(fc == 0), stop=(fc == 3))))(),
                           waits=([ht_rdy[ex], bfree[bank]] + wd_rdy[ex]) if fc == 0 else [], sig=(fc == 3))
            if cb % 2 == 0:
                bfree[bank] = P.op("dve", (lambda b=b, cb=cb, bank=bank: (lambda e: e.tensor_copy(out=Ysb[b][:, 512 * cb:512 * cb + 512],
                                   in_=PS(bank))))(), waits=[tpe, ysb_free[b]], sig=True)
            else:
                bfree[bank] = P.op("act", (lambda b=b, cb=cb, bank=bank: (lambda e: e.copy(out=Ysb[b][:, 512 * cb:512 * cb + 512],
                                   in_=PS(bank))))(), waits=[tpe, ysb_free[b]], sig=True)
            ev.append(bfree[bank])
        dn_done[b] = tpe
        ht_free[b] = tpe
        ysb_free[b] = P.dma("sync", (lambda ex=ex, b=b: (lambda e: e.dma_start(out=Ybuf_v[ex], in_=Ysb[b])))(), f"yout{b}", waits=ev)

    issue_xe(0)
    issue_xe(1)
    issue_loads(0)
    cast_wd(0)
    issue_loads(1)
    stage_A1(0)
    stage_A2(0)
    for ex in range(NE):
        if ex + 2 < NE:
            issue_xe(ex + 2)
        if ex + 1 < NE:
            stage_A1(ex + 1)
        stage_B1(ex)
        if ex + 1 < NE:
            stage_A2(ex + 1)
        stage_B2(ex)
        if ex + 1 < NE:
            cast_wd(ex + 1)
        if ex + 2 < NE:
            issue_loads(ex + 2)
    P.barrier()
    if stop <= 8:
        return finish()

    fgb = V("fgb", F32, [D])
    Y1 = [V("Y1_0", F32, [D]), V("Y1_1", F32, [D])]; Y2 = [V("Y2_0", F32, [D]), V("Y2_1", F32, [D])]
    hz = [V("hz0", F32, [D]), V("hz1", F32, [D])]; ss9 = [V("ss9_0", F32, [4]), V("ss9_1", F32, [4])]
    t_fg = P.dma("sync", lambda e: e.dma_start(out=fgb, in_=fgb_d), "fgb")
    buf_free = [None, None]
    out_v = out.rearrange("(t p) d -> t p d", p=128)
    hbuf_v = hbuf.rearrange("(t p) d -> t p d", p=128)
    t_out = None
    for tt in range(8):
        b = tt % 2
        t_h = P.dma("sync", (lambda tt=tt, b=b: (lambda e: e.dma_start(out=hz[b], in_=hbuf_v[tt])))(), f"hz{b}", waits=[buf_free[b], t_hs])
        t_m1 = P.op("pool", (lambda b=b: (lambda e: e.memset(Y1[b], 0.0)))(), waits=[buf_free[b]])
        t_m2 = P.op("pool", (lambda b=b: (lambda e: e.memset(Y2[b], 0.0)))(), sig=True)
        t_g1 = P.dma("pool", (lambda tt=tt, b=b: (lambda e: e.indirect_dma_start(out=Y1[b], out_offset=None, in_=Ybuf,
                     in_offset=bass.IndirectOffsetOnAxis(ap=dest_i[:, tt:tt + 1], axis=0), bounds_check=NE * CAP - 1, oob_is_err=False)))(),
                     f"ga{b}", waits=[t_m2])
        t_g2_ = P.dma("pool", (lambda tt=tt, b=b: (lambda e: e.indirect_dma_start(out=Y2[b], out_offset=None, in_=Ybuf,
                      in_offset=bass.IndirectOffsetOnAxis(ap=dest_i[:, 8 + tt:9 + tt], axis=0), bounds_check=NE * CAP - 1, oob_is_err=False)))(),
                      f"gb{b}", waits=[t_m2])
        P.op("dve", (lambda b=b: (lambda e: e.memset(ss9[b], 0.0)))(), waits=[buf_free[b]])
        P.op("dve", (lambda tt=tt, b=b: (lambda e: e.scalar_tensor_tensor(out=hz[b], in0=Y1[b], scalar=gates[:, tt:tt + 1], in1=hz[b],
             op0=ALU.mult, op1=ALU.add)))(), waits=[t_h, t_g1])
        t_z9 = P.op("dve", (lambda tt=tt, b=b: (lambda e: e.scalar_tensor_tensor(out=hz[b], in0=Y2[b], scalar=gates[:, 8 + tt:9 + tt], in1=hz[b],
                    op0=ALU.mult, op1=ALU.add)))(), waits=[t_g2_], sig=True)
        t_s9 = P.op("act", (lambda b=b: (lambda e: e.activation(out=Y1[b], in_=hz[b], func=AF.Square, accum_out=ss9[b][:, 0:1])))(),
                    waits=[t_z9], sig=True)
        _t, t_r9 = emit_rstd(ss9[b][:, 3:4], ss9[b][:, 0:1], 1.0 / D, waits=[t_s9])
        t_o = P.op("dve", (lambda b=b: (lambda e: e.scalar_tensor_tensor(out=hz[b], in0=hz[b], scalar=ss9[b][:, 3:4], in1=fgb,
                   op0=ALU.mult, op1=ALU.mult)))(), waits=[t_fg], sig=True)
        t_out = P.dma("sync", (lambda tt=tt, b=b: (lambda e: e.dma_start(out=out_v[tt], in_=hz[b])))(), f"out{b}", waits=[t_o])
        buf_free[b] = t_out
    P.wait_only("sync", [P.dma_last["out0"], P.dma_last["out1"]] + ([P.dma_last["dbg"]] if debug else []))
    P.emit(stack, ps_probe)
    stack.close()
    return nc, list(dbg_out.keys())


_CACHE = {}


def _host_inputs(inputs):
    f32 = np.float32
    x = np.asarray(inputs["x"], f32)
    B = x.shape[0]

    def vecT(v, n):
        return np.ascontiguousarray(np.asarray(v, f32).reshape(n, 128).T)

    com = {
        "ident": np.eye(128, dtype=f32),
        "triu": np.triu(np.ones((128, 128), f32), 1),
        "e128": np.broadcast_to((np.arange(NE, dtype=f32) * CAP)[None, :], (128, NE)).copy(),
        "g1t": vecT(inputs["ln1_g"][0], 16),
        "bglut": vecT(inputs["b_glu"][0], 16),
        "qgt": vecT(inputs["q_norm_g"][0], 6),
        "kvgt": vecT(inputs["kv_norm_g"][0], 4),
        "wdwt": np.ascontiguousarray(np.asarray(inputs["w_dw"][0], f32).T.reshape(8, 128, 31).transpose(1, 0, 2)),
        "bdwt": vecT(inputs["b_dw"][0], 8),
        "lngt": vecT(inputs["conv_ln_g"][0], 8),
        "lnbt": vecT(inputs["conv_ln_b"][0], 8),
        "g2b": np.broadcast_to(np.asarray(inputs["ln2_g"][0], f32)[None, :], (128, D)).copy(),
        "fgb": np.broadcast_to(np.asarray(inputs["final_g"], f32)[None, :], (128, D)).copy(),
        "brb": np.broadcast_to(np.concatenate([np.asarray(inputs["b_group"][0], f32), np.asarray(inputs["b_router"][0], f32)])[None, :],
                               (128, 72)).copy(),
        "w_in": np.asarray(inputs["w_in"][0], f32),
        "w_uq": np.asarray(inputs["w_uq"][0], f32),
        "w_ukv": np.asarray(inputs["w_ukv"][0], f32),
        "w_o": np.asarray(inputs["w_o"][0], f32),
        "w_r": np.ascontiguousarray(np.concatenate([np.asarray(inputs["w_group"][0], f32), np.asarray(inputs["w_router"][0], f32)], axis=1)),
        "w_gate": np.asarray(inputs["w_gate"][0], f32),
        "w_up": np.asarray(inputs["w_up"][0], f32),
        "w_down": np.asarray(inputs["w_down"][0], f32),
    }
    pos = np.arange(S, dtype=f32)
    inv_freq = (f32(10000.0) ** (-np.arange(0, 64, 2, dtype=f32) / f32(64))).astype(f32)
    ang = (pos[:, None] * inv_freq[None, :]).astype(f32)
    cos = np.cos(ang).astype(f32).T
    sin = np.sin(ang).astype(f32).T
    cos64 = np.concatenate([cos, cos], 0)
    sin64 = np.concatenate([-sin, sin], 0)
    in_maps = []
    for c in range(NCORES):
        b, hf = c // 2, c % 2
        own = np.arange(hf * TOWN, (hf + 1) * TOWN)
        oth = np.arange((1 - hf) * TOWN, (2 - hf) * TOWN)
        order = np.concatenate([own, oth])
        xb = x[b]
        xt = np.zeros((D, 2 * TOWN + 32), f32)
        xt[:, 0:2 * TOWN] = xb[order].T
        hm = np.zeros((32,), f32)
        lh = np.arange(own[0] - 16, own[0]); rh = np.arange(own[-1] + 1, own[-1] + 17)
        for i, p in enumerate(np.concatenate([lh, rh])):
            if 0 <= p < S:
                xt[:, 2 * TOWN + i] = xb[p]
                hm[i] = 1.0
        m = dict(com)
        m["xT"] = xt
        m["xown"] = np.ascontiguousarray(xb[own])
        m["cosk"] = np.ascontiguousarray(cos64[:, order])
        m["sink"] = np.ascontiguousarray(sin64[:, order])
        m["halo_mask"] = np.broadcast_to(hm[None, :], (128, 32)).copy()
        in_maps.append(m)
    return in_maps, B


def kernel(**inputs):
    debug = bool(inputs.pop("_debug", False))
    stop = int(inputs.pop("_stop", 99))
    key = ("prog", debug, stop)
    if key not in _CACHE:
        _CACHE[key] = build_program(debug, stop)
    nc, dbg_names = _CACHE[key]
    in_maps, B = _host_inputs(inputs)
    if stop < 8:
        for m in in_maps:
            for k in ("w_gate", "w_up", "w_down"):
                m[k] = m[k][0:1]
    res = run_bass_kernel_spmd(nc, in_maps, core_ids=list(range(NCORES)))
    outp = np.zeros((B, S, D), np.float32)
    for c in range(NCORES):
        b, hf = c // 2, c % 2
        if "out" in res.results[c]:
            outp[b, hf * TOWN:(hf + 1) * TOWN, :] = res.results[c]["out"]
    if debug:
        return outp, [{n: r["dbg_" + n] for n in dbg_names} for r in res.results]
    return outp
```
